# Optimizing a Trainium2 kernel written in Bass

```python
import functools
import jax, jax.numpy as jnp
from jax import lax
import numpy as np

D_MODEL = 2048
BATCH = 16
SEQ = 2048
DEPTH = 1
DEC_BATCH = 128
DEC_SEQ = 8
PAST_LEN = 16384
PAGE_SIZE = 128

MIX_W = D_MODEL
ATT_W = MIX_W // 2
SGU_W = MIX_W - ATT_W
HEAD_DIM = 64
N_HEADS = ATT_W // HEAD_DIM
N_KV_HEADS = 2
GROUP = N_HEADS // N_KV_HEADS
KV_W = N_KV_HEADS * HEAD_DIM
ROT_DIM = HEAD_DIM // 4
ROPE_THETA = 500000.0
WINDOW = 128
ATT_BLOCK = WINDOW
SGU_HEADS = 8
SGU_HD = SGU_W // SGU_HEADS
CHUNK = 128
IN_DIM = ATT_W + 2 * KV_W + 2 * SGU_W
N_KEYS = 128
N_EXPERTS = N_KEYS * N_KEYS
PEER_HEADS = 8
PEER_QDIM = 256
PEER_HALF = PEER_QDIM // 2
PEER_TOPK = 16
PEER_BLOCK = 128
PLE_DIM = 256
EPS = 1e-6

kernel_name = 'hymba_swa_sgu_peer_decoder_step'


def rmsnorm(x, g):
    xf = x.astype(jnp.float32)
    y = xf * lax.rsqrt(jnp.mean(xf * xf, axis=-1, keepdims=True) + EPS)
    return (y * g.astype(jnp.float32)).astype(x.dtype)


def rope(x, pos):
    half = ROT_DIM // 2
    inv = jnp.power(jnp.float32(ROPE_THETA), -jnp.arange(half, dtype=jnp.float32) * 2.0 / ROT_DIM)
    ang = pos[:, None] * inv[None, :]
    cos = jnp.cos(ang)[:, None, :]
    sin = jnp.sin(ang)[:, None, :]
    xr = x[..., :ROT_DIM].astype(jnp.float32)
    x1, x2 = xr[..., :half], xr[..., half:]
    rot = jnp.concatenate([x1 * cos - x2 * sin, x2 * cos + x1 * sin], axis=-1).astype(x.dtype)
    return jnp.concatenate([rot, x[..., ROT_DIM:]], axis=-1)


def sink_attention(q, k, v, mask, sinks):
    s = jnp.einsum('...qkgd,...skd->...kgqs', q, k).astype(jnp.float32) * (HEAD_DIM ** -0.5)
    s = jnp.where(mask, s, -jnp.inf)
    sink = jnp.broadcast_to(sinks.astype(jnp.float32).reshape(N_KV_HEADS, GROUP)[:, :, None, None],
                            s.shape[:-1] + (1,))
    pr = jax.nn.softmax(jnp.concatenate([s, sink], axis=-1), axis=-1)[..., :-1].astype(v.dtype)
    return jnp.einsum('...kgqs,...skd->...qkgd', pr, v)


def attend_prompt(q, k, v, sinks):
    B, S = q.shape[0], q.shape[1]
    nb = S // ATT_BLOCK
    qb = q.reshape(B, nb, ATT_BLOCK, N_KV_HEADS, GROUP, HEAD_DIM)

    def with_prev(t):
        tb = t.reshape(B, nb, ATT_BLOCK, N_KV_HEADS, HEAD_DIM)
        prev = jnp.pad(tb, ((0, 0), (1, 0), (0, 0), (0, 0), (0, 0)))[:, :-1]
        return jnp.concatenate([prev, tb], axis=2)

    qi = jnp.arange(ATT_BLOCK)[:, None]
    sj = jnp.arange(2 * ATT_BLOCK)[None, :]
    rel = qi + ATT_BLOCK - sj
    band = (rel >= 0) & (rel < WINDOW)
    not_first = jnp.arange(nb)[:, None, None] > 0
    mask = (band[None] & (not_first | (sj >= ATT_BLOCK)[None]))[:, None, None]
    o = sink_attention(qb, with_prev(k), with_prev(v), mask, sinks)
    return o.reshape(B, S, N_KV_HEADS, GROUP, HEAD_DIM), k[:, -WINDOW:], v[:, -WINDOW:]


def attend_sample(q, k, v, sinks, cache_k, cache_v):
    T = q.shape[1]
    C = cache_k.shape[1]
    kc = jnp.concatenate([cache_k, k], axis=1)
    vc = jnp.concatenate([cache_v, v], axis=1)
    i = jnp.arange(T)[:, None]
    j = jnp.arange(C + T)[None, :]
    rel = i + C - j
    mask = (rel >= 0) & (rel < WINDOW)
    o = sink_attention(q, kc, vc, mask, sinks)
    return o, kc[:, T:], vc[:, T:]


def sgu(u, v, w, b):
    B, L = u.shape[0], u.shape[1]
    c = min(L, CHUNK)
    wm = w[:, :c, :c] * jnp.tril(jnp.ones((c, c), dtype=w.dtype))
    vc = v.reshape(B, L // c, c, SGU_HEADS, SGU_HD)
    z = jnp.einsum('hts,bnshd->bnthd', wm, vc) + b[:, :c].T[:, :, None]
    return u * z.reshape(B, L, SGU_HEADS, SGU_HD)


def peer(x, wq, k1, k2, u_tab, v_tab):
    B, L, D = x.shape
    n = B * L
    pad = (-n) % PEER_BLOCK
    xt = jnp.pad(x.reshape(n, D), ((0, pad), (0, 0))).reshape(-1, PEER_BLOCK, D)

    def one_block(xb):
        q = jnp.einsum('td,de->te', xb, wq).reshape(PEER_BLOCK, PEER_HEADS, 2, PEER_HALF)
        s1 = jnp.einsum('thc,nc->thn', q[:, :, 0], k1).astype(jnp.float32)
        s2 = jnp.einsum('thc,nc->thn', q[:, :, 1], k2).astype(jnp.float32)
        v1, i1 = lax.top_k(s1, PEER_TOPK)
        v2, i2 = lax.top_k(s2, PEER_TOPK)
        cand = (v1[..., :, None] + v2[..., None, :]).reshape(PEER_BLOCK, PEER_HEADS, PEER_TOPK * PEER_TOPK)
        sc, ci = lax.top_k(cand, PEER_TOPK)
        e = (jnp.take_along_axis(i1, ci // PEER_TOPK, axis=-1) * N_KEYS
             + jnp.take_along_axis(i2, ci % PEER_TOPK, axis=-1))
        g = jax.nn.softmax(sc, axis=-1).astype(xb.dtype)
        ue = jnp.take(u_tab, e, axis=0)
        act = jax.nn.gelu(jnp.einsum('thkd,td->thk', ue, xb))
        ve = jnp.take(v_tab, e, axis=0)
        return jnp.einsum('thk,thkd->td', g * act, ve)

    out = lax.map(one_block, xt)
    return out.reshape(-1, D)[:n].reshape(B, L, D)


def trunk_layer(x, ple, pos, attend, lw):
    (norm_mix, w_in, q_norm, k_norm, sinks, sgu_norm, sgu_w, sgu_b, attn_out_norm, sgu_out_norm,
     w_out, norm_ffn, peer_wq, peer_k1, peer_k2, peer_u, peer_v, ple_w, ple_gate_norm, ple_gate_w) = lw
    B, L, _ = x.shape
    hn = rmsnorm(x, norm_mix)
    proj = jnp.einsum('bld,de->ble', hn, w_in)
    o1 = ATT_W
    o2 = o1 + KV_W
    o3 = o2 + KV_W
    o4 = o3 + SGU_W
    q = rope(rmsnorm(proj[..., :o1].reshape(B, L, N_HEADS, HEAD_DIM), q_norm), pos)
    k = rope(rmsnorm(proj[..., o1:o2].reshape(B, L, N_KV_HEADS, HEAD_DIM), k_norm), pos)
    v = proj[..., o2:o3].reshape(B, L, N_KV_HEADS, HEAD_DIM)
    su = jax.nn.gelu(proj[..., o3:o4]).reshape(B, L, SGU_HEADS, SGU_HD)
    sv = rmsnorm(jax.nn.gelu(proj[..., o4:]).reshape(B, L, SGU_HEADS, SGU_HD), sgu_norm)
    attn, k_state, v_state = attend(q.reshape(B, L, N_KV_HEADS, GROUP, HEAD_DIM), k, v, sinks)
    attn = attn.reshape(B, L, ATT_W)
    sg = sgu(su, sv, sgu_w, sgu_b).reshape(B, L, SGU_W)
    mixed = jnp.concatenate([rmsnorm(attn, attn_out_norm), rmsnorm(sg, sgu_out_norm)], axis=-1)
    h = x + jnp.einsum('ble,ed->bld', mixed, w_out)
    h = h + peer(rmsnorm(h, norm_ffn), peer_wq, peer_k1, peer_k2, peer_u, peer_v)
    gate = jax.nn.sigmoid(jnp.einsum('bld,de->ble', rmsnorm(h, ple_gate_norm), ple_gate_w))
    h = h + jnp.einsum('blp,pd->bld', ple, ple_w) * gate
    return h, k_state, v_state, sv


def setup_inputs(seed: int = 0) -> dict:
    key = jax.random.key(seed)
    ks = jax.random.split(key, 32)
    f32 = jnp.float32
    C = min(WINDOW, PAST_LEN)

    def nrm(k, shape, scale):
        return jax.random.normal(k, shape, f32) * scale

    def gain(k, n):
        return 1.0 + 0.1 * jax.random.normal(k, (DEPTH, n), f32)

    return {
        'x_prompt': nrm(ks[0], (BATCH, SEQ, D_MODEL), 1.0),
        'x_sample': nrm(ks[1], (DEC_BATCH, DEC_SEQ, D_MODEL), 1.0),
        'cache_k': nrm(ks[2], (DEPTH, DEC_BATCH, C, N_KV_HEADS, HEAD_DIM), 1.0),
        'cache_v': nrm(ks[3], (DEPTH, DEC_BATCH, C, N_KV_HEADS, HEAD_DIM), 1.0),
        'p_prompt': nrm(ks[4], (DEPTH, BATCH, SEQ, PLE_DIM), 1.0),
        'p_sample': nrm(ks[5], (DEPTH, DEC_BATCH, DEC_SEQ, PLE_DIM), 1.0),
        'norm_mix': gain(ks[6], D_MODEL),
        'w_in': nrm(ks[7], (DEPTH, D_MODEL, IN_DIM), D_MODEL ** -0.5),
        'q_norm': gain(ks[8], HEAD_DIM),
        'k_norm': gain(ks[9], HEAD_DIM),
        'sinks': nrm(ks[10], (DEPTH, N_HEADS), 1.0),
        'sgu_norm': gain(ks[11], SGU_HD),
        'sgu_w': nrm(ks[12], (DEPTH, SGU_HEADS, CHUNK, CHUNK), CHUNK ** -0.5),
        'sgu_b': 1.0 + 0.1 * jax.random.normal(ks[13], (DEPTH, SGU_HEADS, CHUNK), f32),
        'attn_out_norm': gain(ks[14], ATT_W),
        'sgu_out_norm': gain(ks[15], SGU_W),
        'w_out': nrm(ks[16], (DEPTH, MIX_W, D_MODEL), MIX_W ** -0.5),
        'norm_ffn': gain(ks[17], D_MODEL),
        'peer_wq': nrm(ks[18], (DEPTH, D_MODEL, PEER_HEADS * PEER_QDIM), D_MODEL ** -0.5),
        'peer_k1': nrm(ks[19], (DEPTH, N_KEYS, PEER_HALF), PEER_HALF ** -0.5),
        'peer_k2': nrm(ks[20], (DEPTH, N_KEYS, PEER_HALF), PEER_HALF ** -0.5),
        'peer_u': nrm(ks[21], (DEPTH, N_EXPERTS, D_MODEL), D_MODEL ** -0.5),
        'peer_v': nrm(ks[22], (DEPTH, N_EXPERTS, D_MODEL), PEER_HEADS ** -0.5),
        'ple_w': nrm(ks[23], (DEPTH, PLE_DIM, D_MODEL), PLE_DIM ** -0.5),
        'ple_gate_norm': gain(ks[24], D_MODEL),
        'ple_gate_w': nrm(ks[25], (DEPTH, D_MODEL, D_MODEL), D_MODEL ** -0.5),
    }


def reference(x_prompt, x_sample, cache_k, cache_v, p_prompt, p_sample, norm_mix, w_in, q_norm, k_norm,
              sinks, sgu_norm, sgu_w, sgu_b, attn_out_norm, sgu_out_norm, w_out, norm_ffn, peer_wq,
              peer_k1, peer_k2, peer_u, peer_v, ple_w, ple_gate_norm, ple_gate_w):
    pos_p = jnp.arange(x_prompt.shape[1], dtype=jnp.float32)
    pos_s = PAST_LEN + jnp.arange(x_sample.shape[1], dtype=jnp.float32)
    hp, hs = x_prompt, x_sample
    kp_l, vp_l, ks_l, vs_l, sv_l = [], [], [], [], []
    for i in range(DEPTH):
        lw = (norm_mix[i], w_in[i], q_norm[i], k_norm[i], sinks[i], sgu_norm[i], sgu_w[i], sgu_b[i],
              attn_out_norm[i], sgu_out_norm[i], w_out[i], norm_ffn[i], peer_wq[i], peer_k1[i],
              peer_k2[i], peer_u[i], peer_v[i], ple_w[i], ple_gate_norm[i], ple_gate_w[i])
        hp, kp, vp, _ = trunk_layer(hp, p_prompt[i], pos_p, attend_prompt, lw)
        attend_s = functools.partial(attend_sample, cache_k=cache_k[i], cache_v=cache_v[i])
        hs, kss, vss, svs = trunk_layer(hs, p_sample[i], pos_s, attend_s, lw)
        kp_l.append(kp)
        vp_l.append(vp)
        ks_l.append(kss)
        vs_l.append(vss)
        sv_l.append(svs)
    win_k_prompt = jnp.stack(kp_l)
    win_v_prompt = jnp.stack(vp_l)
    win_k_sample = jnp.stack(ks_l)
    win_v_sample = jnp.stack(vs_l)
    sgu_v_sample = jnp.stack(sv_l)
    return (hp, hs, win_k_prompt, win_v_prompt, win_k_sample, win_v_sample, sgu_v_sample)
```

```python
import os
import types
import numpy as np
from contextlib import ExitStack
import concourse.bass as bass
import concourse.mybir as mybir
from concourse.bass_utils import run_bass_kernel_spmd

F32 = mybir.dt.float32
BF16 = mybir.dt.bfloat16
U32 = mybir.dt.uint32
ALU = mybir.AluOpType
AF = mybir.ActivationFunctionType
AX = mybir.AxisListType

D = 2048
NCORE = 8
NPB = 32
NROW = 4224
EPS = 1e-6
NEG = -30000.0
CG = 4
TG = 8
SAME_ENGINE_FIFO = False


def _freeze(fn):
    if fn.__closure__ is None:
        return fn
    cells = []
    for c in fn.__closure__:
        try:
            cells.append(types.CellType(c.cell_contents))
        except ValueError:
            cells.append(c)
    g = types.FunctionType(fn.__code__, fn.__globals__, fn.__name__, fn.__defaults__, tuple(cells))
    g.__kwdefaults__ = fn.__kwdefaults__
    return g


class Sch:
    CE = ['pe', 'act', 'dve', 'pool']

    def __init__(self, nc, es):
        self.nc, self.es = nc, es
        self.ops = {e: [] for e in self.CE + ['sp']}
        self.res = {}
        self.esem = {e: es.enter_context(nc.semaphore("s_" + e)) for e in self.CE}
        self.dsems = {}
        self.pend = {e: set() for e in self.CE + ['sp']}

    def _ds(self, name):
        if name not in self.dsems:
            self.dsems[name] = [self.es.enter_context(self.nc.semaphore("d_" + name)), 0]
        return self.dsems[name]

    def add(self, eng, fn, r=(), w=(), dma=None):
        idx = len(self.ops[eng])
        deps = set(self.pend[eng])
        self.pend[eng] = set()
        for x in r:
            st = self.res.get(x)
            if st and st[0] is not None:
                deps.add(st[0])
        for x in w:
            st = self.res.get(x)
            if st:
                if st[0] is not None:
                    deps.add(st[0])
                deps.update(st[1])
        if dma is not None:
            d = self._ds(dma)
            d[1] += 16
            tok = ('D', dma, d[1])
        else:
            tok = (eng, idx)
        for x in r:
            self.res.setdefault(x, [None, []])[1].append(tok)
        for x in w:
            self.res[x] = [tok, []]
        self.ops[eng].append(dict(fn=_freeze(fn), deps=deps, dma=dma, sig=False, cnt=0))
        return tok

    def barrier(self, sp=True):
        toks = set()
        for e in self.CE:
            if self.ops[e]:
                toks.add((e, len(self.ops[e]) - 1))
        for name, d in self.dsems.items():
            if d[1] > 0:
                toks.add(('D', name, d[1]))
        for e in self.CE + (['sp'] if sp else []):
            self.pend[e] |= toks

    def emit(self, block):
        for e in self.ops:
            for op in self.ops[e]:
                for d in op['deps']:
                    if d[0] != 'D':
                        self.ops[d[0]][d[1]]['sig'] = True
        for e in self.CE:
            c = 0
            for op in self.ops[e]:
                if op['sig']:
                    c += 1
                op['cnt'] = c

        def mk(en):
            def body(eng):
                waited = {}
                for op in self.ops[en]:
                    need = {}
                    for d in op['deps']:
                        if d[0] == 'D':
                            sem, val = self.dsems[d[1]][0], d[2]
                        else:
                            if d[0] == en and (en == 'pe' or SAME_ENGINE_FIFO):
                                continue
                            sem, val = self.esem[d[0]], self.ops[d[0]][d[1]]['cnt']
                        if need.get(sem.num, (None, 0))[1] < val:
                            need[sem.num] = (sem, val)
                    for num, (sem, val) in need.items():
                        if waited.get(num, 0) < val:
                            eng.wait_ge(sem, val)
                            waited[num] = val
                    ins = op['fn'](eng)
                    if op['dma'] is not None:
                        ins.then_inc(self.dsems[op['dma']][0], 16)
                    elif op['sig']:
                        ins.then_inc(self.esem[en], 1)
                if en == 'sp':
                    for name, d in self.dsems.items():
                        if d[1] > 0:
                            eng.wait_ge(d[0], d[1])
            return body
        block.sync(mk('sp'))
        block.tensor(mk('pe'))
        block.scalar(mk('act'))
        block.vector(mk('dve'))
        block.gpsimd(mk('pool'))


def bc(ap, shape, axis):
    return ap.unsqueeze(axis).to_broadcast(shape)


def build_nc(sb_list=None, dbg=False):
    SKIP = os.environ.get('KSKIP', '')
    if sb_list is None:
        sb_list = list(range(17))
    nc = bass.Bass("TRN2", target_bir_lowering=False)

    def din(name, shape, dt=F32):
        return nc.dram_tensor(name, list(shape), dt, kind="ExternalInput").ap()

    def dout(name, shape):
        return nc.dram_tensor(name, list(shape), F32, kind="ExternalOutput").ap()

    x_d = din("x", [NROW, D]); p_d = din("p", [NROW, 256])
    ck_d = din("ck", [16, 128, 128]); cv_d = din("cv", [16, 128, 128])
    win_d = din("w_in", [D, 3328]); wout_d = din("w_out", [D, D]); wq_d = din("wq", [D, D])
    gw_d = din("gw", [D, D]); plew_d = din("plew", [256, D])
    U_d = din("U", [16384, D]); V_d = din("V", [16384, D])
    k1_d = din("k1", [128, 128]); k2_d = din("k2", [128, 128])
    gcol_d = din("gcol", [128, 4, 16])
    gffn_d = din("gffn", [1, D])
    qn_d = din("qn", [1, 64]); kn_d = din("kn", [1, 64]); sgn_d = din("sgn", [1, 128]); sink_d = din("sinks", [1, 16])
    sguw_d = din("sguw", [8, 128, 128]); sgub_d = din("sgub", [8, 128])
    ident_d = din("ident", [128, 128]); iota_d = din("iota", [128, 128])
    mp_d = din("maskp", [128, 2, 128]); ms_d = din("masks", [128, 17, 128])
    tril_d = din("tril", [128, 128]); bdT_d = din("bdT", [128, 128]); e8_d = din("e8", [8, 128])
    csp_d = din("csp", [128, 16, 2, 8]); css_d = din("css", [128, 2, 8])

    y_d = dout("y", [NROW, D])
    wkp_d = dout("wkp", [2, 128, 128]); wvp_d = dout("wvp", [2, 128, 128])
    wks_d = dout("wks", [16, 128, 128]); wvs_d = dout("wvs", [16, 128, 128])
    svs_d = dout("svs", [128, 1024])
    if dbg:
        dh_d = dout("dbg_h", [256, D]); dh2_d = dout("dbg_h2", [256, D])

    win_s = nc.dram_tensor("win_s", [D, 3328], BF16).ap()
    wout_s = nc.dram_tensor("wout_s", [D, D], BF16).ap()
    wq_s = nc.dram_tensor("wq_s", [D, D], BF16).ap()
    gw_s = nc.dram_tensor("gw_s", [D, D], BF16).ap()
    UT_s = nc.dram_tensor("UT_s", [128, 128, D], BF16).ap()
    VB_s = nc.dram_tensor("VB_s", [128, 128, D], BF16).ap()

    es = ExitStack()
    with es:
        def sb_(name, shape, dt):
            return es.enter_context(nc.sbuf_tensor("t_" + name, list(shape), dt))

        identb = sb_("identb", [128, 128], BF16)
        iotab = sb_("iotab", [128, 128], BF16)
        iota16 = sb_("iota16", [128, 16], F32)
        maskp = sb_("maskp", [128, 2, 128], BF16)
        masks = sb_("masks", [128, 17, 128], BF16)
        sguTp = sb_("sguTp", [128, 8, 128], BF16)
        sguTs = sb_("sguTs", [128, 8, 128], BF16)
        bTp = sb_("bTp", [128, 8], F32)
        bTs = sb_("bTs", [128, 8], F32)
        qg = sb_("qg", [128, 64], F32); kg = sb_("kg", [128, 64], F32); sgn = sb_("sgn", [128, 128], F32)
        esink = sb_("esink", [128, 16], F32)
        k12T = sb_("k12T", [128, 2, 128], BF16)
        csp = sb_("csp", [128, 16, 2, 8], F32); css = sb_("css", [128, 2, 8], F32)
        gcol = sb_("gcol", [128, 4, 16], F32)
        plewb = sb_("plewb", [128, 2, D], BF16)
        kTc = sb_("kTc", [128, 16, 128], BF16)
        vaugc = sb_("vaugc", [128, 16, 2, 65], BF16)
        kTr = sb_("kTr", [128, 3, 128], BF16)
        vaugr = sb_("vaugr", [128, 3, 2, 65], BF16)
        xin = sb_("xin", [128, 2, D], F32)
        pin = sb_("pin", [128, 2, 256], F32)
        stat = sb_("stat", [128, 64], F32)
        mhalf = sb_("mhalf", [128, 16], F32)
        wslot = [sb_("wslot%d" % i, [128, 8192], BF16) for i in range(3)]
        ARENA = 24000
        arena = sb_("arena", [128, ARENA], F32)
        psum = es.enter_context(nc.psum_tensor("psum", [128, 4096], F32))

        def bank(k, n=1):
            return psum[:, k * 512:(k + n) * 512]

        def bankb(k, n=1):
            return bank(k, n).bitcast(BF16)

        class Ar:
            def __init__(s):
                s.o = 0

            def f(s, n):
                a = arena[:, s.o:s.o + n]
                s.o += n
                assert s.o <= ARENA, s.o
                return a

            def b(s, n):
                m = (n + 1) // 2
                a = arena[:, s.o:s.o + m].bitcast(BF16)
                s.o += m
                assert s.o <= ARENA, s.o
                return a

        S = Sch(nc, es)
        A = S.add
        wsl_i = [0]

        def next_slot():
            i = wsl_i[0] % 3
            wsl_i[0] += 1
            return i

        def rsqrt_ops(dst, src, n, eng='dve', rr=(), ww=()):
            A(eng, lambda e: e.tensor_scalar(out=dst, in0=src, scalar1=1.0 / n, scalar2=EPS, op0=ALU.mult, op1=ALU.add), r=rr, w=ww)
            k_ = dst.shape[-1]
            A('pool', lambda e: e.tensor_tensor(out=dst, in0=dst, in1=mhalf[:, 0:k_], op=ALU.pow), r=list(ww) + ['mhalf'], w=ww)

        def transposes16(src_bf, dstT, tcols, pbk, rr, ww, ev_eng):
            pb = bankb(pbk, 2)
            for dc in range(16):
                A('pe', lambda e, dc=dc: e.transpose(out=pb[:, dc * 128:(dc + 1) * 128], in_=src_bf[:, dc * 128:(dc + 1) * 128], identity=identb[:]),
                  r=rr + ['identb'], w=['pb%d' % pbk, 'pb%d' % (pbk + 1)])
            A(ev_eng, lambda e: (e.tensor_copy(out=dstT[:, :, tcols], in_=pb.rearrange("p (a b) -> p a b", b=128)) if ev_eng != 'act'
                                 else e.copy(out=dstT[:, :, tcols], in_=pb.rearrange("p (a b) -> p a b", b=128))),
              r=['pb%d' % pbk, 'pb%d' % (pbk + 1)], w=ww)

        a = Ar()
        A('pool', lambda e: e.memset(mhalf[:], -0.5), w=['mhalf'])
        t_f = a.f(128 * 17)
        A('sp', lambda e: e.dma_start(out=t_f[:, 0:128], in_=ident_d), w=['t_f'], dma='c0')
        A('dve', lambda e: e.tensor_copy(out=identb[:], in_=t_f[:, 0:128]), r=['t_f'], w=['identb'])
        A('sp', lambda e: e.dma_start(out=t_f[:, 0:128], in_=iota_d), w=['t_f'], dma='c0')
        A('dve', lambda e: e.tensor_copy(out=iotab[:], in_=t_f[:, 0:128]), r=['t_f'], w=['iotab'])
        A('dve', lambda e: e.tensor_copy(out=iota16[:], in_=t_f[:, 0:16]), r=['t_f'], w=['iota16'])
        A('sp', lambda e: e.dma_start(out=t_f[:, 0:256].rearrange("p (a b) -> p a b", b=128), in_=mp_d), w=['t_f'], dma='c0')
        A('dve', lambda e: e.tensor_copy(out=maskp[:], in_=t_f[:, 0:256].rearrange("p (a b) -> p a b", b=128)), r=['t_f'], w=['maskp'])
        A('sp', lambda e: e.dma_start(out=t_f[:, 0:128 * 17].rearrange("p (a b) -> p a b", b=128), in_=ms_d), w=['t_f'], dma='c0')
        A('dve', lambda e: e.tensor_copy(out=masks[:], in_=t_f[:, 0:128 * 17].rearrange("p (a b) -> p a b", b=128)), r=['t_f'], w=['masks'])
        for (dst, src, tg_) in ((qg, qn_d, 'qg'), (kg, kn_d, 'kg'), (sgn, sgn_d, 'sgn')):
            A('sp', lambda e, dst=dst, src=src: e.dma_start(out=dst[:], in_=src.partition_broadcast(128)), w=[tg_], dma='c1')
        A('sp', lambda e: e.dma_start(out=esink[:], in_=sink_d.partition_broadcast(128)), w=['esink'], dma='c2')
        A('act', lambda e: e.activation(out=esink[:], in_=esink[:], func=AF.Exp), r=['esink'], w=['esink'])
        A('sp', lambda e: e.dma_start(out=csp[:], in_=csp_d), w=['csp'], dma='c1')
        A('sp', lambda e: e.dma_start(out=css[:], in_=css_d), w=['css'], dma='c1')
        A('sp', lambda e: e.dma_start(out=gcol[:], in_=gcol_d), w=['gcol'], dma='c1')
        A('sp', lambda e: e.dma_start(out=bTp[:], in_=sgub_d.rearrange("h t -> t h"), allow_slow_non_contiguous=True), w=['bTp'], dma='c1')
        for b in range(16):
            A('sp', lambda e, b=b: e.dma_start(out=bTs[b * 8:(b + 1) * 8, :], in_=sgub_d[:, 0:8].rearrange("h t -> t h"), allow_slow_non_contiguous=True), w=['bTs'], dma='c1')
        t_b = a.b(128 * 8)
        for i, kd in enumerate((k1_d, k2_d)):
            A('sp', lambda e, kd=kd: e.dma_start(out=t_f[:, 0:128], in_=kd), w=['t_f'], dma='c0')
            A('dve', lambda e: e.tensor_copy(out=t_b[:, 0:128], in_=t_f[:, 0:128]), r=['t_f'], w=['t_b'])
            A('pe', lambda e: e.transpose(out=bankb(0)[:, 0:128], in_=t_b[:, 0:128], identity=identb[:]), r=['t_b', 'identb'], w=['pb0'])
            A('dve', lambda e, i=i: e.tensor_copy(out=k12T[:, i, :], in_=bankb(0)[:, 0:128]), r=['pb0'], w=['k12T'])
        trilf = a.f(128)
        bdTf = a.f(128)
        A('sp', lambda e: e.dma_start(out=trilf, in_=tril_d), w=['trilf'], dma='c3')
        A('sp', lambda e: e.dma_start(out=bdTf, in_=bdT_d), w=['bdTf'], dma='c4')
        for h in range(8):
            A('sp', lambda e, h=h: e.dma_start(out=t_f[:, 0:128], in_=sguw_d[h]), w=['t_f'], dma='c0')
            A('dve', lambda e: e.tensor_tensor(out=t_b[:, 0:128], in0=t_f[:, 0:128], in1=trilf, op=ALU.mult), r=['t_f', 'trilf'], w=['t_b'])
            A('pe', lambda e: e.transpose(out=bankb(0)[:, 0:128], in_=t_b[:, 0:128], identity=identb[:]), r=['t_b', 'identb'], w=['pb0'])
            A('dve', lambda e, h=h: e.tensor_copy(out=sguTp[:, h, :], in_=bankb(0)[:, 0:128]), r=['pb0'], w=['sguTp'])
        e8f = a.f(128); e8b = a.b(128); w8f = a.f(128); w8b = a.b(128)
        A('sp', lambda e: e.dma_start(out=e8f[0:8, :], in_=e8_d), w=['e8f'], dma='c5')
        A('dve', lambda e: e.tensor_copy(out=e8b[0:8, :], in_=e8f[0:8, :]), r=['e8f'], w=['e8b'])
        for h in range(8):
            A('sp', lambda e, h=h: e.dma_start(out=w8f[0:8, :].rearrange("p (a b) -> p a b", b=8),
                                               in_=bc(sguw_d[h, 0:8, 0:8], [8, 16, 8], 1)), w=['w8f'], dma='c9')
            A('dve', lambda e: e.tensor_copy(out=w8b[0:8, :], in_=w8f[0:8, :]), r=['w8f'], w=['w8b'])
            A('pe', lambda e: e.matmul(out=bank(2)[:, 0:128], lhsT=w8b[0:8, :], rhs=e8b[0:8, :], start=True, stop=True), r=['w8b', 'e8b'], w=['pb2'])
            A('dve', lambda e, h=h: e.tensor_tensor(out=sguTs[:, h, :], in0=bank(2)[:, 0:128], in1=bdTf, op=ALU.mult), r=['pb2', 'bdTf'], w=['sguTs'])
        plf = a.f(D)
        for kc in range(2):
            A('sp', lambda e, kc=kc: e.dma_start(out=plf, in_=plew_d[kc * 128:(kc + 1) * 128, :]), w=['plf'], dma='c7')
            A('dve', lambda e, kc=kc: e.tensor_copy(out=plewb[:, kc, :], in_=plf), r=['plf'], w=['plewb'])
        ckf = a.f(16 * 128).rearrange("p (a b) -> p a b", b=128)
        ckb = a.b(16 * 128).rearrange("p (a b) -> p a b", b=128)
        A('sp', lambda e: e.dma_start(out=ckf, in_=ck_d.rearrange("b s c -> s b c")), w=['ckf'], dma='c8')
        A('dve', lambda e: e.tensor_copy(out=ckb, in_=ckf), r=['ckf'], w=['ckb'])
        for b in range(16):
            A('pe', lambda e, b=b: e.transpose(out=bankb(0)[:, (b % 8) * 128:(b % 8 + 1) * 128], in_=ckb[:, b, :], identity=identb[:]), r=['ckb', 'identb'], w=['pb0'])
            if b % 8 == 7:
                A('dve', lambda e, b=b: e.tensor_copy(out=kTc[:, b - 7:b + 1, :], in_=bankb(0).rearrange("p (a b) -> p a b", b=128)), r=['pb0'], w=['kTc'])
        A('sp', lambda e: e.dma_start(out=ckf, in_=cv_d.rearrange("b s c -> s b c")), w=['ckf'], dma='c8')
        A('dve', lambda e: e.memset(vaugc[:], 1.0), w=['vaugc'])
        A('dve', lambda e: e.tensor_copy(out=vaugc[:, :, :, 0:64], in_=ckf.rearrange("p a (k c) -> p a k c", c=64)), r=['ckf'], w=['vaugc'])
        A('dve', lambda e: e.memset(vaugr[:], 1.0), w=['vaugr'])
        A('sp', lambda e: e.dma_start(out=wks_d[:, 0:120, :], in_=ck_d[:, 8:128, :]), dma='o_w')
        A('sp', lambda e: e.dma_start(out=wvs_d[:, 0:120, :], in_=cv_d[:, 8:128, :]), dma='o_w')
        S.barrier()

        a = Ar()
        NBC = 3
        cvf = [a.f(3328) for _ in range(NBC)]
        cvb = [a.b(3328) for _ in range(NBC)]
        jobs = []
        for (src, dst, ncol, gi) in ((win_d, win_s, 3328, 0), (wout_d, wout_s, D, 1), (wq_d, wq_s, D, 2), (gw_d, gw_s, D, 3)):
            for dc in range(16):
                jobs.append((src, dst, ncol, gi, dc))

        def cv_store(k):
            src, dst, ncol, gi, dc = jobs[k]
            s = k % NBC
            A('sp', lambda e: e.dma_start(out=dst[dc * 128:(dc + 1) * 128, :], in_=cvb[s][:, 0:ncol]), r=['cvb%d' % s], dma='cs%d' % s)

        for k, (src, dst, ncol, gi, dc) in enumerate(jobs):
            s = k % NBC
            A('sp', lambda e: e.dma_start(out=cvf[s][:, 0:ncol], in_=src[dc * 128:(dc + 1) * 128, :]), w=['cvf%d' % s], dma='cv%d' % s)
            if k >= 2:
                cv_store(k - 2)
            if dc % 2:
                A('act', lambda e: e.activation(out=cvb[s][:, 0:ncol], in_=cvf[s][:, 0:ncol], func=AF.Copy, scale=gcol[:, gi, dc:dc + 1]),
                  r=['cvf%d' % s, 'gcol'], w=['cvb%d' % s])
            else:
                A('dve', lambda e: e.tensor_scalar(out=cvb[s][:, 0:ncol], in0=cvf[s][:, 0:ncol], scalar1=gcol[:, gi, dc:dc + 1], scalar2=None, op0=ALU.mult),
                  r=['cvf%d' % s, 'gcol'], w=['cvb%d' % s])
        cv_store(len(jobs) - 2)
        cv_store(len(jobs) - 1)
        S.barrier()
        a = Ar()
        NBU = 3
        uf = [a.f(D) for _ in range(NBU)]
        ub = [a.b(D) for _ in range(NBU)]
        ut = [a.b(D) for _ in range(NBU)]
        vf_ = [a.f(D) for _ in range(NBU)]
        vb_ = [a.b(D) for _ in range(NBU)]
        gfr2 = a.f(D)
        A('sp', lambda e: e.dma_start(out=gfr2, in_=gffn_d.partition_broadcast(128)), w=['gfr2'], dma='c6')
        Uv = U_d.rearrange("(i j) d -> j i d", j=128)
        Vv = V_d.rearrange("(i j) d -> j i d", j=128)

        def uv_store(j):
            s = j % NBU
            A('sp', lambda e: e.dma_start(out=UT_s[j], in_=ut[s]), r=['ut%d' % s], dma='us%d' % s)
            A('sp', lambda e: e.dma_start(out=VB_s[j], in_=vb_[s]), r=['vb%d' % s], dma='vs%d' % s)

        for j in range(128):
            s = j % NBU
            A('sp', lambda e: e.dma_start(out=uf[s], in_=Uv[j]), w=['uf%d' % s], dma='uf%d' % s)
            A('sp', lambda e: e.dma_start(out=vf_[s], in_=Vv[j]), w=['vf%d' % s], dma='vf%d' % s)
            if j >= 2:
                uv_store(j - 2)
            A('dve', lambda e: e.tensor_tensor(out=ub[s], in0=uf[s], in1=gfr2, op=ALU.mult), r=['uf%d' % s, 'gfr2'], w=['ub%d' % s])
            pk = 2 * s
            pb = bankb(pk, 2)
            for dc in range(16):
                A('pe', lambda e, dc=dc: e.transpose(out=pb[:, dc * 128:(dc + 1) * 128], in_=ub[s][:, dc * 128:(dc + 1) * 128], identity=identb[:]),
                  r=['ub%d' % s, 'identb'], w=['pb%d' % pk, 'pb%d' % (pk + 1)])
            A('act', lambda e: e.copy(out=ut[s], in_=pb), r=['pb%d' % pk, 'pb%d' % (pk + 1)], w=['ut%d' % s])
            A('act', lambda e: e.copy(out=vb_[s], in_=vf_[s]), r=['vf%d' % s], w=['vb%d' % s])
        uv_store(126)
        uv_store(127)
        S.barrier()

        win_v = win_s.rearrange("(dc p) c -> p dc c", p=128)
        wout_v = wout_s.rearrange("(dc p) c -> p dc c", p=128)
        wq_v = wq_s.rearrange("(dc p) c -> p dc c", p=128)
        gw_v = gw_s.rearrange("(dc p) c -> p dc c", p=128)

        def load_w(view, c0, w):
            i = next_slot()
            ws = wslot[i][:, 0:16 * w].rearrange("p (a b) -> p a b", b=w)
            A('sp', lambda e: e.dma_start(out=ws, in_=view[:, :, c0:c0 + w]), r=['scr'], w=['ws%d' % i], dma='ws%d' % i)
            return ws, 'ws%d' % i

        for sb in sb_list:
            sample = (sb == 16)
            ntb = 1 if sample else 2
            T = 128 * ntb
            r0 = sb * 256
            a = Ar()
            xT_f = a.f(16 * 128)
            xT = xT_f.bitcast(BF16).rearrange("p (a b) -> p a b", b=256)
            mixT = a.b(16 * 256).rearrange("p (a b) -> p a b", b=256)
            tmpb = a.b(D)
            junk = a.f(D)
            qf = a.f(2 * 1024).rearrange("p (a b) -> p a b", b=1024)
            qb = a.b(2 * 1024).rearrange("p (a b) -> p a b", b=1024)
            kvf = a.f(2 * 256).rearrange("p (a b) -> p a b", b=256)
            kb = a.b(2 * 128).rearrange("p (a b) -> p a b", b=128)
            su = a.f(2 * 1024).rearrange("p (a b) -> p a b", b=1024)
            svf = a.f(2 * 1024).rearrange("p (a b) -> p a b", b=1024)
            svb = a.b(2 * 1024).rearrange("p (a b) -> p a b", b=1024)
            tmpf = a.f(1024)
            rt = a.f(128)
            tk = [a.f(128) for _ in range(2)]
            qT = a.b(2 * 1024).rearrange("p (t h q) -> p t h q", t=2, h=8)
            pT = [a.b(16 * 128).rearrange("p (h q) -> p h q", q=128) for _ in range(2)]
            attnf = a.f(1024)
            mixed_f = a.f(D)
            mixed = mixed_f.bitcast(BF16).rearrange("p (a b) -> p a b", b=D)
            tq = [xT_f[:, tb * 1024:(tb + 1) * 1024] for tb in range(2)]
            tsv = [mixed_f[:, tb * 1024:(tb + 1) * 1024] for tb in range(2)]

            for tb in range(ntb):
                A('sp', lambda e, tb=tb: e.dma_start(out=xin[:, tb, :], in_=x_d[r0 + tb * 128:r0 + (tb + 1) * 128, :]), w=['xin%d' % tb], dma='xi%d' % tb)
                A('sp', lambda e, tb=tb: e.dma_start(out=pin[:, tb, :], in_=p_d[r0 + tb * 128:r0 + (tb + 1) * 128, :]), w=['pin%d' % tb], dma='pi%d' % tb)
            for tb in range(ntb):
                X = 'xin%d' % tb
                A('act', lambda e, tb=tb: e.activation(out=junk, in_=xin[:, tb, :], func=AF.Square, accum_out=stat[:, 16 + tb:17 + tb]), r=[X], w=['junk', 'st_a%d' % tb])
                rsqrt_ops(stat[:, tb:tb + 1], stat[:, 16 + tb:17 + tb], D, rr=['st_a%d' % tb], ww=['rstd1_%d' % tb])
                A('dve', lambda e, tb=tb: e.tensor_copy(out=tmpb, in_=xin[:, tb, :]), r=[X], w=['tmpb'])
                transposes16(tmpb, xT, slice(tb * 128, (tb + 1) * 128), 0, ['tmpb'], ['xT'], 'dve')
            slices = [(0, 512, 'q'), (512, 512, 'q'), (1024, 256, 'kv'), (2304, 512, 'sv'), (2816, 512, 'sv'), (1280, 512, 'su'), (1792, 512, 'su')]
            for si, (c0, w, kind) in enumerate(slices):
                ws, wr = load_w(win_v, c0, w)
                for tb in range(ntb):
                    pk = 2 + (si * 2 + tb) % 2
                    ps = bank(pk)[:, 0:w]
                    for dc in range(16):
                        A('pe', lambda e, dc=dc, tb=tb, ps=ps, ws=ws: e.matmul(out=ps, lhsT=xT[:, dc, tb * 128:(tb + 1) * 128], rhs=ws[:, dc, :], start=(dc == 0), stop=(dc == 15)),
                          r=['xT', wr], w=['pb%d' % pk])
                    rs = stat[:, tb:tb + 1]
                    if kind == 'q':
                        dst, fn, wn = qf[:, tb, c0:c0 + w], AF.Copy, 'qf%d' % tb
                    elif kind == 'kv':
                        dst, fn, wn = kvf[:, tb, :], AF.Copy, 'kvf%d' % tb
                    elif kind == 'su':
                        dst, fn, wn = su[:, tb, c0 - 1280:c0 - 1280 + w], AF.Gelu_apprx_tanh, 'su%d' % tb
                    else:
                        dst, fn, wn = svf[:, tb, c0 - 2304:c0 - 2304 + w], AF.Gelu_apprx_tanh, 'svf%d' % tb
                    A('act', lambda e, dst=dst, fn=fn, ps=ps, rs=rs: e.activation(out=dst, in_=ps, func=fn, scale=rs), r=['pb%d' % pk, 'rstd1_%d' % tb], w=[wn])

            TF = ['tmpf', 'tmpf1', 'tmpf2', 'tmpf3']

            def headnorm_ops(buf, nh, hd, gain, gtag, btag, scr, stag, rtc, rtag, extra_w=()):
                v3 = buf.rearrange("p (h c) -> p h c", c=hd)
                sc_ = scr[:, 0:nh * hd]
                t3 = sc_.rearrange("p (h c) -> p h c", c=hd)
                return [
                    ('dve', lambda e: e.tensor_tensor(out=sc_, in0=buf, in1=buf, op=ALU.mult), [btag], [stag] + list(extra_w)),
                    ('dve', lambda e: e.tensor_reduce(out=rtc, in_=t3, axis=AX.X, op=ALU.add), [stag], [rtag]),
                    ('dve', lambda e: e.tensor_scalar(out=rtc, in0=rtc, scalar1=1.0 / hd, scalar2=EPS, op0=ALU.mult, op1=ALU.add), [rtag], [rtag]),
                    ('pool', lambda e: e.tensor_tensor(out=rtc, in0=rtc, in1=mhalf[:, 0:nh], op=ALU.pow), [rtag, 'mhalf'], [rtag]),
                    ('dve', lambda e: e.tensor_tensor(out=v3, in0=v3, in1=bc(rtc, [128, nh, hd], 2), op=ALU.mult), [btag, rtag], [btag]),
                    ('dve', lambda e: e.tensor_tensor(out=v3, in0=v3, in1=bc(gain, [128, nh, hd], 1), op=ALU.mult), [btag, gtag], [btag]),
                ]

            def rope_ops(buf, nh, cs, btag, scr, stag):
                v3 = buf.rearrange("p (h c) -> p h c", c=64)
                x1, x2 = v3[:, :, 0:8], v3[:, :, 8:16]
                cb = bc(cs[:, 0, :], [128, nh, 8], 1)
                sn = bc(cs[:, 1, :], [128, nh, 8], 1)
                n8 = nh * 8
                t = [scr[:, i * n8:(i + 1) * n8].rearrange("p (h c) -> p h c", c=8) for i in range(4)]
                st = [stag + '_%d' % i for i in range(4)]
                return [
                    ('dve', lambda e: e.tensor_tensor(out=t[0], in0=x1, in1=cb, op=ALU.mult), [btag, stag], [st[0]]),
                    ('dve', lambda e: e.tensor_tensor(out=t[1], in0=x2, in1=sn, op=ALU.mult), [btag, stag], [st[1]]),
                    ('dve', lambda e: e.tensor_tensor(out=t[2], in0=x2, in1=cb, op=ALU.mult), [btag, stag], [st[2]]),
                    ('dve', lambda e: e.tensor_tensor(out=t[3], in0=x1, in1=sn, op=ALU.mult), [btag, stag], [st[3]]),
                    ('dve', lambda e: e.tensor_tensor(out=x1, in0=t[0], in1=t[1], op=ALU.subtract), st, [btag]),
                    ('dve', lambda e: e.tensor_tensor(out=x2, in0=t[2], in1=t[3], op=ALU.add), st, [btag]),
                ]

            chains, delays = [], []
            for tb in range(ntb):
                gb = sb * 2 + tb
                blk = gb % 16
                cs = css[:] if sample else csp[:, blk, :, :]
                slot = gb % 3
                QF, KVF, SVF = 'qf%d' % tb, 'kvf%d' % tb, 'svf%d' % tb
                cq = headnorm_ops(qf[:, tb, :], 16, 64, qg[:], 'qg', QF, tq[tb], 'tq%d' % tb, rt[:, tb * 16:tb * 16 + 16], 'rtq%d' % tb, extra_w=['xT'])
                cq += rope_ops(qf[:, tb, :], 16, cs, QF, tq[tb], 'tq%d' % tb)
                cq.append(('dve', lambda e, tb=tb: e.tensor_copy(out=qb[:, tb, :].rearrange("p (h g c) -> p g h c", g=2, c=64),
                                                                 in_=qf[:, tb, :].rearrange("p (g h c) -> p g h c", g=2, c=64)), [QF], ['qb%d' % tb]))
                ck = headnorm_ops(kvf[:, tb, 0:128], 2, 64, kg[:], 'kg', KVF, tk[tb], 'tk%d' % tb, rt[:, 32 + tb * 2:34 + tb * 2], 'rtk%d' % tb)
                ck += rope_ops(kvf[:, tb, 0:128], 2, cs, KVF, tk[tb], 'tk%d' % tb)
                ck.append(('dve', lambda e, tb=tb: e.tensor_copy(out=kb[:, tb, :], in_=kvf[:, tb, 0:128]), [KVF], ['kb%d' % tb]))
                ck.append(('dve', lambda e, tb=tb, slot=slot: e.tensor_copy(out=vaugr[:, slot, :, 0:64], in_=kvf[:, tb, 128:256].rearrange("p (k c) -> p k c", c=64)),
                           [KVF], ['vaug%d' % slot]))
                csv_ = headnorm_ops(svf[:, tb, :], 8, 128, sgn[:], 'sgn', SVF, tsv[tb], 'mixed%d' % tb, rt[:, 40 + tb * 8:48 + tb * 8], 'rtsv%d' % tb)
                csv_.append(('pool', lambda e, tb=tb: e.tensor_copy(out=svb[:, tb, :], in_=svf[:, tb, :]), [SVF], ['svb%d' % tb]))
                chains += [cq, ck, csv_]
                delays += [0, 0, 5]
            rnd = 0
            while any(chains):
                for ch, dl in zip(chains, delays):
                    if ch and rnd >= dl:
                        eng_, fn_, r_, w_ = ch.pop(0)
                        A(eng_, fn_, r=r_, w=w_)
                rnd += 1

            for tb in range(ntb):
                gb = sb * 2 + tb
                blk = gb % 16
                slot = gb % 3
                if sample:
                    for b in range(16):
                        A('sp', lambda e, b=b: e.dma_start(out=wks_d[b, 120:128, :], in_=kvf[b * 8:(b + 1) * 8, 0, 0:128]), r=['kvf0'], dma='o_w')
                        A('sp', lambda e, b=b: e.dma_start(out=wvs_d[b, 120:128, :], in_=kvf[b * 8:(b + 1) * 8, 0, 128:256]), r=['kvf0'], dma='o_w')
                    A('sp', lambda e: e.dma_start(out=svs_d, in_=svf[:, 0, :]), r=['svf0'], dma='o_w')
                elif blk == 15:
                    seq = gb // 16
                    A('sp', lambda e, tb=tb, seq=seq: e.dma_start(out=wkp_d[seq], in_=kvf[:, tb, 0:128]), r=['kvf%d' % tb], dma='o_w')
                    A('sp', lambda e, tb=tb, seq=seq: e.dma_start(out=wvp_d[seq], in_=kvf[:, tb, 128:256]), r=['kvf%d' % tb], dma='o_w')
                pbq = bankb(0)
                for h in range(8):
                    A('pe', lambda e, h=h, tb=tb: e.transpose(out=pbq[:, h * 128:(h + 1) * 128], in_=qb[:, tb, h * 128:(h + 1) * 128], identity=identb[:]),
                      r=['qb%d' % tb, 'identb'], w=['pb0'])
                A('act', lambda e, tb=tb: e.copy(out=qT[:, tb, :, :], in_=pbq.rearrange("p (h q) -> p h q", q=128)), r=['pb0'], w=['qT%d' % tb])
                A('pe', lambda e, tb=tb: e.transpose(out=bankb(1)[:, 0:128], in_=kb[:, tb, :], identity=identb[:]), r=['kb%d' % tb, 'identb'], w=['pb1'])
                A('act', lambda e, slot=slot: e.copy(out=kTr[:, slot, :], in_=bankb(1)[:, 0:128]), r=['pb1'], w=['kT%d' % slot])

                sT = sguTs if sample else sguTp
                bT = bTs if sample else bTp
                zp = bank(0, 2)
                for h in range(8):
                    A('pe', lambda e, h=h, tb=tb, sT=sT: e.matmul(out=zp[:, h * 128:(h + 1) * 128], lhsT=sT[:, h, :], rhs=svb[:, tb, h * 128:(h + 1) * 128], start=True, stop=True),
                      r=['svb%d' % tb, 'sguTp', 'sguTs'], w=['pb0', 'pb1'])
                t3 = tmpf.rearrange("p (h c) -> p h c", c=128)
                A('dve', lambda e, bT=bT: e.tensor_tensor(out=t3, in0=zp.rearrange("p (h c) -> p h c", c=128), in1=bc(bT[:], [128, 8, 128], 2), op=ALU.add),
                  r=['pb0', 'pb1', 'bTp', 'bTs'], w=TF)
                A('dve', lambda e, tb=tb: e.tensor_tensor(out=tmpf, in0=tmpf, in1=su[:, tb, :], op=ALU.mult), r=TF + ['su%d' % tb], w=TF)
                A('act', lambda e, tb=tb: e.activation(out=junk[:, 0:1024], in_=tmpf, func=AF.Square, accum_out=stat[:, 20 + tb:21 + tb]), r=TF, w=['junk', 'st_c%d' % tb])
                rsqrt_ops(stat[:, 4 + tb:5 + tb], stat[:, 20 + tb:21 + tb], 1024, rr=['st_c%d' % tb], ww=['rstd_s%d' % tb])
                A('dve', lambda e, tb=tb: e.tensor_scalar(out=mixed[:, tb, 1024:2048], in0=tmpf, scalar1=stat[:, 4 + tb:5 + tb], scalar2=None, op0=ALU.mult),
                  r=TF + ['rstd_s%d' % tb], w=['mixed%d' % tb])

                if sample:
                    kbl = [(kTc[:, b, :], vaugc[:, b, :, :], masks[:, b, :], ['kTc'], ['vaugc']) for b in range(16)]
                    kbl.append((kTr[:, slot, :], vaugr[:, slot, :, :], masks[:, 16, :], ['kT%d' % slot], ['vaug%d' % slot]))
                else:
                    kbl = []
                    if blk > 0:
                        ps_ = (gb - 1) % 3
                        kbl.append((kTr[:, ps_, :], vaugr[:, ps_, :, :], maskp[:, 0, :], ['kT%d' % ps_], ['vaug%d' % ps_]))
                    kbl.append((kTr[:, slot, :], vaugr[:, slot, :, :], maskp[:, 1, :], ['kT%d' % slot], ['vaug%d' % slot]))
                nkb = len(kbl)
                for ki, (kT_, va_, mk_, kres, vres) in enumerate(kbl):
                    pti = ki % 2
                    for kh in range(2):
                        for half in range(2):
                            pk = 2 + (kh * 2 + half) % 2
                            ps = bank(pk)
                            A('pe', lambda e, kh=kh, half=half, ps=ps, kT_=kT_, tb=tb: e.matmul(
                                out=ps, lhsT=kT_[kh * 64:(kh + 1) * 64, :], rhs=qT[kh * 64:(kh + 1) * 64, tb, half * 4:(half + 1) * 4, :], start=True, stop=False),
                              r=kres + ['qT%d' % tb], w=['pb%d' % pk])
                            for hh in range(4):
                                A('pe', lambda e, ps=ps, mk_=mk_, hh=hh: e.matmul(out=ps[:, hh * 128:(hh + 1) * 128], lhsT=identb[:], rhs=mk_, start=False, stop=True),
                                  r=['identb', 'maskp', 'masks'], w=['pb%d' % pk])
                            h0 = kh * 8 + half * 4
                            A('act', lambda e, ps=ps, pti=pti, h0=h0: e.activation(out=pT[pti][:, h0:h0 + 4, :], in_=ps.rearrange("p (h q) -> p h q", q=128), func=AF.Exp, scale=0.125),
                              r=['pb%d' % pk], w=['pT%d_%d' % (pti, h0)])
                    for h in range(16):
                        pk = 4 + h // 4
                        o = bank(pk)[:, (h % 4) * 128:(h % 4) * 128 + 65]
                        h0 = (h // 4) * 4
                        A('pe', lambda e, h=h, o=o, pti=pti, va_=va_, ki=ki: e.matmul(out=o, lhsT=pT[pti][:, h, :], rhs=va_[:, h // 8, :], start=(ki == 0 and h % 4 == 0), stop=(ki == nkb - 1)),
                          r=['pT%d_%d' % (pti, h0)] + vres, w=['pb%d' % pk])
                o4 = bank(4, 4).rearrange("p (h c) -> p h c", c=128)
                ob = ['pb4', 'pb5', 'pb6', 'pb7']
                A('dve', lambda e: e.tensor_tensor(out=rt[:, 64:80], in0=o4[:, :, 64], in1=esink[:], op=ALU.add), r=ob + ['esink'], w=['rt2'])
                A('dve', lambda e: e.reciprocal(out=rt[:, 64:80], in_=rt[:, 64:80]), r=['rt2'], w=['rt2'])
                A('dve', lambda e: e.tensor_tensor(out=attnf.rearrange("p (h c) -> p h c", c=64), in0=o4[:, :, 0:64], in1=bc(rt[:, 64:80], [128, 16, 64], 2), op=ALU.mult),
                  r=ob + ['rt2'], w=['attnf'])
                A('act', lambda e, tb=tb: e.activation(out=junk[:, 1024:2048], in_=attnf, func=AF.Square, accum_out=stat[:, 18 + tb:19 + tb]), r=['attnf'], w=['junkb', 'st_b%d' % tb])
                rsqrt_ops(stat[:, 2 + tb:3 + tb], stat[:, 18 + tb:19 + tb], 1024, rr=['st_b%d' % tb], ww=['rstd_a%d' % tb])
                A('dve', lambda e, tb=tb: e.tensor_scalar(out=mixed[:, tb, 0:1024], in0=attnf, scalar1=stat[:, 2 + tb:3 + tb], scalar2=None, op0=ALU.mult),
                  r=['attnf', 'rstd_a%d' % tb], w=['mixed%d' % tb])
                transposes16(mixed[:, tb, :], mixT, slice(tb * 128, (tb + 1) * 128), 0, ['mixed%d' % tb], ['mixT'], 'act')
            for si in range(4):
                ws, wr = load_w(wout_v, si * 512, 512)
                for tb in range(ntb):
                    pk = 2 + (si * 2 + tb) % 2
                    ps = bank(pk)
                    for dc in range(16):
                        A('pe', lambda e, dc=dc, tb=tb, ps=ps, ws=ws: e.matmul(out=ps, lhsT=mixT[:, dc, tb * 128:(tb + 1) * 128], rhs=ws[:, dc, :], start=(dc == 0), stop=(dc == 15)),
                          r=['mixT', wr], w=['pb%d' % pk])
                    A('dve', lambda e, tb=tb, si=si, ps=ps: e.tensor_tensor(out=xin[:, tb, si * 512:(si + 1) * 512], in0=xin[:, tb, si * 512:(si + 1) * 512], in1=ps, op=ALU.add),
                      r=['pb%d' % pk, 'xin%d' % tb], w=['xin%d' % tb])
            if dbg and sb == 0:
                for tb in range(ntb):
                    A('sp', lambda e, tb=tb: e.dma_start(out=dh_d[tb * 128:(tb + 1) * 128, :], in_=xin[:, tb, :]), r=['xin%d' % tb], dma='o_w')
            S.barrier(sp=False)

            a = Ar()
            hnT = a.b(16 * 256).rearrange("p (a b) -> p a b", b=256)
            selT = a.b(3 * 256).rearrange("p (a b) -> p a b", b=256)
            off_keep = a.o
            tmpb = a.b(D)
            junk = a.f(D)
            qTp = a.b(16 * 256).rearrange("p (g t) -> p g t", t=256)
            sc = a.f(D)
            work = a.f(D)
            vv = a.f(256)
            ixu = a.f(256).bitcast(U32)
            ixf = a.f(256)
            cand = a.f(D)
            cv_ = a.f(128); ciu = a.f(128).bitcast(U32); cif = a.f(128)
            k1f = a.f(128); k2f = a.f(128); ee = a.f(128)
            eq = a.f(D)
            sel = a.b(3 * 128).rearrange("p (a b) -> p a b", b=128)

            for tb in range(ntb):
                X = 'xin%d' % tb
                A('act', lambda e, tb=tb: e.activation(out=junk, in_=xin[:, tb, :], func=AF.Square, accum_out=stat[:, 22 + tb:23 + tb]), r=[X], w=['junk', 'st_d%d' % tb])
                rsqrt_ops(stat[:, 6 + tb:7 + tb], stat[:, 22 + tb:23 + tb], D, rr=['st_d%d' % tb], ww=['rstd2_%d' % tb])
                A('dve', lambda e, tb=tb: e.tensor_scalar(out=tmpb, in0=xin[:, tb, :], scalar1=stat[:, 6 + tb:7 + tb], scalar2=None, op0=ALU.mult), r=[X, 'rstd2_%d' % tb], w=['tmpb'])
                transposes16(tmpb, hnT, slice(tb * 128, (tb + 1) * 128), 0, ['tmpb'], ['hnT'], 'act')
            for si in range(4):
                ws, wr = load_w(wq_v, si * 512, 512)
                for gl in range(4):
                    g = si * 4 + gl
                    pk = 2 + g % 2
                    ps = bank(pk)[:, 0:T]
                    for dc in range(16):
                        A('pe', lambda e, dc=dc, gl=gl, ps=ps, ws=ws: e.matmul(out=ps, lhsT=ws[:, dc, gl * 128:(gl + 1) * 128], rhs=hnT[:, dc, 0:T], start=(dc == 0), stop=(dc == 15)),
                          r=['hnT', wr], w=['pb%d' % pk])
                    ev = 'act' if g % 2 else 'dve'
                    A(ev, lambda e, g=g, ps=ps, ev=ev: (e.copy(out=qTp[:, g, 0:T], in_=ps) if ev == 'act' else e.tensor_copy(out=qTp[:, g, 0:T], in_=ps)),
                      r=['pb%d' % pk], w=['qTp'])
            for tb in range(ntb):
                sp4 = bank(4, 4)
                for g in range(16):
                    A('pe', lambda e, g=g, tb=tb: e.matmul(out=sp4[:, g * 128:(g + 1) * 128], lhsT=qTp[:, g, tb * 128:(tb + 1) * 128], rhs=k12T[:, g % 2, :], start=True, stop=True),
                      r=['qTp', 'k12T'], w=['pb4', 'pb5', 'pb6', 'pb7'])
                A('act', lambda e: e.copy(out=sc, in_=sp4), r=['pb4', 'pb5', 'pb6', 'pb7'], w=['sc'])
                v3 = vv.rearrange("p (g k) -> p g k", k=16)
                i3 = ixu.rearrange("p (g k) -> p g k", k=16)
                SG = [sc[:, g * 128:(g + 1) * 128] for g in range(16)]
                WG = [work[:, g * 128:(g + 1) * 128] for g in range(16)]
                R0 = ['sc']
                for g in range(16):
                    A('dve', lambda e, g=g: e.max(out=v3[:, g, 0:8], in_=SG[g]), r=R0, w=['vv%d' % g])
                for g in range(16):
                    A('dve', lambda e, g=g: e.max_index(out=i3[:, g, 0:8], in_max=v3[:, g, 0:8], in_values=SG[g]), r=R0 + ['vv%d' % g], w=['ix%d' % g])
                for g in range(16):
                    A('dve', lambda e, g=g: e.match_replace(out=WG[g], in_to_replace=v3[:, g, 0:8], in_values=SG[g], imm_value=-1e30), r=R0 + ['vv%d' % g], w=['wk%d' % g])
                for g in range(16):
                    A('dve', lambda e, g=g: e.max(out=v3[:, g, 8:16], in_=WG[g]), r=['wk%d' % g], w=['vv%d' % g])
                for g in range(16):
                    A('dve', lambda e, g=g: e.max_index(out=i3[:, g, 8:16], in_max=v3[:, g, 8:16], in_values=WG[g]), r=['wk%d' % g, 'vv%d' % g], w=['ix%d' % g])
                allv = ['vv%d' % g for g in range(16)]
                alli = ['ix%d' % g for g in range(16)]
                A('dve', lambda e: e.tensor_copy(out=ixf, in_=ixu), r=alli, w=['ixf'])
                v4 = vv.rearrange("p (h s k) -> p h s k", s=2, k=16)
                c4 = cand.rearrange("p (h a b) -> p h a b", a=16, b=16)
                A('dve', lambda e: e.tensor_tensor(out=c4, in0=bc(v4[:, :, 0, :], [128, 8, 16, 16], 3), in1=bc(v4[:, :, 1, :], [128, 8, 16, 16], 2), op=ALU.add), r=allv, w=['cand'])
                cv3 = cv_.rearrange("p (h k) -> p h k", k=16)
                ci3 = ciu.rearrange("p (h k) -> p h k", k=16)
                CH = [cand[:, h * 256:(h + 1) * 256] for h in range(8)]
                WH = [work[:, h * 256:(h + 1) * 256] for h in range(8)]
                for h in range(8):
                    A('dve', lambda e, h=h: e.max(out=cv3[:, h, 0:8], in_=CH[h]), r=['cand'], w=['cv%d' % h])
                for h in range(8):
                    A('dve', lambda e, h=h: e.max_index(out=ci3[:, h, 0:8], in_max=cv3[:, h, 0:8], in_values=CH[h]), r=['cand', 'cv%d' % h], w=['ci%d' % h])
                for h in range(8):
                    A('dve', lambda e, h=h: e.match_replace(out=WH[h], in_to_replace=cv3[:, h, 0:8], in_values=CH[h], imm_value=-1e30),
                      r=['cand', 'cv%d' % h], w=['wk%d' % (2 * h), 'wk%d' % (2 * h + 1)])
                for h in range(8):
                    A('dve', lambda e, h=h: e.max(out=cv3[:, h, 8:16], in_=WH[h]), r=['wk%d' % (2 * h)], w=['cv%d' % h])
                for h in range(8):
                    A('dve', lambda e, h=h: e.max_index(out=ci3[:, h, 8:16], in_max=cv3[:, h, 8:16], in_values=WH[h]), r=['wk%d' % (2 * h), 'cv%d' % h], w=['ci%d' % h])
                allc = ['cv%d' % h for h in range(8)]
                allci = ['ci%d' % h for h in range(8)]
                e3 = ee.rearrange("p (h k) -> p h k", k=16)
                A('dve', lambda e: e.tensor_tensor(out=e3, in0=cv3, in1=bc(cv3[:, :, 0], [128, 8, 16], 2), op=ALU.subtract), r=allc, w=['ee'])
                A('act', lambda e: e.activation(out=ee, in_=ee, func=AF.Exp), r=['ee'], w=['ee'])
                A('dve', lambda e: e.tensor_reduce(out=rt[:, 32:40], in_=e3, axis=AX.X, op=ALU.add), r=['ee'], w=['rt3'])
                A('dve', lambda e: e.reciprocal(out=rt[:, 32:40], in_=rt[:, 32:40]), r=['rt3'], w=['rt3'])
                A('dve', lambda e: e.tensor_tensor(out=sel[:, 2, :].rearrange("p (h k) -> p h k", k=16), in0=e3, in1=bc(rt[:, 32:40], [128, 8, 16], 2), op=ALU.mult),
                  r=['ee', 'rt3'], w=['sel'])
                cifu = cif.bitcast(U32)
                A('dve', lambda e: e.tensor_scalar(out=cifu, in0=ciu, scalar1=15, scalar2=None, op0=ALU.bitwise_and), r=allci, w=['cif'])
                A('dve', lambda e: e.tensor_copy(out=k2f, in_=cifu), r=['cif'], w=['k2f'])
                A('dve', lambda e: e.tensor_scalar(out=cifu, in0=ciu, scalar1=4, scalar2=None, op0=ALU.logical_shift_right), r=allci + ['k2f'], w=['cif'])
                A('dve', lambda e: e.tensor_copy(out=k1f, in_=cifu), r=['cif'], w=['k1f'])
                q4 = eq.rearrange("p (h a b) -> p h a b", a=16, b=16)
                if4 = ixf.rearrange("p (h s k) -> p h s k", s=2, k=16)
                for side, kf_ in ((0, k1f), (1, k2f)):
                    kk = kf_.rearrange("p (h k) -> p h k", k=16)
                    A('dve', lambda e, kk=kk: e.tensor_tensor(out=q4, in0=bc(kk, [128, 8, 16, 16], 3), in1=bc(bc(iota16[:], [128, 16, 16], 1), [128, 8, 16, 16], 1), op=ALU.is_equal),
                      r=['k1f', 'k2f', 'iota16'], w=['eq'])
                    A('dve', lambda e, side=side: e.tensor_tensor(out=q4, in0=q4, in1=bc(if4[:, :, side, :], [128, 8, 16, 16], 2), op=ALU.mult), r=['eq', 'ixf'], w=['eq'])
                    A('dve', lambda e: e.tensor_reduce(out=k1f if False else work[:, 0:128], in_=q4.rearrange("p h a b -> p (h a) b"), axis=AX.X, op=ALU.add), r=['eq'], w=['wk0'])
                    A('dve', lambda e, side=side: e.tensor_copy(out=sel[:, side, :], in_=work[:, 0:128]), r=['wk0'], w=['sel'])
                for i in range(3):
                    A('pe', lambda e, i=i: e.transpose(out=bankb(0)[:, i * 128:(i + 1) * 128], in_=sel[:, i, :], identity=identb[:]), r=['sel', 'identb'], w=['pb0'])
                A('act', lambda e, tb=tb: e.copy(out=selT[:, :, tb * 128:(tb + 1) * 128], in_=bankb(0)[:, 0:384].rearrange("p (a b) -> p a b", b=128)), r=['pb0'], w=['selT'])
            S.barrier(sp=False)

            a = Ar()
            a.o = off_keep
            G = a.b(128 * 256).rearrange("p (j t) -> p j t", t=256)
            NPB_ = 4
            P1 = [a.b(TG * 128).rearrange("p (t i) -> p t i", i=128) for _ in range(NPB_)]
            P2 = [a.b(TG * 128).rearrange("p (t i) -> p t i", i=128) for _ in range(NPB_)]
            gtmp = [a.b(256) for _ in range(2)]
            ntg = T // TG
            for tg in range(0 if 'G' in SKIP else ntg):
                s = tg % NPB_
                t0 = tg * TG
                iob = bc(iotab[:], [128, TG, 128], 1)
                A('dve', lambda e, s=s, t0=t0, iob=iob: e.tensor_tensor(out=P1[s], in0=iob, in1=bc(selT[:, 0, t0:t0 + TG], [128, TG, 128], 2), op=ALU.is_equal),
                  r=['selT', 'iotab'], w=['P1_%d' % s])
                A('dve', lambda e, s=s, t0=t0, iob=iob: e.tensor_tensor(out=P2[s], in0=iob, in1=bc(selT[:, 1, t0:t0 + TG], [128, TG, 128], 2), op=ALU.is_equal),
                  r=['selT', 'iotab'], w=['P2_%d' % s])
                A('pool', lambda e, s=s, t0=t0: e.tensor_tensor(out=P2[s], in0=P2[s], in1=bc(selT[:, 2, t0:t0 + TG], [128, TG, 128], 2), op=ALU.mult),
                  r=['selT', 'P2_%d' % s], w=['P2_%d' % s])
                for q in range(TG // 4):
                    pk = (tg * (TG // 4) + q) % 2
                    for tt in range(4):
                        t = q * 4 + tt
                        A('pe', lambda e, s=s, t=t, tt=tt, pk=pk: e.matmul(out=bank(pk)[:, tt * 128:(tt + 1) * 128], lhsT=P1[s][:, t, :], rhs=P2[s][:, t, :], start=True, stop=True),
                          r=['P1_%d' % s, 'P2_%d' % s], w=['pb%d' % pk])
                    tok0 = t0 + q * 4
                    A('act', lambda e, pk=pk, tok0=tok0: e.copy(out=G[:, :, tok0:tok0 + 4], in_=bank(pk).rearrange("p (t j) -> p j t", j=128)), r=['pb%d' % pk], w=['G'])
            for cg in range(0 if 'A' in SKIP else 128 // CG):
                i = next_slot()
                ws = wslot[i][:, 0:CG * D].rearrange("p (j x) -> p j x", x=D)
                A('sp', lambda e, cg=cg, ws=ws: e.dma_start(out=ws, in_=UT_s[cg * CG:(cg + 1) * CG].rearrange("j p x -> p j x")), r=['scr'], w=['ws%d' % i], dma='ws%d' % i)
                for jl in range(CG):
                    j = cg * CG + jl
                    pk = 2 + j % 2
                    ps = bank(pk)[:, 0:T]
                    for dc in range(16):
                        A('pe', lambda e, dc=dc, jl=jl, ps=ps, ws=ws: e.matmul(out=ps, lhsT=ws[:, jl, dc * 128:(dc + 1) * 128], rhs=hnT[:, dc, 0:T], start=(dc == 0), stop=(dc == 15)),
                          r=['hnT', 'ws%d' % i], w=['pb%d' % pk])
                    gs = j % 2
                    A('act', lambda e, ps=ps, gs=gs: e.activation(out=gtmp[gs][:, 0:T], in_=ps, func=AF.Gelu_apprx_tanh), r=['pb%d' % pk], w=['gtmp%d' % gs])
                    me = 'dve' if j % 2 == 0 else 'pool'
                    A(me, lambda e, j=j, gs=gs: e.tensor_tensor(out=G[:, j, 0:T], in0=G[:, j, 0:T], in1=gtmp[gs][:, 0:T], op=ALU.mult), r=['G', 'gtmp%d' % gs], w=['Gw%d' % j])
            allG = ['Gw%d' % j for j in range(128)]
            for dh in range(0 if 'B' in SKIP else 2):
                for cg in range(128 // CG):
                    i = next_slot()
                    ws = wslot[i][:, 0:CG * 1024].rearrange("p (j x) -> p j x", x=1024)
                    A('sp', lambda e, cg=cg, ws=ws, dh=dh: e.dma_start(out=ws, in_=VB_s[cg * CG:(cg + 1) * CG, :, dh * 1024:(dh + 1) * 1024].rearrange("j i x -> i j x")),
                      r=['scr'], w=['ws%d' % i], dma='ws%d' % i)
                    for jl in range(CG):
                        j = cg * CG + jl
                        for tb in range(ntb):
                            for ds in range(2):
                                pk = 4 + tb * 2 + ds
                                A('pe', lambda e, j=j, jl=jl, tb=tb, ds=ds, pk=pk, ws=ws: e.matmul(out=bank(pk), lhsT=G[:, j, tb * 128:(tb + 1) * 128], rhs=ws[:, jl, ds * 512:(ds + 1) * 512],
                                                                                                       start=(j == 0), stop=(j == 127)),
                                  r=(allG if j == 0 else ['Gw%d' % j]) + ['ws%d' % i], w=['pb%d' % pk])
                for tb in range(ntb):
                    for ds in range(2):
                        pk = 4 + tb * 2 + ds
                        c0 = dh * 1024 + ds * 512
                        A('dve', lambda e, tb=tb, pk=pk, c0=c0: e.tensor_tensor(out=xin[:, tb, c0:c0 + 512], in0=xin[:, tb, c0:c0 + 512], in1=bank(pk), op=ALU.add),
                          r=['pb%d' % pk, 'xin%d' % tb], w=['xin%d' % tb])
            if dbg and sb == 0:
                for tb in range(ntb):
                    A('sp', lambda e, tb=tb: e.dma_start(out=dh2_d[tb * 128:(tb + 1) * 128, :], in_=xin[:, tb, :]), r=['xin%d' % tb], dma='o_w')
            S.barrier(sp=False)

            a = Ar()
            h3T = a.b(16 * 256).rearrange("p (a b) -> p a b", b=256)
            tmpb = a.b(D)
            junk = a.f(D)
            pb_ = a.b(256)
            pT_ = a.b(2 * 256).rearrange("p (a b) -> p a b", b=256)
            gate = a.f(2 * D).rearrange("p (a b) -> p a b", b=D)
            tmp2 = a.f(512)
            for tb in range(ntb):
                X = 'xin%d' % tb
                A('act', lambda e, tb=tb: e.activation(out=junk, in_=xin[:, tb, :], func=AF.Square, accum_out=stat[:, 24 + tb:25 + tb]), r=[X], w=['junk', 'st_e%d' % tb])
                rsqrt_ops(stat[:, 8 + tb:9 + tb], stat[:, 24 + tb:25 + tb], D, rr=['st_e%d' % tb], ww=['rstd3_%d' % tb])
                A('dve', lambda e, tb=tb: e.tensor_copy(out=tmpb, in_=xin[:, tb, :]), r=[X], w=['tmpb'])
                transposes16(tmpb, h3T, slice(tb * 128, (tb + 1) * 128), 0, ['tmpb'], ['h3T'], 'act')
                A('dve', lambda e, tb=tb: e.tensor_copy(out=pb_, in_=pin[:, tb, :]), r=['pin%d' % tb], w=['pb_'])
                for kc in range(2):
                    A('pe', lambda e, kc=kc: e.transpose(out=bankb(1)[:, kc * 128:(kc + 1) * 128], in_=pb_[:, kc * 128:(kc + 1) * 128], identity=identb[:]), r=['pb_', 'identb'], w=['pb1'])
                A('dve', lambda e, tb=tb: e.tensor_copy(out=pT_[:, :, tb * 128:(tb + 1) * 128], in_=bankb(1)[:, 0:256].rearrange("p (a b) -> p a b", b=128)), r=['pb1'], w=['pT_'])
            for si in range(4):
                ws, wr = load_w(gw_v, si * 512, 512)
                for tb in range(ntb):
                    pk = 2 + (si * 2 + tb) % 2
                    ps = bank(pk)
                    for dc in range(16):
                        A('pe', lambda e, dc=dc, tb=tb, ps=ps, ws=ws: e.matmul(out=ps, lhsT=h3T[:, dc, tb * 128:(tb + 1) * 128], rhs=ws[:, dc, :], start=(dc == 0), stop=(dc == 15)),
                          r=['h3T', wr], w=['pb%d' % pk])
                    A('act', lambda e, tb=tb, si=si, ps=ps: e.activation(out=gate[:, tb, si * 512:(si + 1) * 512], in_=ps, func=AF.Sigmoid, scale=stat[:, 8 + tb:9 + tb]),
                      r=['pb%d' % pk, 'rstd3_%d' % tb], w=['gate%d' % tb])
            for tb in range(ntb):
                for si in range(4):
                    pk = 4 + si
                    ps = bank(pk)
                    for kc in range(2):
                        A('pe', lambda e, kc=kc, tb=tb, si=si, ps=ps: e.matmul(out=ps, lhsT=pT_[:, kc, tb * 128:(tb + 1) * 128], rhs=plewb[:, kc, si * 512:(si + 1) * 512], start=(kc == 0), stop=(kc == 1)),
                          r=['pT_', 'plewb'], w=['pb%d' % pk])
                    A('dve', lambda e, tb=tb, si=si, ps=ps: e.tensor_tensor(out=tmp2, in0=ps, in1=gate[:, tb, si * 512:(si + 1) * 512], op=ALU.mult), r=['pb%d' % pk, 'gate%d' % tb], w=['tmp2'])
                    A('dve', lambda e, tb=tb, si=si: e.tensor_tensor(out=xin[:, tb, si * 512:(si + 1) * 512], in0=xin[:, tb, si * 512:(si + 1) * 512], in1=tmp2, op=ALU.add),
                      r=['tmp2', 'xin%d' % tb], w=['xin%d' % tb])
                A('sp', lambda e, tb=tb: e.dma_start(out=y_d[r0 + tb * 128:r0 + (tb + 1) * 128, :], in_=xin[:, tb, :]), r=['xin%d' % tb], dma='o_y%d' % tb)
            S.barrier(sp=False)

        block = es.enter_context(nc.Block())
        S.emit(block)
    return nc


def _consts():
    c = {}
    c["ident"] = np.eye(128, dtype=np.float32)
    c["iota"] = np.tile(np.arange(128, dtype=np.float32)[None, :], (128, 1))
    s = np.arange(128)[:, None]
    q = np.arange(128)[None, :]
    mprev = np.where(s > q, 0.0, NEG)
    mcur = np.where(s <= q, 0.0, NEG)
    c["maskp"] = np.stack([mprev, mcur], axis=1).astype(np.float32)
    ms = np.full((128, 17, 128), NEG, dtype=np.float32)
    qb, ql = np.arange(128) // 8, np.arange(128) % 8
    for b in range(16):
        vis = (qb[None, :] == b) & (np.arange(128)[:, None] > ql[None, :])
        ms[:, b, :] = np.where(vis, 0.0, NEG)
    sb_, sl = np.arange(128) // 8, np.arange(128) % 8
    vis = (sb_[:, None] == qb[None, :]) & (sl[:, None] <= ql[None, :])
    ms[:, 16, :] = np.where(vis, 0.0, NEG)
    c["masks"] = ms
    c["tril"] = (np.arange(128)[None, :] <= np.arange(128)[:, None]).astype(np.float32)
    c["bdT"] = vis.astype(np.float32)
    c["e8"] = (np.arange(8)[:, None] == (np.arange(128) % 8)[None, :]).astype(np.float32)
    half = 8
    inv = np.power(np.float32(500000.0), -np.arange(half, dtype=np.float32) * np.float32(2.0) / np.float32(16)).astype(np.float32)
    pos_p = (np.arange(16)[None, :] * 128 + np.arange(128)[:, None]).astype(np.float32)
    ang = (pos_p[:, :, None] * inv[None, None, :]).astype(np.float32)
    c["csp"] = np.stack([np.cos(ang), np.sin(ang)], axis=2).astype(np.float32)
    pos_s = (16384 + np.arange(128) % 8).astype(np.float32)
    angs = (pos_s[:, None] * inv[None, :]).astype(np.float32)
    c["css"] = np.stack([np.cos(angs), np.sin(angs)], axis=1).astype(np.float32)
    return c


def make_in_maps(inputs, cores=range(NCORE)):
    f = lambda k: np.ascontiguousarray(np.asarray(inputs[k], dtype=np.float32)[0])
    shared = dict(
        w_in=f("w_in"), w_out=f("w_out"), wq=f("peer_wq"), gw=f("ple_gate_w"), plew=f("ple_w"),
        U=f("peer_u"), V=f("peer_v"), k1=f("peer_k1"), k2=f("peer_k2"),
        gffn=f("norm_ffn")[None, :], qn=f("q_norm")[None, :], kn=f("k_norm")[None, :], sgn=f("sgu_norm")[None, :],
        sinks=f("sinks")[None, :], sguw=f("sgu_w"), sgub=f("sgu_b"),
    )
    gout = np.concatenate([f("attn_out_norm"), f("sgu_out_norm")])
    cols = [f("norm_mix"), gout, f("norm_ffn"), f("ple_gate_norm")]
    shared["gcol"] = np.ascontiguousarray(np.stack([g.reshape(16, 128).T for g in cols], axis=1))
    shared.update(_consts())
    xp = np.asarray(inputs["x_prompt"], dtype=np.float32)
    xs = np.asarray(inputs["x_sample"], dtype=np.float32)
    pp = np.asarray(inputs["p_prompt"], dtype=np.float32)[0]
    ps = np.asarray(inputs["p_sample"], dtype=np.float32)[0]
    ck = np.asarray(inputs["cache_k"], dtype=np.float32)[0]
    cv = np.asarray(inputs["cache_v"], dtype=np.float32)[0]
    maps = []
    for c in cores:
        m = dict(shared)
        m["x"] = np.concatenate([xp[2 * c:2 * c + 2].reshape(4096, D), xs[16 * c:16 * c + 16].reshape(128, D)], axis=0)
        m["p"] = np.concatenate([pp[2 * c:2 * c + 2].reshape(4096, 256), ps[16 * c:16 * c + 16].reshape(128, 256)], axis=0)
        m["ck"] = np.ascontiguousarray(ck[16 * c:16 * c + 16].reshape(16, 128, 128))
        m["cv"] = np.ascontiguousarray(cv[16 * c:16 * c + 16].reshape(16, 128, 128))
        maps.append(m)
    return maps


def kernel(**inputs):
    nc = build_nc()
    maps = make_in_maps(inputs)
    res = run_bass_kernel_spmd(nc, maps, core_ids=list(range(NCORE)))
    R = res.results
    y = np.stack([r["y"] for r in R])
    y_prompt = y[:, :4096].reshape(16, 2048, D)
    y_sample = y[:, 4096:].reshape(128, 8, D)
    wkp = np.concatenate([r["wkp"] for r in R]).reshape(1, 16, 128, 2, 64)
    wvp = np.concatenate([r["wvp"] for r in R]).reshape(1, 16, 128, 2, 64)
    wks = np.concatenate([r["wks"] for r in R]).reshape(1, 128, 128, 2, 64)
    wvs = np.concatenate([r["wvs"] for r in R]).reshape(1, 128, 128, 2, 64)
    svs = np.concatenate([r["svs"] for r in R]).reshape(1, 128, 8, 8, 128)
    return (y_prompt.astype(np.float32), y_sample.astype(np.float32), wkp.astype(np.float32), wvp.astype(np.float32),
            wks.astype(np.float32), wvs.astype(np.float32), svs.astype(np.float32))
```

```python
import os
import types
import numpy as np
from contextlib import ExitStack
import concourse.bass as bass
import concourse.mybir as mybir
from concourse.bass_utils import run_bass_kernel_spmd

F32 = mybir.dt.float32
BF16 = mybir.dt.bfloat16
U32 = mybir.dt.uint32
ALU = mybir.AluOpType
AF = mybir.ActivationFunctionType
AX = mybir.AxisListType

D = 2048
NCORE = 8
NPB = 32
NROW = 4224
EPS = 1e-6
NEG = -30000.0
CG = 4
TG = 8
SAME_ENGINE_FIFO = False


def _freeze(fn):
    if fn.__closure__ is None:
        return fn
    cells = []
    for c in fn.__closure__:
        try:
            cells.append(types.CellType(c.cell_contents))
        except ValueError:
            cells.append(c)
    g = types.FunctionType(fn.__code__, fn.__globals__, fn.__name__, fn.__defaults__, tuple(cells))
    g.__kwdefaults__ = fn.__kwdefaults__
    return g


class Sch:
    CE = ['pe', 'act', 'dve', 'pool']

    def __init__(self, nc, es):
        self.nc, self.es = nc, es
        self.ops = {e: [] for e in self.CE + ['sp']}
        self.res = {}
        self.esem = {e: es.enter_context(nc.semaphore("s_" + e)) for e in self.CE}
        self.dsems = {}
        self.pend = {e: set() for e in self.CE + ['sp']}

    def _ds(self, name):
        if name not in self.dsems:
            self.dsems[name] = [self.es.enter_context(self.nc.semaphore("d_" + name)), 0]
        return self.dsems[name]

    def add(self, eng, fn, r=(), w=(), dma=None):
        idx = len(self.ops[eng])
        deps = set(self.pend[eng])
        self.pend[eng] = set()
        for x in r:
            st = self.res.get(x)
            if st and st[0] is not None:
                deps.add(st[0])
        for x in w:
            st = self.res.get(x)
            if st:
                if st[0] is not None:
                    deps.add(st[0])
                deps.update(st[1])
        if dma is not None:
            d = self._ds(dma)
            d[1] += 16
            tok = ('D', dma, d[1])
        else:
            tok = (eng, idx)
        for x in r:
            self.res.setdefault(x, [None, []])[1].append(tok)
        for x in w:
            self.res[x] = [tok, []]
        self.ops[eng].append(dict(fn=_freeze(fn), deps=deps, dma=dma, sig=False, cnt=0))
        return tok

    def barrier(self):
        toks = set()
        for e in self.CE:
            if self.ops[e]:
                toks.add((e, len(self.ops[e]) - 1))
        for name, d in self.dsems.items():
            if d[1] > 0:
                toks.add(('D', name, d[1]))
        for e in self.CE + ['sp']:
            self.pend[e] |= toks

    def emit(self, block):
        for e in self.ops:
            for op in self.ops[e]:
                for d in op['deps']:
                    if d[0] != 'D':
                        self.ops[d[0]][d[1]]['sig'] = True
        for e in self.CE:
            c = 0
            for op in self.ops[e]:
                if op['sig']:
                    c += 1
                op['cnt'] = c

        def mk(en):
            def body(eng):
                waited = {}
                for op in self.ops[en]:
                    need = {}
                    for d in op['deps']:
                        if d[0] == 'D':
                            sem, val = self.dsems[d[1]][0], d[2]
                        else:
                            if d[0] == en and (en == 'pe' or SAME_ENGINE_FIFO):
                                continue
                            sem, val = self.esem[d[0]], self.ops[d[0]][d[1]]['cnt']
                        if need.get(sem.num, (None, 0))[1] < val:
                            need[sem.num] = (sem, val)
                    for num, (sem, val) in need.items():
                        if waited.get(num, 0) < val:
                            eng.wait_ge(sem, val)
                            waited[num] = val
                    ins = op['fn'](eng)
                    if op['dma'] is not None:
                        ins.then_inc(self.dsems[op['dma']][0], 16)
                    elif op['sig']:
                        ins.then_inc(self.esem[en], 1)
                if en == 'sp':
                    for name, d in self.dsems.items():
                        if d[1] > 0:
                            eng.wait_ge(d[0], d[1])
            return body
        block.sync(mk('sp'))
        block.tensor(mk('pe'))
        block.scalar(mk('act'))
        block.vector(mk('dve'))
        block.gpsimd(mk('pool'))


def bc(ap, shape, axis):
    return ap.unsqueeze(axis).to_broadcast(shape)


def build_nc(sb_list=None, dbg=False):
    SKIP = os.environ.get('KSKIP', '')
    if sb_list is None:
        sb_list = list(range(17))
    nc = bass.Bass("TRN2", target_bir_lowering=False)

    def din(name, shape, dt=F32):
        return nc.dram_tensor(name, list(shape), dt, kind="ExternalInput").ap()

    def dout(name, shape):
        return nc.dram_tensor(name, list(shape), F32, kind="ExternalOutput").ap()

    x_d = din("x", [NROW, D]); p_d = din("p", [NROW, 256])
    ck_d = din("ck", [16, 128, 128]); cv_d = din("cv", [16, 128, 128])
    win_d = din("w_in", [D, 3328]); wout_d = din("w_out", [D, D]); wq_d = din("wq", [D, D])
    gw_d = din("gw", [D, D]); plew_d = din("plew", [256, D])
    U_d = din("U", [16384, D]); V_d = din("V", [16384, D])
    k1_d = din("k1", [128, 128]); k2_d = din("k2", [128, 128])
    gcol_d = din("gcol", [128, 4, 16])
    gffn_d = din("gffn", [1, D])
    qn_d = din("qn", [1, 64]); kn_d = din("kn", [1, 64]); sgn_d = din("sgn", [1, 128]); sink_d = din("sinks", [1, 16])
    sguw_d = din("sguw", [8, 128, 128]); sgub_d = din("sgub", [8, 128])
    ident_d = din("ident", [128, 128]); iota_d = din("iota", [128, 128])
    mp_d = din("maskp", [128, 2, 128]); ms_d = din("masks", [128, 17, 128])
    tril_d = din("tril", [128, 128]); bdT_d = din("bdT", [128, 128]); e8_d = din("e8", [8, 128])
    csp_d = din("csp", [128, 16, 2, 8]); css_d = din("css", [128, 2, 8])

    y_d = dout("y", [NROW, D])
    wkp_d = dout("wkp", [2, 128, 128]); wvp_d = dout("wvp", [2, 128, 128])
    wks_d = dout("wks", [16, 128, 128]); wvs_d = dout("wvs", [16, 128, 128])
    svs_d = dout("svs", [128, 1024])
    if dbg:
        dh_d = dout("dbg_h", [256, D]); dh2_d = dout("dbg_h2", [256, D])

    win_s = nc.dram_tensor("win_s", [D, 3328], BF16).ap()
    wout_s = nc.dram_tensor("wout_s", [D, D], BF16).ap()
    wq_s = nc.dram_tensor("wq_s", [D, D], BF16).ap()
    gw_s = nc.dram_tensor("gw_s", [D, D], BF16).ap()
    UT_s = nc.dram_tensor("UT_s", [128, 128, D], BF16).ap()
    VB_s = nc.dram_tensor("VB_s", [128, 128, D], BF16).ap()

    es = ExitStack()
    with es:
        def sb_(name, shape, dt):
            return es.enter_context(nc.sbuf_tensor("t_" + name, list(shape), dt))

        identb = sb_("identb", [128, 128], BF16)
        iotab = sb_("iotab", [128, 128], BF16)
        iota16 = sb_("iota16", [128, 16], F32)
        maskp = sb_("maskp", [128, 2, 128], BF16)
        masks = sb_("masks", [128, 17, 128], BF16)
        sguTp = sb_("sguTp", [128, 8, 128], BF16)
        sguTs = sb_("sguTs", [128, 8, 128], BF16)
        bTp = sb_("bTp", [128, 8], F32)
        bTs = sb_("bTs", [128, 8], F32)
        qg = sb_("qg", [128, 64], F32); kg = sb_("kg", [128, 64], F32); sgn = sb_("sgn", [128, 128], F32)
        esink = sb_("esink", [128, 16], F32)
        k12T = sb_("k12T", [128, 2, 128], BF16)
        csp = sb_("csp", [128, 16, 2, 8], F32); css = sb_("css", [128, 2, 8], F32)
        gcol = sb_("gcol", [128, 4, 16], F32)
        plewb = sb_("plewb", [128, 2, D], BF16)
        kTc = sb_("kTc", [128, 16, 128], BF16)
        vaugc = sb_("vaugc", [128, 16, 2, 65], BF16)
        kTr = sb_("kTr", [128, 3, 128], BF16)
        vaugr = sb_("vaugr", [128, 3, 2, 65], BF16)
        xin = sb_("xin", [128, 2, D], F32)
        pin = sb_("pin", [128, 2, 256], F32)
        stat = sb_("stat", [128, 64], F32)
        mhalf = sb_("mhalf", [128, 16], F32)
        wslot = [sb_("wslot%d" % i, [128, 8192], BF16) for i in range(3)]
        ARENA = 24000
        arena = sb_("arena", [128, ARENA], F32)
        psum = es.enter_context(nc.psum_tensor("psum", [128, 4096], F32))

        def bank(k, n=1):
            return psum[:, k * 512:(k + n) * 512]

        def bankb(k, n=1):
            return bank(k, n).bitcast(BF16)

        class Ar:
            def __init__(s):
                s.o = 0

            def f(s, n):
                a = arena[:, s.o:s.o + n]
                s.o += n
                assert s.o <= ARENA, s.o
                return a

            def b(s, n):
                m = (n + 1) // 2
                a = arena[:, s.o:s.o + m].bitcast(BF16)
                s.o += m
                assert s.o <= ARENA, s.o
                return a

        S = Sch(nc, es)
        A = S.add
        wsl_i = [0]

        def next_slot():
            i = wsl_i[0] % 3
            wsl_i[0] += 1
            return i

        def rsqrt_ops(dst, src, n, eng='dve', rr=(), ww=()):
            A(eng, lambda e: e.tensor_scalar(out=dst, in0=src, scalar1=1.0 / n, scalar2=EPS, op0=ALU.mult, op1=ALU.add), r=rr, w=ww)
            k_ = dst.shape[-1]
            A('pool', lambda e: e.tensor_tensor(out=dst, in0=dst, in1=mhalf[:, 0:k_], op=ALU.pow), r=list(ww) + ['mhalf'], w=ww)

        def transposes16(src_bf, dstT, tcols, pbk, rr, ww, ev_eng):
            pb = bankb(pbk, 2)
            for dc in range(16):
                A('pe', lambda e, dc=dc: e.transpose(out=pb[:, dc * 128:(dc + 1) * 128], in_=src_bf[:, dc * 128:(dc + 1) * 128], identity=identb[:]),
                  r=rr + ['identb'], w=['pb%d' % pbk, 'pb%d' % (pbk + 1)])
            A(ev_eng, lambda e: (e.tensor_copy(out=dstT[:, :, tcols], in_=pb.rearrange("p (a b) -> p a b", b=128)) if ev_eng != 'act'
                                 else e.copy(out=dstT[:, :, tcols], in_=pb.rearrange("p (a b) -> p a b", b=128))),
              r=['pb%d' % pbk, 'pb%d' % (pbk + 1)], w=ww)

        a = Ar()
        A('pool', lambda e: e.memset(mhalf[:], -0.5), w=['mhalf'])
        t_f = a.f(128 * 17)
        A('sp', lambda e: e.dma_start(out=t_f[:, 0:128], in_=ident_d), w=['t_f'], dma='c0')
        A('dve', lambda e: e.tensor_copy(out=identb[:], in_=t_f[:, 0:128]), r=['t_f'], w=['identb'])
        A('sp', lambda e: e.dma_start(out=t_f[:, 0:128], in_=iota_d), w=['t_f'], dma='c0')
        A('dve', lambda e: e.tensor_copy(out=iotab[:], in_=t_f[:, 0:128]), r=['t_f'], w=['iotab'])
        A('dve', lambda e: e.tensor_copy(out=iota16[:], in_=t_f[:, 0:16]), r=['t_f'], w=['iota16'])
        A('sp', lambda e: e.dma_start(out=t_f[:, 0:256].rearrange("p (a b) -> p a b", b=128), in_=mp_d), w=['t_f'], dma='c0')
        A('dve', lambda e: e.tensor_copy(out=maskp[:], in_=t_f[:, 0:256].rearrange("p (a b) -> p a b", b=128)), r=['t_f'], w=['maskp'])
        A('sp', lambda e: e.dma_start(out=t_f[:, 0:128 * 17].rearrange("p (a b) -> p a b", b=128), in_=ms_d), w=['t_f'], dma='c0')
        A('dve', lambda e: e.tensor_copy(out=masks[:], in_=t_f[:, 0:128 * 17].rearrange("p (a b) -> p a b", b=128)), r=['t_f'], w=['masks'])
        for (dst, src, tg_) in ((qg, qn_d, 'qg'), (kg, kn_d, 'kg'), (sgn, sgn_d, 'sgn')):
            A('sp', lambda e, dst=dst, src=src: e.dma_start(out=dst[:], in_=src.partition_broadcast(128)), w=[tg_], dma='c1')
        A('sp', lambda e: e.dma_start(out=esink[:], in_=sink_d.partition_broadcast(128)), w=['esink'], dma='c2')
        A('act', lambda e: e.activation(out=esink[:], in_=esink[:], func=AF.Exp), r=['esink'], w=['esink'])
        A('sp', lambda e: e.dma_start(out=csp[:], in_=csp_d), w=['csp'], dma='c1')
        A('sp', lambda e: e.dma_start(out=css[:], in_=css_d), w=['css'], dma='c1')
        A('sp', lambda e: e.dma_start(out=gcol[:], in_=gcol_d), w=['gcol'], dma='c1')
        A('sp', lambda e: e.dma_start(out=bTp[:], in_=sgub_d.rearrange("h t -> t h"), allow_slow_non_contiguous=True), w=['bTp'], dma='c1')
        for b in range(16):
            A('sp', lambda e, b=b: e.dma_start(out=bTs[b * 8:(b + 1) * 8, :], in_=sgub_d[:, 0:8].rearrange("h t -> t h"), allow_slow_non_contiguous=True), w=['bTs'], dma='c1')
        t_b = a.b(128 * 8)
        for i, kd in enumerate((k1_d, k2_d)):
            A('sp', lambda e, kd=kd: e.dma_start(out=t_f[:, 0:128], in_=kd), w=['t_f'], dma='c0')
            A('dve', lambda e: e.tensor_copy(out=t_b[:, 0:128], in_=t_f[:, 0:128]), r=['t_f'], w=['t_b'])
            A('pe', lambda e: e.transpose(out=bankb(0)[:, 0:128], in_=t_b[:, 0:128], identity=identb[:]), r=['t_b', 'identb'], w=['pb0'])
            A('dve', lambda e, i=i: e.tensor_copy(out=k12T[:, i, :], in_=bankb(0)[:, 0:128]), r=['pb0'], w=['k12T'])
        trilf = a.f(128)
        bdTf = a.f(128)
        A('sp', lambda e: e.dma_start(out=trilf, in_=tril_d), w=['trilf'], dma='c3')
        A('sp', lambda e: e.dma_start(out=bdTf, in_=bdT_d), w=['bdTf'], dma='c4')
        for h in range(8):
            A('sp', lambda e, h=h: e.dma_start(out=t_f[:, 0:128], in_=sguw_d[h]), w=['t_f'], dma='c0')
            A('dve', lambda e: e.tensor_tensor(out=t_b[:, 0:128], in0=t_f[:, 0:128], in1=trilf, op=ALU.mult), r=['t_f', 'trilf'], w=['t_b'])
            A('pe', lambda e: e.transpose(out=bankb(0)[:, 0:128], in_=t_b[:, 0:128], identity=identb[:]), r=['t_b', 'identb'], w=['pb0'])
            A('dve', lambda e, h=h: e.tensor_copy(out=sguTp[:, h, :], in_=bankb(0)[:, 0:128]), r=['pb0'], w=['sguTp'])
        e8f = a.f(128); e8b = a.b(128); w8f = a.f(128); w8b = a.b(128)
        A('sp', lambda e: e.dma_start(out=e8f[0:8, :], in_=e8_d), w=['e8f'], dma='c5')
        A('dve', lambda e: e.tensor_copy(out=e8b[0:8, :], in_=e8f[0:8, :]), r=['e8f'], w=['e8b'])
        for h in range(8):
            A('sp', lambda e, h=h: e.dma_start(out=w8f[0:8, :].rearrange("p (a b) -> p a b", b=8),
                                               in_=bc(sguw_d[h, 0:8, 0:8], [8, 16, 8], 1)), w=['w8f'], dma='c9')
            A('dve', lambda e: e.tensor_copy(out=w8b[0:8, :], in_=w8f[0:8, :]), r=['w8f'], w=['w8b'])
            A('pe', lambda e: e.matmul(out=bank(2)[:, 0:128], lhsT=w8b[0:8, :], rhs=e8b[0:8, :], start=True, stop=True), r=['w8b', 'e8b'], w=['pb2'])
            A('dve', lambda e, h=h: e.tensor_tensor(out=sguTs[:, h, :], in0=bank(2)[:, 0:128], in1=bdTf, op=ALU.mult), r=['pb2', 'bdTf'], w=['sguTs'])
        plf = a.f(D)
        for kc in range(2):
            A('sp', lambda e, kc=kc: e.dma_start(out=plf, in_=plew_d[kc * 128:(kc + 1) * 128, :]), w=['plf'], dma='c7')
            A('dve', lambda e, kc=kc: e.tensor_copy(out=plewb[:, kc, :], in_=plf), r=['plf'], w=['plewb'])
        ckf = a.f(16 * 128).rearrange("p (a b) -> p a b", b=128)
        ckb = a.b(16 * 128).rearrange("p (a b) -> p a b", b=128)
        A('sp', lambda e: e.dma_start(out=ckf, in_=ck_d.rearrange("b s c -> s b c")), w=['ckf'], dma='c8')
        A('dve', lambda e: e.tensor_copy(out=ckb, in_=ckf), r=['ckf'], w=['ckb'])
        for b in range(16):
            A('pe', lambda e, b=b: e.transpose(out=bankb(0)[:, (b % 8) * 128:(b % 8 + 1) * 128], in_=ckb[:, b, :], identity=identb[:]), r=['ckb', 'identb'], w=['pb0'])
            if b % 8 == 7:
                A('dve', lambda e, b=b: e.tensor_copy(out=kTc[:, b - 7:b + 1, :], in_=bankb(0).rearrange("p (a b) -> p a b", b=128)), r=['pb0'], w=['kTc'])
        A('sp', lambda e: e.dma_start(out=ckf, in_=cv_d.rearrange("b s c -> s b c")), w=['ckf'], dma='c8')
        A('dve', lambda e: e.memset(vaugc[:], 1.0), w=['vaugc'])
        A('dve', lambda e: e.tensor_copy(out=vaugc[:, :, :, 0:64], in_=ckf.rearrange("p a (k c) -> p a k c", c=64)), r=['ckf'], w=['vaugc'])
        A('dve', lambda e: e.memset(vaugr[:], 1.0), w=['vaugr'])
        A('sp', lambda e: e.dma_start(out=wks_d[:, 0:120, :], in_=ck_d[:, 8:128, :]), dma='o_w')
        A('sp', lambda e: e.dma_start(out=wvs_d[:, 0:120, :], in_=cv_d[:, 8:128, :]), dma='o_w')
        S.barrier()

        a = Ar()
        NBC = 3
        cvf = [a.f(3328) for _ in range(NBC)]
        cvb = [a.b(3328) for _ in range(NBC)]
        jobs = []
        for (src, dst, ncol, gi) in ((win_d, win_s, 3328, 0), (wout_d, wout_s, D, 1), (wq_d, wq_s, D, 2), (gw_d, gw_s, D, 3)):
            for dc in range(16):
                jobs.append((src, dst, ncol, gi, dc))

        def cv_store(k):
            src, dst, ncol, gi, dc = jobs[k]
            s = k % NBC
            A('sp', lambda e: e.dma_start(out=dst[dc * 128:(dc + 1) * 128, :], in_=cvb[s][:, 0:ncol]), r=['cvb%d' % s], dma='cs%d' % s)

        for k, (src, dst, ncol, gi, dc) in enumerate(jobs):
            s = k % NBC
            A('sp', lambda e: e.dma_start(out=cvf[s][:, 0:ncol], in_=src[dc * 128:(dc + 1) * 128, :]), w=['cvf%d' % s], dma='cv%d' % s)
            if k >= 2:
                cv_store(k - 2)
            if dc % 2:
                A('act', lambda e: e.activation(out=cvb[s][:, 0:ncol], in_=cvf[s][:, 0:ncol], func=AF.Copy, scale=gcol[:, gi, dc:dc + 1]),
                  r=['cvf%d' % s, 'gcol'], w=['cvb%d' % s])
            else:
                A('dve', lambda e: e.tensor_scalar(out=cvb[s][:, 0:ncol], in0=cvf[s][:, 0:ncol], scalar1=gcol[:, gi, dc:dc + 1], scalar2=None, op0=ALU.mult),
                  r=['cvf%d' % s, 'gcol'], w=['cvb%d' % s])
        cv_store(len(jobs) - 2)
        cv_store(len(jobs) - 1)
        S.barrier()
        a = Ar()
        NBU = 3
        uf = [a.f(D) for _ in range(NBU)]
        ub = [a.b(D) for _ in range(NBU)]
        ut = [a.b(D) for _ in range(NBU)]
        vf_ = [a.f(D) for _ in range(NBU)]
        vb_ = [a.b(D) for _ in range(NBU)]
        gfr2 = a.f(D)
        A('sp', lambda e: e.dma_start(out=gfr2, in_=gffn_d.partition_broadcast(128)), w=['gfr2'], dma='c6')
        Uv = U_d.rearrange("(i j) d -> j i d", j=128)
        Vv = V_d.rearrange("(i j) d -> j i d", j=128)

        def uv_store(j):
            s = j % NBU
            A('sp', lambda e: e.dma_start(out=UT_s[j], in_=ut[s]), r=['ut%d' % s], dma='us%d' % s)
            A('sp', lambda e: e.dma_start(out=VB_s[j], in_=vb_[s]), r=['vb%d' % s], dma='vs%d' % s)

        for j in range(128):
            s = j % NBU
            A('sp', lambda e: e.dma_start(out=uf[s], in_=Uv[j]), w=['uf%d' % s], dma='uf%d' % s)
            A('sp', lambda e: e.dma_start(out=vf_[s], in_=Vv[j]), w=['vf%d' % s], dma='vf%d' % s)
            if j >= 2:
                uv_store(j - 2)
            A('dve', lambda e: e.tensor_tensor(out=ub[s], in0=uf[s], in1=gfr2, op=ALU.mult), r=['uf%d' % s, 'gfr2'], w=['ub%d' % s])
            pk = 2 * s
            pb = bankb(pk, 2)
            for dc in range(16):
                A('pe', lambda e, dc=dc: e.transpose(out=pb[:, dc * 128:(dc + 1) * 128], in_=ub[s][:, dc * 128:(dc + 1) * 128], identity=identb[:]),
                  r=['ub%d' % s, 'identb'], w=['pb%d' % pk, 'pb%d' % (pk + 1)])
            A('act', lambda e: e.copy(out=ut[s], in_=pb), r=['pb%d' % pk, 'pb%d' % (pk + 1)], w=['ut%d' % s])
            A('act', lambda e: e.copy(out=vb_[s], in_=vf_[s]), r=['vf%d' % s], w=['vb%d' % s])
        uv_store(126)
        uv_store(127)
        S.barrier()

        win_v = win_s.rearrange("(dc p) c -> p dc c", p=128)
        wout_v = wout_s.rearrange("(dc p) c -> p dc c", p=128)
        wq_v = wq_s.rearrange("(dc p) c -> p dc c", p=128)
        gw_v = gw_s.rearrange("(dc p) c -> p dc c", p=128)

        def load_w(view, c0, w):
            i = next_slot()
            ws = wslot[i][:, 0:16 * w].rearrange("p (a b) -> p a b", b=w)
            A('sp', lambda e: e.dma_start(out=ws, in_=view[:, :, c0:c0 + w]), r=['scr'], w=['ws%d' % i], dma='ws%d' % i)
            return ws, 'ws%d' % i

        for sb in sb_list:
            sample = (sb == 16)
            ntb = 1 if sample else 2
            T = 128 * ntb
            r0 = sb * 256
            a = Ar()
            xT = a.b(16 * 256).rearrange("p (a b) -> p a b", b=256)
            mixT_f = a.f(16 * 128)
            mixT = mixT_f.bitcast(BF16).rearrange("p (a b) -> p a b", b=256)
            tmpb = a.b(D)
            junk = a.f(D)
            qf = a.f(2 * 1024).rearrange("p (a b) -> p a b", b=1024)
            qb = a.b(2 * 1024).rearrange("p (a b) -> p a b", b=1024)
            kvf = a.f(2 * 256).rearrange("p (a b) -> p a b", b=256)
            kb = a.b(2 * 128).rearrange("p (a b) -> p a b", b=128)
            su = a.f(2 * 1024).rearrange("p (a b) -> p a b", b=1024)
            svf = a.f(2 * 1024).rearrange("p (a b) -> p a b", b=1024)
            svb = a.b(2 * 1024).rearrange("p (a b) -> p a b", b=1024)
            tmpf = a.f(1024)
            rt = a.f(128)
            tk = [a.f(128) for _ in range(2)]
            qT = a.b(2 * 1024).rearrange("p (t h q) -> p t h q", t=2, h=8)
            pT = [a.b(16 * 128).rearrange("p (h q) -> p h q", q=128) for _ in range(2)]
            attnf = a.f(1024)
            mixed_f = a.f(D)
            mixed = mixed_f.bitcast(BF16).rearrange("p (a b) -> p a b", b=D)
            tq = [mixT_f[:, tb * 1024:(tb + 1) * 1024] for tb in range(2)]
            TQT = ['tq%d' % t for t in range(2)] + ['tq%d_%d' % (t, i) for t in range(2) for i in range(4)]
            tsv = [mixed_f[:, tb * 1024:(tb + 1) * 1024] for tb in range(2)]

            for tb in range(ntb):
                A('sp', lambda e, tb=tb: e.dma_start(out=xin[:, tb, :], in_=x_d[r0 + tb * 128:r0 + (tb + 1) * 128, :]), w=['xin%d' % tb], dma='xi%d' % tb)
                A('sp', lambda e, tb=tb: e.dma_start(out=pin[:, tb, :], in_=p_d[r0 + tb * 128:r0 + (tb + 1) * 128, :]), w=['pin%d' % tb], dma='pi%d' % tb)
            for tb in range(ntb):
                X = 'xin%d' % tb
                A('act', lambda e, tb=tb: e.activation(out=junk, in_=xin[:, tb, :], func=AF.Square, accum_out=stat[:, 16 + tb:17 + tb]), r=[X], w=['junk', 'st_a%d' % tb])
                rsqrt_ops(stat[:, tb:tb + 1], stat[:, 16 + tb:17 + tb], D, rr=['st_a%d' % tb], ww=['rstd1_%d' % tb])
                A('dve', lambda e, tb=tb: e.tensor_copy(out=tmpb, in_=xin[:, tb, :]), r=[X], w=['tmpb'])
                transposes16(tmpb, xT, slice(tb * 128, (tb + 1) * 128), 0, ['tmpb'], ['xT'], 'dve')
            slices = [(0, 512, 'q'), (512, 512, 'q'), (1024, 256, 'kv'), (2304, 512, 'sv'), (2816, 512, 'sv'), (1280, 512, 'su'), (1792, 512, 'su')]
            for si, (c0, w, kind) in enumerate(slices):
                ws, wr = load_w(win_v, c0, w)
                for tb in range(ntb):
                    pk = 2 + (si * 2 + tb) % 2
                    ps = bank(pk)[:, 0:w]
                    for dc in range(16):
                        A('pe', lambda e, dc=dc, tb=tb, ps=ps, ws=ws: e.matmul(out=ps, lhsT=xT[:, dc, tb * 128:(tb + 1) * 128], rhs=ws[:, dc, :], start=(dc == 0), stop=(dc == 15)),
                          r=['xT', wr], w=['pb%d' % pk])
                    rs = stat[:, tb:tb + 1]
                    if kind == 'q':
                        dst, fn, wn = qf[:, tb, c0:c0 + w], AF.Copy, 'qf%d' % tb
                    elif kind == 'kv':
                        dst, fn, wn = kvf[:, tb, :], AF.Copy, 'kvf%d' % tb
                    elif kind == 'su':
                        dst, fn, wn = su[:, tb, c0 - 1280:c0 - 1280 + w], AF.Gelu_apprx_tanh, 'su%d' % tb
                    else:
                        dst, fn, wn = svf[:, tb, c0 - 2304:c0 - 2304 + w], AF.Gelu_apprx_tanh, 'svf%d' % tb
                    A('act', lambda e, dst=dst, fn=fn, ps=ps, rs=rs: e.activation(out=dst, in_=ps, func=fn, scale=rs), r=['pb%d' % pk, 'rstd1_%d' % tb], w=[wn])

            TF = ['tmpf', 'tmpf1', 'tmpf2', 'tmpf3']

            def headnorm_ops(buf, nh, hd, gain, gtag, btag, scr, stag, rtc, rtag, extra_w=()):
                v3 = buf.rearrange("p (h c) -> p h c", c=hd)
                sc_ = scr[:, 0:nh * hd]
                t3 = sc_.rearrange("p (h c) -> p h c", c=hd)
                return [
                    ('dve', lambda e: e.tensor_tensor(out=sc_, in0=buf, in1=buf, op=ALU.mult), [btag], [stag] + list(extra_w)),
                    ('dve', lambda e: e.tensor_reduce(out=rtc, in_=t3, axis=AX.X, op=ALU.add), [stag], [rtag]),
                    ('dve', lambda e: e.tensor_scalar(out=rtc, in0=rtc, scalar1=1.0 / hd, scalar2=EPS, op0=ALU.mult, op1=ALU.add), [rtag], [rtag]),
                    ('pool', lambda e: e.tensor_tensor(out=rtc, in0=rtc, in1=mhalf[:, 0:nh], op=ALU.pow), [rtag, 'mhalf'], [rtag]),
                    ('dve', lambda e: e.tensor_tensor(out=v3, in0=v3, in1=bc(rtc, [128, nh, hd], 2), op=ALU.mult), [btag, rtag], [btag]),
                    ('dve', lambda e: e.tensor_tensor(out=v3, in0=v3, in1=bc(gain, [128, nh, hd], 1), op=ALU.mult), [btag, gtag], [btag]),
                ]

            def rope_ops(buf, nh, cs, btag, scr, stag):
                v3 = buf.rearrange("p (h c) -> p h c", c=64)
                x1, x2 = v3[:, :, 0:8], v3[:, :, 8:16]
                cb = bc(cs[:, 0, :], [128, nh, 8], 1)
                sn = bc(cs[:, 1, :], [128, nh, 8], 1)
                n8 = nh * 8
                t = [scr[:, i * n8:(i + 1) * n8].rearrange("p (h c) -> p h c", c=8) for i in range(4)]
                st = [stag + '_%d' % i for i in range(4)]
                return [
                    ('dve', lambda e: e.tensor_tensor(out=t[0], in0=x1, in1=cb, op=ALU.mult), [btag, stag], [st[0]]),
                    ('dve', lambda e: e.tensor_tensor(out=t[1], in0=x2, in1=sn, op=ALU.mult), [btag, stag], [st[1]]),
                    ('dve', lambda e: e.tensor_tensor(out=t[2], in0=x2, in1=cb, op=ALU.mult), [btag, stag], [st[2]]),
                    ('dve', lambda e: e.tensor_tensor(out=t[3], in0=x1, in1=sn, op=ALU.mult), [btag, stag], [st[3]]),
                    ('dve', lambda e: e.tensor_tensor(out=x1, in0=t[0], in1=t[1], op=ALU.subtract), st, [btag]),
                    ('dve', lambda e: e.tensor_tensor(out=x2, in0=t[2], in1=t[3], op=ALU.add), st, [btag]),
                ]

            chains, delays = [], []
            for tb in range(ntb):
                gb = sb * 2 + tb
                blk = gb % 16
                cs = css[:] if sample else csp[:, blk, :, :]
                slot = gb % 3
                QF, KVF, SVF = 'qf%d' % tb, 'kvf%d' % tb, 'svf%d' % tb
                cq = headnorm_ops(qf[:, tb, :], 16, 64, qg[:], 'qg', QF, tq[tb], 'tq%d' % tb, rt[:, tb * 16:tb * 16 + 16], 'rtq%d' % tb)
                cq += rope_ops(qf[:, tb, :], 16, cs, QF, tq[tb], 'tq%d' % tb)
                cq.append(('dve', lambda e, tb=tb: e.tensor_copy(out=qb[:, tb, :].rearrange("p (h g c) -> p g h c", g=2, c=64),
                                                                 in_=qf[:, tb, :].rearrange("p (g h c) -> p g h c", g=2, c=64)), [QF], ['qb%d' % tb]))
                ck = headnorm_ops(kvf[:, tb, 0:128], 2, 64, kg[:], 'kg', KVF, tk[tb], 'tk%d' % tb, rt[:, 32 + tb * 2:34 + tb * 2], 'rtk%d' % tb)
                ck += rope_ops(kvf[:, tb, 0:128], 2, cs, KVF, tk[tb], 'tk%d' % tb)
                ck.append(('dve', lambda e, tb=tb: e.tensor_copy(out=kb[:, tb, :], in_=kvf[:, tb, 0:128]), [KVF], ['kb%d' % tb]))
                ck.append(('dve', lambda e, tb=tb, slot=slot: e.tensor_copy(out=vaugr[:, slot, :, 0:64], in_=kvf[:, tb, 128:256].rearrange("p (k c) -> p k c", c=64)),
                           [KVF], ['vaug%d' % slot]))
                csv_ = headnorm_ops(svf[:, tb, :], 8, 128, sgn[:], 'sgn', SVF, tsv[tb], 'mixed%d' % tb, rt[:, 40 + tb * 8:48 + tb * 8], 'rtsv%d' % tb)
                csv_.append(('pool', lambda e, tb=tb: e.tensor_copy(out=svb[:, tb, :], in_=svf[:, tb, :]), [SVF], ['svb%d' % tb]))
                chains += [cq, ck, csv_]
                delays += [0, 4, 9]
            rnd = 0
            while any(chains):
                for ch, dl in zip(chains, delays):
                    if ch and rnd >= dl:
                        eng_, fn_, r_, w_ = ch.pop(0)
                        A(eng_, fn_, r=r_, w=w_)
                rnd += 1

            for tb in range(ntb):
                gb = sb * 2 + tb
                blk = gb % 16
                slot = gb % 3
                if sample:
                    for b in range(16):
                        A('sp', lambda e, b=b: e.dma_start(out=wks_d[b, 120:128, :], in_=kvf[b * 8:(b + 1) * 8, 0, 0:128]), r=['kvf0'], dma='o_w')
                        A('sp', lambda e, b=b: e.dma_start(out=wvs_d[b, 120:128, :], in_=kvf[b * 8:(b + 1) * 8, 0, 128:256]), r=['kvf0'], dma='o_w')
                    A('sp', lambda e: e.dma_start(out=svs_d, in_=svf[:, 0, :]), r=['svf0'], dma='o_w')
                elif blk == 15:
                    seq = gb // 16
                    A('sp', lambda e, tb=tb, seq=seq: e.dma_start(out=wkp_d[seq], in_=kvf[:, tb, 0:128]), r=['kvf%d' % tb], dma='o_w')
                    A('sp', lambda e, tb=tb, seq=seq: e.dma_start(out=wvp_d[seq], in_=kvf[:, tb, 128:256]), r=['kvf%d' % tb], dma='o_w')
                pbq = bankb(0)
                for h in range(8):
                    A('pe', lambda e, h=h, tb=tb: e.transpose(out=pbq[:, h * 128:(h + 1) * 128], in_=qb[:, tb, h * 128:(h + 1) * 128], identity=identb[:]),
                      r=['qb%d' % tb, 'identb'], w=['pb0'])
                A('act', lambda e, tb=tb: e.copy(out=qT[:, tb, :, :], in_=pbq.rearrange("p (h q) -> p h q", q=128)), r=['pb0'], w=['qT%d' % tb])
                A('pe', lambda e, tb=tb: e.transpose(out=bankb(1)[:, 0:128], in_=kb[:, tb, :], identity=identb[:]), r=['kb%d' % tb, 'identb'], w=['pb1'])
                A('act', lambda e, slot=slot: e.copy(out=kTr[:, slot, :], in_=bankb(1)[:, 0:128]), r=['pb1'], w=['kT%d' % slot])

                sT = sguTs if sample else sguTp
                bT = bTs if sample else bTp
                zp = bank(0, 2)
                for h in range(8):
                    A('pe', lambda e, h=h, tb=tb, sT=sT: e.matmul(out=zp[:, h * 128:(h + 1) * 128], lhsT=sT[:, h, :], rhs=svb[:, tb, h * 128:(h + 1) * 128], start=True, stop=True),
                      r=['svb%d' % tb, 'sguTp', 'sguTs'], w=['pb0', 'pb1'])
                t3 = tmpf.rearrange("p (h c) -> p h c", c=128)
                A('dve', lambda e, bT=bT: e.tensor_tensor(out=t3, in0=zp.rearrange("p (h c) -> p h c", c=128), in1=bc(bT[:], [128, 8, 128], 2), op=ALU.add),
                  r=['pb0', 'pb1', 'bTp', 'bTs'], w=TF)
                A('dve', lambda e, tb=tb: e.tensor_tensor(out=tmpf, in0=tmpf, in1=su[:, tb, :], op=ALU.mult), r=TF + ['su%d' % tb], w=TF)
                A('act', lambda e, tb=tb: e.activation(out=junk[:, 0:1024], in_=tmpf, func=AF.Square, accum_out=stat[:, 20 + tb:21 + tb]), r=TF, w=['junk', 'st_c%d' % tb])
                rsqrt_ops(stat[:, 4 + tb:5 + tb], stat[:, 20 + tb:21 + tb], 1024, rr=['st_c%d' % tb], ww=['rstd_s%d' % tb])
                A('dve', lambda e, tb=tb: e.tensor_scalar(out=mixed[:, tb, 1024:2048], in0=tmpf, scalar1=stat[:, 4 + tb:5 + tb], scalar2=None, op0=ALU.mult),
                  r=TF + ['rstd_s%d' % tb], w=['mixed%d' % tb])

                if sample:
                    kbl = [(kTc[:, b, :], vaugc[:, b, :, :], masks[:, b, :], ['kTc'], ['vaugc']) for b in range(16)]
                    kbl.append((kTr[:, slot, :], vaugr[:, slot, :, :], masks[:, 16, :], ['kT%d' % slot], ['vaug%d' % slot]))
                else:
                    kbl = []
                    if blk > 0:
                        ps_ = (gb - 1) % 3
                        kbl.append((kTr[:, ps_, :], vaugr[:, ps_, :, :], maskp[:, 0, :], ['kT%d' % ps_], ['vaug%d' % ps_]))
                    kbl.append((kTr[:, slot, :], vaugr[:, slot, :, :], maskp[:, 1, :], ['kT%d' % slot], ['vaug%d' % slot]))
                nkb = len(kbl)
                for ki, (kT_, va_, mk_, kres, vres) in enumerate(kbl):
                    pti = ki % 2
                    for kh in range(2):
                        for half in range(2):
                            pk = 2 + (kh * 2 + half) % 2
                            ps = bank(pk)
                            A('pe', lambda e, kh=kh, half=half, ps=ps, kT_=kT_, tb=tb: e.matmul(
                                out=ps, lhsT=kT_[kh * 64:(kh + 1) * 64, :], rhs=qT[kh * 64:(kh + 1) * 64, tb, half * 4:(half + 1) * 4, :], start=True, stop=False),
                              r=kres + ['qT%d' % tb], w=['pb%d' % pk])
                            for hh in range(4):
                                A('pe', lambda e, ps=ps, mk_=mk_, hh=hh: e.matmul(out=ps[:, hh * 128:(hh + 1) * 128], lhsT=identb[:], rhs=mk_, start=False, stop=True),
                                  r=['identb', 'maskp', 'masks'], w=['pb%d' % pk])
                            h0 = kh * 8 + half * 4
                            A('act', lambda e, ps=ps, pti=pti, h0=h0: e.activation(out=pT[pti][:, h0:h0 + 4, :], in_=ps.rearrange("p (h q) -> p h q", q=128), func=AF.Exp, scale=0.125),
                              r=['pb%d' % pk], w=['pT%d_%d' % (pti, h0)])
                    for h in range(16):
                        pk = 4 + h // 4
                        o = bank(pk)[:, (h % 4) * 128:(h % 4) * 128 + 65]
                        h0 = (h // 4) * 4
                        A('pe', lambda e, h=h, o=o, pti=pti, va_=va_, ki=ki: e.matmul(out=o, lhsT=pT[pti][:, h, :], rhs=va_[:, h // 8, :], start=(ki == 0 and h % 4 == 0), stop=(ki == nkb - 1)),
                          r=['pT%d_%d' % (pti, h0)] + vres, w=['pb%d' % pk])
                o4 = bank(4, 4).rearrange("p (h c) -> p h c", c=128)
                ob = ['pb4', 'pb5', 'pb6', 'pb7']
                A('dve', lambda e: e.tensor_tensor(out=rt[:, 64:80], in0=o4[:, :, 64], in1=esink[:], op=ALU.add), r=ob + ['esink'], w=['rt2'])
                A('dve', lambda e: e.reciprocal(out=rt[:, 64:80], in_=rt[:, 64:80]), r=['rt2'], w=['rt2'])
                A('dve', lambda e: e.tensor_tensor(out=attnf.rearrange("p (h c) -> p h c", c=64), in0=o4[:, :, 0:64], in1=bc(rt[:, 64:80], [128, 16, 64], 2), op=ALU.mult),
                  r=ob + ['rt2'], w=['attnf'])
                A('act', lambda e, tb=tb: e.activation(out=junk[:, 1024:2048], in_=attnf, func=AF.Square, accum_out=stat[:, 18 + tb:19 + tb]), r=['attnf'], w=['junkb', 'st_b%d' % tb])
                rsqrt_ops(stat[:, 2 + tb:3 + tb], stat[:, 18 + tb:19 + tb], 1024, rr=['st_b%d' % tb], ww=['rstd_a%d' % tb])
                A('dve', lambda e, tb=tb: e.tensor_scalar(out=mixed[:, tb, 0:1024], in0=attnf, scalar1=stat[:, 2 + tb:3 + tb], scalar2=None, op0=ALU.mult),
                  r=['attnf', 'rstd_a%d' % tb], w=['mixed%d' % tb])
            for tb in range(ntb):
                transposes16(mixed[:, tb, :], mixT, slice(tb * 128, (tb + 1) * 128), 0, ['mixed%d' % tb], ['mixT'] + TQT, 'act')
            for si in range(4):
                ws, wr = load_w(wout_v, si * 512, 512)
                for tb in range(ntb):
                    pk = 2 + (si * 2 + tb) % 2
                    ps = bank(pk)
                    for dc in range(16):
                        A('pe', lambda e, dc=dc, tb=tb, ps=ps, ws=ws: e.matmul(out=ps, lhsT=mixT[:, dc, tb * 128:(tb + 1) * 128], rhs=ws[:, dc, :], start=(dc == 0), stop=(dc == 15)),
                          r=['mixT', wr], w=['pb%d' % pk])
                    A('dve', lambda e, tb=tb, si=si, ps=ps: e.tensor_tensor(out=xin[:, tb, si * 512:(si + 1) * 512], in0=xin[:, tb, si * 512:(si + 1) * 512], in1=ps, op=ALU.add),
                      r=['pb%d' % pk, 'xin%d' % tb], w=['xin%d' % tb])
            if dbg and sb == 0:
                for tb in range(ntb):
                    A('sp', lambda e, tb=tb: e.dma_start(out=dh_d[tb * 128:(tb + 1) * 128, :], in_=xin[:, tb, :]), r=['xin%d' % tb], dma='o_w')
            S.barrier()

            a = Ar()
            hnT = a.b(16 * 256).rearrange("p (a b) -> p a b", b=256)
            selT = a.b(3 * 256).rearrange("p (a b) -> p a b", b=256)
            off_keep = a.o
            tmpb = a.b(D)
            junk = a.f(D)
            qTp = a.b(16 * 256).rearrange("p (g t) -> p g t", t=256)
            sc = a.f(D)
            work = a.f(D)
            vv = a.f(256)
            ixu = a.f(256).bitcast(U32)
            ixf = a.f(256)
            cand = a.f(D)
            cv_ = a.f(128); ciu = a.f(128).bitcast(U32); cif = a.f(128)
            k1f = a.f(128); k2f = a.f(128); ee = a.f(128)
            eq = a.f(D)
            sel = a.b(3 * 128).rearrange("p (a b) -> p a b", b=128)

            for tb in range(ntb):
                X = 'xin%d' % tb
                A('act', lambda e, tb=tb: e.activation(out=junk, in_=xin[:, tb, :], func=AF.Square, accum_out=stat[:, 22 + tb:23 + tb]), r=[X], w=['junk', 'st_d%d' % tb])
                rsqrt_ops(stat[:, 6 + tb:7 + tb], stat[:, 22 + tb:23 + tb], D, rr=['st_d%d' % tb], ww=['rstd2_%d' % tb])
                A('dve', lambda e, tb=tb: e.tensor_scalar(out=tmpb, in0=xin[:, tb, :], scalar1=stat[:, 6 + tb:7 + tb], scalar2=None, op0=ALU.mult), r=[X, 'rstd2_%d' % tb], w=['tmpb'])
                transposes16(tmpb, hnT, slice(tb * 128, (tb + 1) * 128), 0, ['tmpb'], ['hnT'], 'act')
            for si in range(4):
                ws, wr = load_w(wq_v, si * 512, 512)
                for gl in range(4):
                    g = si * 4 + gl
                    pk = 2 + g % 2
                    ps = bank(pk)[:, 0:T]
                    for dc in range(16):
                        A('pe', lambda e, dc=dc, gl=gl, ps=ps, ws=ws: e.matmul(out=ps, lhsT=ws[:, dc, gl * 128:(gl + 1) * 128], rhs=hnT[:, dc, 0:T], start=(dc == 0), stop=(dc == 15)),
                          r=['hnT', wr], w=['pb%d' % pk])
                    ev = 'act' if g % 2 else 'dve'
                    A(ev, lambda e, g=g, ps=ps, ev=ev: (e.copy(out=qTp[:, g, 0:T], in_=ps) if ev == 'act' else e.tensor_copy(out=qTp[:, g, 0:T], in_=ps)),
                      r=['pb%d' % pk], w=['qTp'])
            for tb in range(ntb):
                sp4 = bank(4, 4)
                for g in range(16):
                    A('pe', lambda e, g=g, tb=tb: e.matmul(out=sp4[:, g * 128:(g + 1) * 128], lhsT=qTp[:, g, tb * 128:(tb + 1) * 128], rhs=k12T[:, g % 2, :], start=True, stop=True),
                      r=['qTp', 'k12T'], w=['pb4', 'pb5', 'pb6', 'pb7'])
                A('act', lambda e: e.copy(out=sc, in_=sp4), r=['pb4', 'pb5', 'pb6', 'pb7'], w=['sc'])
                v3 = vv.rearrange("p (g k) -> p g k", k=16)
                i3 = ixu.rearrange("p (g k) -> p g k", k=16)
                SG = [sc[:, g * 128:(g + 1) * 128] for g in range(16)]
                WG = [work[:, g * 128:(g + 1) * 128] for g in range(16)]
                R0 = ['sc']
                for g in range(16):
                    A('dve', lambda e, g=g: e.max(out=v3[:, g, 0:8], in_=SG[g]), r=R0, w=['vv%d' % g])
                for g in range(16):
                    A('dve', lambda e, g=g: e.max_index(out=i3[:, g, 0:8], in_max=v3[:, g, 0:8], in_values=SG[g]), r=R0 + ['vv%d' % g], w=['ix%d' % g])
                for g in range(16):
                    A('dve', lambda e, g=g: e.match_replace(out=WG[g], in_to_replace=v3[:, g, 0:8], in_values=SG[g], imm_value=-1e30), r=R0 + ['vv%d' % g], w=['wk%d' % g])
                for g in range(16):
                    A('dve', lambda e, g=g: e.max(out=v3[:, g, 8:16], in_=WG[g]), r=['wk%d' % g], w=['vv%d' % g])
                for g in range(16):
                    A('dve', lambda e, g=g: e.max_index(out=i3[:, g, 8:16], in_max=v3[:, g, 8:16], in_values=WG[g]), r=['wk%d' % g, 'vv%d' % g], w=['ix%d' % g])
                allv = ['vv%d' % g for g in range(16)]
                alli = ['ix%d' % g for g in range(16)]
                A('dve', lambda e: e.tensor_copy(out=ixf, in_=ixu), r=alli, w=['ixf'])
                v4 = vv.rearrange("p (h s k) -> p h s k", s=2, k=16)
                c4 = cand.rearrange("p (h a b) -> p h a b", a=16, b=16)
                A('dve', lambda e: e.tensor_tensor(out=c4, in0=bc(v4[:, :, 0, :], [128, 8, 16, 16], 3), in1=bc(v4[:, :, 1, :], [128, 8, 16, 16], 2), op=ALU.add), r=allv, w=['cand'])
                cv3 = cv_.rearrange("p (h k) -> p h k", k=16)
                ci3 = ciu.rearrange("p (h k) -> p h k", k=16)
                CH = [cand[:, h * 256:(h + 1) * 256] for h in range(8)]
                WH = [work[:, h * 256:(h + 1) * 256] for h in range(8)]
                for h in range(8):
                    A('dve', lambda e, h=h: e.max(out=cv3[:, h, 0:8], in_=CH[h]), r=['cand'], w=['cv%d' % h])
                for h in range(8):
                    A('dve', lambda e, h=h: e.max_index(out=ci3[:, h, 0:8], in_max=cv3[:, h, 0:8], in_values=CH[h]), r=['cand', 'cv%d' % h], w=['ci%d' % h])
                for h in range(8):
                    A('dve', lambda e, h=h: e.match_replace(out=WH[h], in_to_replace=cv3[:, h, 0:8], in_values=CH[h], imm_value=-1e30),
                      r=['cand', 'cv%d' % h], w=['wk%d' % (2 * h), 'wk%d' % (2 * h + 1)])
                for h in range(8):
                    A('dve', lambda e, h=h: e.max(out=cv3[:, h, 8:16], in_=WH[h]), r=['wk%d' % (2 * h)], w=['cv%d' % h])
                for h in range(8):
                    A('dve', lambda e, h=h: e.max_index(out=ci3[:, h, 8:16], in_max=cv3[:, h, 8:16], in_values=WH[h]), r=['wk%d' % (2 * h), 'cv%d' % h], w=['ci%d' % h])
                allc = ['cv%d' % h for h in range(8)]
                allci = ['ci%d' % h for h in range(8)]
                e3 = ee.rearrange("p (h k) -> p h k", k=16)
                A('dve', lambda e: e.tensor_tensor(out=e3, in0=cv3, in1=bc(cv3[:, :, 0], [128, 8, 16], 2), op=ALU.subtract), r=allc, w=['ee'])
                A('act', lambda e: e.activation(out=ee, in_=ee, func=AF.Exp), r=['ee'], w=['ee'])
                A('dve', lambda e: e.tensor_reduce(out=rt[:, 32:40], in_=e3, axis=AX.X, op=ALU.add), r=['ee'], w=['rt3'])
                A('dve', lambda e: e.reciprocal(out=rt[:, 32:40], in_=rt[:, 32:40]), r=['rt3'], w=['rt3'])
                A('dve', lambda e: e.tensor_tensor(out=sel[:, 2, :].rearrange("p (h k) -> p h k", k=16), in0=e3, in1=bc(rt[:, 32:40], [128, 8, 16], 2), op=ALU.mult),
                  r=['ee', 'rt3'], w=['sel'])
                cifu = cif.bitcast(U32)
                A('dve', lambda e: e.tensor_scalar(out=cifu, in0=ciu, scalar1=15, scalar2=None, op0=ALU.bitwise_and), r=allci, w=['cif'])
                A('dve', lambda e: e.tensor_copy(out=k2f, in_=cifu), r=['cif'], w=['k2f'])
                A('dve', lambda e: e.tensor_scalar(out=cifu, in0=ciu, scalar1=4, scalar2=None, op0=ALU.logical_shift_right), r=allci + ['k2f'], w=['cif'])
                A('dve', lambda e: e.tensor_copy(out=k1f, in_=cifu), r=['cif'], w=['k1f'])
                q4 = eq.rearrange("p (h a b) -> p h a b", a=16, b=16)
                if4 = ixf.rearrange("p (h s k) -> p h s k", s=2, k=16)
                for side, kf_ in ((0, k1f), (1, k2f)):
                    kk = kf_.rearrange("p (h k) -> p h k", k=16)
                    A('dve', lambda e, kk=kk: e.tensor_tensor(out=q4, in0=bc(kk, [128, 8, 16, 16], 3), in1=bc(bc(iota16[:], [128, 16, 16], 1), [128, 8, 16, 16], 1), op=ALU.is_equal),
                      r=['k1f', 'k2f', 'iota16'], w=['eq'])
                    A('dve', lambda e, side=side: e.tensor_tensor(out=q4, in0=q4, in1=bc(if4[:, :, side, :], [128, 8, 16, 16], 2), op=ALU.mult), r=['eq', 'ixf'], w=['eq'])
                    A('dve', lambda e: e.tensor_reduce(out=k1f if False else work[:, 0:128], in_=q4.rearrange("p h a b -> p (h a) b"), axis=AX.X, op=ALU.add), r=['eq'], w=['wk0'])
                    A('dve', lambda e, side=side: e.tensor_copy(out=sel[:, side, :], in_=work[:, 0:128]), r=['wk0'], w=['sel'])
                for i in range(3):
                    A('pe', lambda e, i=i: e.transpose(out=bankb(0)[:, i * 128:(i + 1) * 128], in_=sel[:, i, :], identity=identb[:]), r=['sel', 'identb'], w=['pb0'])
                A('act', lambda e, tb=tb: e.copy(out=selT[:, :, tb * 128:(tb + 1) * 128], in_=bankb(0)[:, 0:384].rearrange("p (a b) -> p a b", b=128)), r=['pb0'], w=['selT'])
            S.barrier()

            a = Ar()
            a.o = off_keep
            G = a.b(128 * 256).rearrange("p (j t) -> p j t", t=256)
            NPB_ = 4
            P1 = [a.b(TG * 128).rearrange("p (t i) -> p t i", i=128) for _ in range(NPB_)]
            P2 = [a.b(TG * 128).rearrange("p (t i) -> p t i", i=128) for _ in range(NPB_)]
            gtmp = [a.b(256) for _ in range(2)]
            ntg = T // TG
            for tg in range(0 if 'G' in SKIP else ntg):
                s = tg % NPB_
                t0 = tg * TG
                iob = bc(iotab[:], [128, TG, 128], 1)
                A('dve', lambda e, s=s, t0=t0, iob=iob: e.tensor_tensor(out=P1[s], in0=iob, in1=bc(selT[:, 0, t0:t0 + TG], [128, TG, 128], 2), op=ALU.is_equal),
                  r=['selT', 'iotab'], w=['P1_%d' % s])
                A('dve', lambda e, s=s, t0=t0, iob=iob: e.tensor_tensor(out=P2[s], in0=iob, in1=bc(selT[:, 1, t0:t0 + TG], [128, TG, 128], 2), op=ALU.is_equal),
                  r=['selT', 'iotab'], w=['P2_%d' % s])
                A('pool', lambda e, s=s, t0=t0: e.tensor_tensor(out=P2[s], in0=P2[s], in1=bc(selT[:, 2, t0:t0 + TG], [128, TG, 128], 2), op=ALU.mult),
                  r=['selT', 'P2_%d' % s], w=['P2_%d' % s])
                for q in range(TG // 4):
                    pk = (tg * (TG // 4) + q) % 2
                    for tt in range(4):
                        t = q * 4 + tt
                        A('pe', lambda e, s=s, t=t, tt=tt, pk=pk: e.matmul(out=bank(pk)[:, tt * 128:(tt + 1) * 128], lhsT=P1[s][:, t, :], rhs=P2[s][:, t, :], start=True, stop=True),
                          r=['P1_%d' % s, 'P2_%d' % s], w=['pb%d' % pk])
                    tok0 = t0 + q * 4
                    A('act', lambda e, pk=pk, tok0=tok0: e.copy(out=G[:, :, tok0:tok0 + 4], in_=bank(pk).rearrange("p (t j) -> p j t", j=128)), r=['pb%d' % pk], w=['G'])
            for cg in range(0 if 'A' in SKIP else 128 // CG):
                i = next_slot()
                ws = wslot[i][:, 0:CG * D].rearrange("p (j x) -> p j x", x=D)
                A('sp', lambda e, cg=cg, ws=ws: e.dma_start(out=ws, in_=UT_s[cg * CG:(cg + 1) * CG].rearrange("j p x -> p j x")), r=['scr'], w=['ws%d' % i], dma='ws%d' % i)
                for jl in range(CG):
                    j = cg * CG + jl
                    pk = 2 + j % 2
                    ps = bank(pk)[:, 0:T]
                    for dc in range(16):
                        A('pe', lambda e, dc=dc, jl=jl, ps=ps, ws=ws: e.matmul(out=ps, lhsT=ws[:, jl, dc * 128:(dc + 1) * 128], rhs=hnT[:, dc, 0:T], start=(dc == 0), stop=(dc == 15)),
                          r=['hnT', 'ws%d' % i], w=['pb%d' % pk])
                    gs = j % 2
                    A('act', lambda e, ps=ps, gs=gs: e.activation(out=gtmp[gs][:, 0:T], in_=ps, func=AF.Gelu_apprx_tanh), r=['pb%d' % pk], w=['gtmp%d' % gs])
                    me = 'dve' if j % 2 == 0 else 'pool'
                    A(me, lambda e, j=j, gs=gs: e.tensor_tensor(out=G[:, j, 0:T], in0=G[:, j, 0:T], in1=gtmp[gs][:, 0:T], op=ALU.mult), r=['G', 'gtmp%d' % gs], w=['Gw%d' % j])
            allG = ['Gw%d' % j for j in range(128)]
            for dh in range(0 if 'B' in SKIP else 2):
                for cg in range(128 // CG):
                    i = next_slot()
                    ws = wslot[i][:, 0:CG * 1024].rearrange("p (j x) -> p j x", x=1024)
                    A('sp', lambda e, cg=cg, ws=ws, dh=dh: e.dma_start(out=ws, in_=VB_s[cg * CG:(cg + 1) * CG, :, dh * 1024:(dh + 1) * 1024].rearrange("j i x -> i j x")),
                      r=['scr'], w=['ws%d' % i], dma='ws%d' % i)
                    for jl in range(CG):
                        j = cg * CG + jl
                        for tb in range(ntb):
                            for ds in range(2):
                                pk = 4 + tb * 2 + ds
                                A('pe', lambda e, j=j, jl=jl, tb=tb, ds=ds, pk=pk, ws=ws: e.matmul(out=bank(pk), lhsT=G[:, j, tb * 128:(tb + 1) * 128], rhs=ws[:, jl, ds * 512:(ds + 1) * 512],
                                                                                                       start=(j == 0), stop=(j == 127)),
                                  r=(allG if j == 0 else ['Gw%d' % j]) + ['ws%d' % i], w=['pb%d' % pk])
                for tb in range(ntb):
                    for ds in range(2):
                        pk = 4 + tb * 2 + ds
                        c0 = dh * 1024 + ds * 512
                        A('dve', lambda e, tb=tb, pk=pk, c0=c0: e.tensor_tensor(out=xin[:, tb, c0:c0 + 512], in0=xin[:, tb, c0:c0 + 512], in1=bank(pk), op=ALU.add),
                          r=['pb%d' % pk, 'xin%d' % tb], w=['xin%d' % tb])
            if dbg and sb == 0:
                for tb in range(ntb):
                    A('sp', lambda e, tb=tb: e.dma_start(out=dh2_d[tb * 128:(tb + 1) * 128, :], in_=xin[:, tb, :]), r=['xin%d' % tb], dma='o_w')
            S.barrier()

            a = Ar()
            h3T = a.b(16 * 256).rearrange("p (a b) -> p a b", b=256)
            tmpb = a.b(D)
            junk = a.f(D)
            pb_ = a.b(256)
            pT_ = a.b(2 * 256).rearrange("p (a b) -> p a b", b=256)
            gate = a.f(2 * D).rearrange("p (a b) -> p a b", b=D)
            tmp2 = a.f(512)
            for tb in range(ntb):
                X = 'xin%d' % tb
                A('act', lambda e, tb=tb: e.activation(out=junk, in_=xin[:, tb, :], func=AF.Square, accum_out=stat[:, 24 + tb:25 + tb]), r=[X], w=['junk', 'st_e%d' % tb])
                rsqrt_ops(stat[:, 8 + tb:9 + tb], stat[:, 24 + tb:25 + tb], D, rr=['st_e%d' % tb], ww=['rstd3_%d' % tb])
                A('dve', lambda e, tb=tb: e.tensor_copy(out=tmpb, in_=xin[:, tb, :]), r=[X], w=['tmpb'])
                transposes16(tmpb, h3T, slice(tb * 128, (tb + 1) * 128), 0, ['tmpb'], ['h3T'], 'act')
                A('dve', lambda e, tb=tb: e.tensor_copy(out=pb_, in_=pin[:, tb, :]), r=['pin%d' % tb], w=['pb_'])
                for kc in range(2):
                    A('pe', lambda e, kc=kc: e.transpose(out=bankb(1)[:, kc * 128:(kc + 1) * 128], in_=pb_[:, kc * 128:(kc + 1) * 128], identity=identb[:]), r=['pb_', 'identb'], w=['pb1'])
                A('dve', lambda e, tb=tb: e.tensor_copy(out=pT_[:, :, tb * 128:(tb + 1) * 128], in_=bankb(1)[:, 0:256].rearrange("p (a b) -> p a b", b=128)), r=['pb1'], w=['pT_'])
            for si in range(4):
                ws, wr = load_w(gw_v, si * 512, 512)
                for tb in range(ntb):
                    pk = 2 + (si * 2 + tb) % 2
                    ps = bank(pk)
                    for dc in range(16):
                        A('pe', lambda e, dc=dc, tb=tb, ps=ps, ws=ws: e.matmul(out=ps, lhsT=h3T[:, dc, tb * 128:(tb + 1) * 128], rhs=ws[:, dc, :], start=(dc == 0), stop=(dc == 15)),
                          r=['h3T', wr], w=['pb%d' % pk])
                    A('act', lambda e, tb=tb, si=si, ps=ps: e.activation(out=gate[:, tb, si * 512:(si + 1) * 512], in_=ps, func=AF.Sigmoid, scale=stat[:, 8 + tb:9 + tb]),
                      r=['pb%d' % pk, 'rstd3_%d' % tb], w=['gate%d' % tb])
            for tb in range(ntb):
                for si in range(4):
                    pk = 4 + si
                    ps = bank(pk)
                    for kc in range(2):
                        A('pe', lambda e, kc=kc, tb=tb, si=si, ps=ps: e.matmul(out=ps, lhsT=pT_[:, kc, tb * 128:(tb + 1) * 128], rhs=plewb[:, kc, si * 512:(si + 1) * 512], start=(kc == 0), stop=(kc == 1)),
                          r=['pT_', 'plewb'], w=['pb%d' % pk])
                    A('dve', lambda e, tb=tb, si=si, ps=ps: e.tensor_tensor(out=tmp2, in0=ps, in1=gate[:, tb, si * 512:(si + 1) * 512], op=ALU.mult), r=['pb%d' % pk, 'gate%d' % tb], w=['tmp2'])
                    A('dve', lambda e, tb=tb, si=si: e.tensor_tensor(out=xin[:, tb, si * 512:(si + 1) * 512], in0=xin[:, tb, si * 512:(si + 1) * 512], in1=tmp2, op=ALU.add),
                      r=['tmp2', 'xin%d' % tb], w=['xin%d' % tb])
                A('sp', lambda e, tb=tb: e.dma_start(out=y_d[r0 + tb * 128:r0 + (tb + 1) * 128, :], in_=xin[:, tb, :]), r=['xin%d' % tb], dma='o_y%d' % tb)
            S.barrier()

        block = es.enter_context(nc.Block())
        S.emit(block)
    return nc


def _consts():
    c = {}
    c["ident"] = np.eye(128, dtype=np.float32)
    c["iota"] = np.tile(np.arange(128, dtype=np.float32)[None, :], (128, 1))
    s = np.arange(128)[:, None]
    q = np.arange(128)[None, :]
    mprev = np.where(s > q, 0.0, NEG)
    mcur = np.where(s <= q, 0.0, NEG)
    c["maskp"] = np.stack([mprev, mcur], axis=1).astype(np.float32)
    ms = np.full((128, 17, 128), NEG, dtype=np.float32)
    qb, ql = np.arange(128) // 8, np.arange(128) % 8
    for b in range(16):
        vis = (qb[None, :] == b) & (np.arange(128)[:, None] > ql[None, :])
        ms[:, b, :] = np.where(vis, 0.0, NEG)
    sb_, sl = np.arange(128) // 8, np.arange(128) % 8
    vis = (sb_[:, None] == qb[None, :]) & (sl[:, None] <= ql[None, :])
    ms[:, 16, :] = np.where(vis, 0.0, NEG)
    c["masks"] = ms
    c["tril"] = (np.arange(128)[None, :] <= np.arange(128)[:, None]).astype(np.float32)
    c["bdT"] = vis.astype(np.float32)
    c["e8"] = (np.arange(8)[:, None] == (np.arange(128) % 8)[None, :]).astype(np.float32)
    half = 8
    inv = np.power(np.float32(500000.0), -np.arange(half, dtype=np.float32) * np.float32(2.0) / np.float32(16)).astype(np.float32)
    pos_p = (np.arange(16)[None, :] * 128 + np.arange(128)[:, None]).astype(np.float32)
    ang = (pos_p[:, :, None] * inv[None, None, :]).astype(np.float32)
    c["csp"] = np.stack([np.cos(ang), np.sin(ang)], axis=2).astype(np.float32)
    pos_s = (16384 + np.arange(128) % 8).astype(np.float32)
    angs = (pos_s[:, None] * inv[None, :]).astype(np.float32)
    c["css"] = np.stack([np.cos(angs), np.sin(angs)], axis=1).astype(np.float32)
    return c


def make_in_maps(inputs, cores=range(NCORE)):
    f = lambda k: np.ascontiguousarray(np.asarray(inputs[k], dtype=np.float32)[0])
    shared = dict(
        w_in=f("w_in"), w_out=f("w_out"), wq=f("peer_wq"), gw=f("ple_gate_w"), plew=f("ple_w"),
        U=f("peer_u"), V=f("peer_v"), k1=f("peer_k1"), k2=f("peer_k2"),
        gffn=f("norm_ffn")[None, :], qn=f("q_norm")[None, :], kn=f("k_norm")[None, :], sgn=f("sgu_norm")[None, :],
        sinks=f("sinks")[None, :], sguw=f("sgu_w"), sgub=f("sgu_b"),
    )
    gout = np.concatenate([f("attn_out_norm"), f("sgu_out_norm")])
    cols = [f("norm_mix"), gout, f("norm_ffn"), f("ple_gate_norm")]
    shared["gcol"] = np.ascontiguousarray(np.stack([g.reshape(16, 128).T for g in cols], axis=1))
    shared.update(_consts())
    xp = np.asarray(inputs["x_prompt"], dtype=np.float32)
    xs = np.asarray(inputs["x_sample"], dtype=np.float32)
    pp = np.asarray(inputs["p_prompt"], dtype=np.float32)[0]
    ps = np.asarray(inputs["p_sample"], dtype=np.float32)[0]
    ck = np.asarray(inputs["cache_k"], dtype=np.float32)[0]
    cv = np.asarray(inputs["cache_v"], dtype=np.float32)[0]
    maps = []
    for c in cores:
        m = dict(shared)
        m["x"] = np.concatenate([xp[2 * c:2 * c + 2].reshape(4096, D), xs[16 * c:16 * c + 16].reshape(128, D)], axis=0)
        m["p"] = np.concatenate([pp[2 * c:2 * c + 2].reshape(4096, 256), ps[16 * c:16 * c + 16].reshape(128, 256)], axis=0)
        m["ck"] = np.ascontiguousarray(ck[16 * c:16 * c + 16].reshape(16, 128, 128))
        m["cv"] = np.ascontiguousarray(cv[16 * c:16 * c + 16].reshape(16, 128, 128))
        maps.append(m)
    return maps


def kernel(**inputs):
    nc = build_nc()
    maps = make_in_maps(inputs)
    res = run_bass_kernel_spmd(nc, maps, core_ids=list(range(NCORE)))
    R = res.results
    y = np.stack([r["y"] for r in R])
    y_prompt = y[:, :4096].reshape(16, 2048, D)
    y_sample = y[:, 4096:].reshape(128, 8, D)
    wkp = np.concatenate([r["wkp"] for r in R]).reshape(1, 16, 128, 2, 64)
    wvp = np.concatenate([r["wvp"] for r in R]).reshape(1, 16, 128, 2, 64)
    wks = np.concatenate([r["wks"] for r in R]).reshape(1, 128, 128, 2, 64)
    wvs = np.concatenate([r["wvs"] for r in R]).reshape(1, 128, 128, 2, 64)
    svs = np.concatenate([r["svs"] for r in R]).reshape(1, 128, 8, 8, 128)
    return (y_prompt.astype(np.float32), y_sample.astype(np.float32), wkp.astype(np.float32), wvp.astype(np.float32),
            wks.astype(np.float32), wvs.astype(np.float32), svs.astype(np.float32))
```

```python
import os
import types
import numpy as np
from contextlib import ExitStack
import concourse.bass as bass
import concourse.mybir as mybir
from concourse.bass_utils import run_bass_kernel_spmd

F32 = mybir.dt.float32
BF16 = mybir.dt.bfloat16
U32 = mybir.dt.uint32
ALU = mybir.AluOpType
AF = mybir.ActivationFunctionType
AX = mybir.AxisListType

D = 2048
NCORE = 8
NPB = 32
NROW = 4224
EPS = 1e-6
NEG = -30000.0
CG = 4
TG = 8
SAME_ENGINE_FIFO = False


def _freeze(fn):
    if fn.__closure__ is None:
        return fn
    cells = []
    for c in fn.__closure__:
        try:
            cells.append(types.CellType(c.cell_contents))
        except ValueError:
            cells.append(c)
    g = types.FunctionType(fn.__code__, fn.__globals__, fn.__name__, fn.__defaults__, tuple(cells))
    g.__kwdefaults__ = fn.__kwdefaults__
    return g


class Sch:
    CE = ['pe', 'act', 'dve', 'pool']

    def __init__(self, nc, es):
        self.nc, self.es = nc, es
        self.ops = {e: [] for e in self.CE + ['sp']}
        self.res = {}
        self.esem = {e: es.enter_context(nc.semaphore("s_" + e)) for e in self.CE}
        self.dsems = {}
        self.pend = {e: set() for e in self.CE + ['sp']}

    def _ds(self, name):
        if name not in self.dsems:
            self.dsems[name] = [self.es.enter_context(self.nc.semaphore("d_" + name)), 0]
        return self.dsems[name]

    def add(self, eng, fn, r=(), w=(), dma=None):
        idx = len(self.ops[eng])
        deps = set(self.pend[eng])
        self.pend[eng] = set()
        for x in r:
            st = self.res.get(x)
            if st and st[0] is not None:
                deps.add(st[0])
        for x in w:
            st = self.res.get(x)
            if st:
                if st[0] is not None:
                    deps.add(st[0])
                deps.update(st[1])
        if dma is not None:
            d = self._ds(dma)
            d[1] += 16
            tok = ('D', dma, d[1])
        else:
            tok = (eng, idx)
        for x in r:
            self.res.setdefault(x, [None, []])[1].append(tok)
        for x in w:
            self.res[x] = [tok, []]
        self.ops[eng].append(dict(fn=_freeze(fn), deps=deps, dma=dma, sig=False, cnt=0))
        return tok

    def barrier(self):
        toks = set()
        for e in self.CE:
            if self.ops[e]:
                toks.add((e, len(self.ops[e]) - 1))
        for name, d in self.dsems.items():
            if d[1] > 0:
                toks.add(('D', name, d[1]))
        for e in self.CE + ['sp']:
            self.pend[e] |= toks

    def emit(self, block):
        for e in self.ops:
            for op in self.ops[e]:
                for d in op['deps']:
                    if d[0] != 'D':
                        self.ops[d[0]][d[1]]['sig'] = True
        for e in self.CE:
            c = 0
            for op in self.ops[e]:
                if op['sig']:
                    c += 1
                op['cnt'] = c

        def mk(en):
            def body(eng):
                waited = {}
                for op in self.ops[en]:
                    need = {}
                    for d in op['deps']:
                        if d[0] == 'D':
                            sem, val = self.dsems[d[1]][0], d[2]
                        else:
                            if d[0] == en and (en == 'pe' or SAME_ENGINE_FIFO):
                                continue
                            sem, val = self.esem[d[0]], self.ops[d[0]][d[1]]['cnt']
                        if need.get(sem.num, (None, 0))[1] < val:
                            need[sem.num] = (sem, val)
                    for num, (sem, val) in need.items():
                        if waited.get(num, 0) < val:
                            eng.wait_ge(sem, val)
                            waited[num] = val
                    ins = op['fn'](eng)
                    if op['dma'] is not None:
                        ins.then_inc(self.dsems[op['dma']][0], 16)
                    elif op['sig']:
                        ins.then_inc(self.esem[en], 1)
                if en == 'sp':
                    for name, d in self.dsems.items():
                        if d[1] > 0:
                            eng.wait_ge(d[0], d[1])
            return body
        block.sync(mk('sp'))
        block.tensor(mk('pe'))
        block.scalar(mk('act'))
        block.vector(mk('dve'))
        block.gpsimd(mk('pool'))


def bc(ap, shape, axis):
    return ap.unsqueeze(axis).to_broadcast(shape)


def build_nc(sb_list=None, dbg=False):
    SKIP = os.environ.get('KSKIP', '')
    if sb_list is None:
        sb_list = list(range(17))
    nc = bass.Bass("TRN2", target_bir_lowering=False)

    def din(name, shape, dt=F32):
        return nc.dram_tensor(name, list(shape), dt, kind="ExternalInput").ap()

    def dout(name, shape):
        return nc.dram_tensor(name, list(shape), F32, kind="ExternalOutput").ap()

    x_d = din("x", [NROW, D]); p_d = din("p", [NROW, 256])
    ck_d = din("ck", [16, 128, 128]); cv_d = din("cv", [16, 128, 128])
    win_d = din("w_in", [D, 3328]); wout_d = din("w_out", [D, D]); wq_d = din("wq", [D, D])
    gw_d = din("gw", [D, D]); plew_d = din("plew", [256, D])
    U_d = din("U", [16384, D]); V_d = din("V", [16384, D])
    k1_d = din("k1", [128, 128]); k2_d = din("k2", [128, 128])
    gcol_d = din("gcol", [128, 4, 16])
    gffn_d = din("gffn", [1, D])
    qn_d = din("qn", [1, 64]); kn_d = din("kn", [1, 64]); sgn_d = din("sgn", [1, 128]); sink_d = din("sinks", [1, 16])
    sguw_d = din("sguw", [8, 128, 128]); sgub_d = din("sgub", [8, 128])
    ident_d = din("ident", [128, 128]); iota_d = din("iota", [128, 128])
    mp_d = din("maskp", [128, 2, 128]); ms_d = din("masks", [128, 17, 128])
    tril_d = din("tril", [128, 128]); bdT_d = din("bdT", [128, 128]); e8_d = din("e8", [8, 128])
    csp_d = din("csp", [128, 16, 2, 8]); css_d = din("css", [128, 2, 8])

    y_d = dout("y", [NROW, D])
    wkp_d = dout("wkp", [2, 128, 128]); wvp_d = dout("wvp", [2, 128, 128])
    wks_d = dout("wks", [16, 128, 128]); wvs_d = dout("wvs", [16, 128, 128])
    svs_d = dout("svs", [128, 1024])
    if dbg:
        dh_d = dout("dbg_h", [256, D]); dh2_d = dout("dbg_h2", [256, D])

    win_s = nc.dram_tensor("win_s", [D, 3328], BF16).ap()
    wout_s = nc.dram_tensor("wout_s", [D, D], BF16).ap()
    wq_s = nc.dram_tensor("wq_s", [D, D], BF16).ap()
    gw_s = nc.dram_tensor("gw_s", [D, D], BF16).ap()
    UT_s = nc.dram_tensor("UT_s", [128, 128, D], BF16).ap()
    VB_s = nc.dram_tensor("VB_s", [128, 128, D], BF16).ap()

    es = ExitStack()
    with es:
        def sb_(name, shape, dt):
            return es.enter_context(nc.sbuf_tensor("t_" + name, list(shape), dt))

        identb = sb_("identb", [128, 128], BF16)
        iotab = sb_("iotab", [128, 128], BF16)
        iota16 = sb_("iota16", [128, 16], F32)
        maskp = sb_("maskp", [128, 2, 128], BF16)
        masks = sb_("masks", [128, 17, 128], BF16)
        sguTp = sb_("sguTp", [128, 8, 128], BF16)
        sguTs = sb_("sguTs", [128, 8, 128], BF16)
        bTp = sb_("bTp", [128, 8], F32)
        bTs = sb_("bTs", [128, 8], F32)
        qg = sb_("qg", [128, 64], F32); kg = sb_("kg", [128, 64], F32); sgn = sb_("sgn", [128, 128], F32)
        esink = sb_("esink", [128, 16], F32)
        k12T = sb_("k12T", [128, 2, 128], BF16)
        csp = sb_("csp", [128, 16, 2, 8], F32); css = sb_("css", [128, 2, 8], F32)
        gcol = sb_("gcol", [128, 4, 16], F32)
        plewb = sb_("plewb", [128, 2, D], BF16)
        kTc = sb_("kTc", [128, 16, 128], BF16)
        vaugc = sb_("vaugc", [128, 16, 2, 65], BF16)
        kTr = sb_("kTr", [128, 3, 128], BF16)
        vaugr = sb_("vaugr", [128, 3, 2, 65], BF16)
        xin = sb_("xin", [128, 2, D], F32)
        pin = sb_("pin", [128, 2, 256], F32)
        stat = sb_("stat", [128, 64], F32)
        mhalf = sb_("mhalf", [128, 16], F32)
        wslot = [sb_("wslot%d" % i, [128, 8192], BF16) for i in range(3)]
        ARENA = 24000
        arena = sb_("arena", [128, ARENA], F32)
        psum = es.enter_context(nc.psum_tensor("psum", [128, 4096], F32))

        def bank(k, n=1):
            return psum[:, k * 512:(k + n) * 512]

        def bankb(k, n=1):
            return bank(k, n).bitcast(BF16)

        class Ar:
            def __init__(s):
                s.o = 0

            def f(s, n):
                a = arena[:, s.o:s.o + n]
                s.o += n
                assert s.o <= ARENA, s.o
                return a

            def b(s, n):
                m = (n + 1) // 2
                a = arena[:, s.o:s.o + m].bitcast(BF16)
                s.o += m
                assert s.o <= ARENA, s.o
                return a

        S = Sch(nc, es)
        A = S.add
        wsl_i = [0]

        def next_slot():
            i = wsl_i[0] % 3
            wsl_i[0] += 1
            return i

        def rsqrt_ops(dst, src, n, eng='dve', rr=(), ww=()):
            A(eng, lambda e: e.tensor_scalar(out=dst, in0=src, scalar1=1.0 / n, scalar2=EPS, op0=ALU.mult, op1=ALU.add), r=rr, w=ww)
            k_ = dst.shape[-1]
            A('pool', lambda e: e.tensor_tensor(out=dst, in0=dst, in1=mhalf[:, 0:k_], op=ALU.pow), r=list(ww) + ['mhalf'], w=ww)

        def transposes16(src_bf, dstT, tcols, pbk, rr, ww, ev_eng):
            pb = bankb(pbk, 2)
            for dc in range(16):
                A('pe', lambda e, dc=dc: e.transpose(out=pb[:, dc * 128:(dc + 1) * 128], in_=src_bf[:, dc * 128:(dc + 1) * 128], identity=identb[:]),
                  r=rr + ['identb'], w=['pb%d' % pbk, 'pb%d' % (pbk + 1)])
            A(ev_eng, lambda e: (e.tensor_copy(out=dstT[:, :, tcols], in_=pb.rearrange("p (a b) -> p a b", b=128)) if ev_eng != 'act'
                                 else e.copy(out=dstT[:, :, tcols], in_=pb.rearrange("p (a b) -> p a b", b=128))),
              r=['pb%d' % pbk, 'pb%d' % (pbk + 1)], w=ww)

        a = Ar()
        A('pool', lambda e: e.memset(mhalf[:], -0.5), w=['mhalf'])
        t_f = a.f(128 * 17)
        A('sp', lambda e: e.dma_start(out=t_f[:, 0:128], in_=ident_d), w=['t_f'], dma='c0')
        A('dve', lambda e: e.tensor_copy(out=identb[:], in_=t_f[:, 0:128]), r=['t_f'], w=['identb'])
        A('sp', lambda e: e.dma_start(out=t_f[:, 0:128], in_=iota_d), w=['t_f'], dma='c0')
        A('dve', lambda e: e.tensor_copy(out=iotab[:], in_=t_f[:, 0:128]), r=['t_f'], w=['iotab'])
        A('dve', lambda e: e.tensor_copy(out=iota16[:], in_=t_f[:, 0:16]), r=['t_f'], w=['iota16'])
        A('sp', lambda e: e.dma_start(out=t_f[:, 0:256].rearrange("p (a b) -> p a b", b=128), in_=mp_d), w=['t_f'], dma='c0')
        A('dve', lambda e: e.tensor_copy(out=maskp[:], in_=t_f[:, 0:256].rearrange("p (a b) -> p a b", b=128)), r=['t_f'], w=['maskp'])
        A('sp', lambda e: e.dma_start(out=t_f[:, 0:128 * 17].rearrange("p (a b) -> p a b", b=128), in_=ms_d), w=['t_f'], dma='c0')
        A('dve', lambda e: e.tensor_copy(out=masks[:], in_=t_f[:, 0:128 * 17].rearrange("p (a b) -> p a b", b=128)), r=['t_f'], w=['masks'])
        for (dst, src, tg_) in ((qg, qn_d, 'qg'), (kg, kn_d, 'kg'), (sgn, sgn_d, 'sgn')):
            A('sp', lambda e, dst=dst, src=src: e.dma_start(out=dst[:], in_=src.partition_broadcast(128)), w=[tg_], dma='c1')
        A('sp', lambda e: e.dma_start(out=esink[:], in_=sink_d.partition_broadcast(128)), w=['esink'], dma='c2')
        A('act', lambda e: e.activation(out=esink[:], in_=esink[:], func=AF.Exp), r=['esink'], w=['esink'])
        A('sp', lambda e: e.dma_start(out=csp[:], in_=csp_d), w=['csp'], dma='c1')
        A('sp', lambda e: e.dma_start(out=css[:], in_=css_d), w=['css'], dma='c1')
        A('sp', lambda e: e.dma_start(out=gcol[:], in_=gcol_d), w=['gcol'], dma='c1')
        A('sp', lambda e: e.dma_start(out=bTp[:], in_=sgub_d.rearrange("h t -> t h"), allow_slow_non_contiguous=True), w=['bTp'], dma='c1')
        for b in range(16):
            A('sp', lambda e, b=b: e.dma_start(out=bTs[b * 8:(b + 1) * 8, :], in_=sgub_d[:, 0:8].rearrange("h t -> t h"), allow_slow_non_contiguous=True), w=['bTs'], dma='c1')
        t_b = a.b(128 * 8)
        for i, kd in enumerate((k1_d, k2_d)):
            A('sp', lambda e, kd=kd: e.dma_start(out=t_f[:, 0:128], in_=kd), w=['t_f'], dma='c0')
            A('dve', lambda e: e.tensor_copy(out=t_b[:, 0:128], in_=t_f[:, 0:128]), r=['t_f'], w=['t_b'])
            A('pe', lambda e: e.transpose(out=bankb(0)[:, 0:128], in_=t_b[:, 0:128], identity=identb[:]), r=['t_b', 'identb'], w=['pb0'])
            A('dve', lambda e, i=i: e.tensor_copy(out=k12T[:, i, :], in_=bankb(0)[:, 0:128]), r=['pb0'], w=['k12T'])
        trilf = a.f(128)
        bdTf = a.f(128)
        A('sp', lambda e: e.dma_start(out=trilf, in_=tril_d), w=['trilf'], dma='c3')
        A('sp', lambda e: e.dma_start(out=bdTf, in_=bdT_d), w=['bdTf'], dma='c4')
        for h in range(8):
            A('sp', lambda e, h=h: e.dma_start(out=t_f[:, 0:128], in_=sguw_d[h]), w=['t_f'], dma='c0')
            A('dve', lambda e: e.tensor_tensor(out=t_b[:, 0:128], in0=t_f[:, 0:128], in1=trilf, op=ALU.mult), r=['t_f', 'trilf'], w=['t_b'])
            A('pe', lambda e: e.transpose(out=bankb(0)[:, 0:128], in_=t_b[:, 0:128], identity=identb[:]), r=['t_b', 'identb'], w=['pb0'])
            A('dve', lambda e, h=h: e.tensor_copy(out=sguTp[:, h, :], in_=bankb(0)[:, 0:128]), r=['pb0'], w=['sguTp'])
        e8f = a.f(128); e8b = a.b(128); w8f = a.f(128); w8b = a.b(128)
        A('sp', lambda e: e.dma_start(out=e8f[0:8, :], in_=e8_d), w=['e8f'], dma='c5')
        A('dve', lambda e: e.tensor_copy(out=e8b[0:8, :], in_=e8f[0:8, :]), r=['e8f'], w=['e8b'])
        for h in range(8):
            A('sp', lambda e, h=h: e.dma_start(out=w8f[0:8, :].rearrange("p (a b) -> p a b", b=8),
                                               in_=bc(sguw_d[h, 0:8, 0:8], [8, 16, 8], 1)), w=['w8f'], dma='c9')
            A('dve', lambda e: e.tensor_copy(out=w8b[0:8, :], in_=w8f[0:8, :]), r=['w8f'], w=['w8b'])
            A('pe', lambda e: e.matmul(out=bank(2)[:, 0:128], lhsT=w8b[0:8, :], rhs=e8b[0:8, :], start=True, stop=True), r=['w8b', 'e8b'], w=['pb2'])
            A('dve', lambda e, h=h: e.tensor_tensor(out=sguTs[:, h, :], in0=bank(2)[:, 0:128], in1=bdTf, op=ALU.mult), r=['pb2', 'bdTf'], w=['sguTs'])
        plf = a.f(D)
        for kc in range(2):
            A('sp', lambda e, kc=kc: e.dma_start(out=plf, in_=plew_d[kc * 128:(kc + 1) * 128, :]), w=['plf'], dma='c7')
            A('dve', lambda e, kc=kc: e.tensor_copy(out=plewb[:, kc, :], in_=plf), r=['plf'], w=['plewb'])
        ckf = a.f(16 * 128).rearrange("p (a b) -> p a b", b=128)
        ckb = a.b(16 * 128).rearrange("p (a b) -> p a b", b=128)
        A('sp', lambda e: e.dma_start(out=ckf, in_=ck_d.rearrange("b s c -> s b c")), w=['ckf'], dma='c8')
        A('dve', lambda e: e.tensor_copy(out=ckb, in_=ckf), r=['ckf'], w=['ckb'])
        for b in range(16):
            A('pe', lambda e, b=b: e.transpose(out=bankb(0)[:, (b % 8) * 128:(b % 8 + 1) * 128], in_=ckb[:, b, :], identity=identb[:]), r=['ckb', 'identb'], w=['pb0'])
            if b % 8 == 7:
                A('dve', lambda e, b=b: e.tensor_copy(out=kTc[:, b - 7:b + 1, :], in_=bankb(0).rearrange("p (a b) -> p a b", b=128)), r=['pb0'], w=['kTc'])
        A('sp', lambda e: e.dma_start(out=ckf, in_=cv_d.rearrange("b s c -> s b c")), w=['ckf'], dma='c8')
        A('dve', lambda e: e.memset(vaugc[:], 1.0), w=['vaugc'])
        A('dve', lambda e: e.tensor_copy(out=vaugc[:, :, :, 0:64], in_=ckf.rearrange("p a (k c) -> p a k c", c=64)), r=['ckf'], w=['vaugc'])
        A('dve', lambda e: e.memset(vaugr[:], 1.0), w=['vaugr'])
        A('sp', lambda e: e.dma_start(out=wks_d[:, 0:120, :], in_=ck_d[:, 8:128, :]), dma='o_w')
        A('sp', lambda e: e.dma_start(out=wvs_d[:, 0:120, :], in_=cv_d[:, 8:128, :]), dma='o_w')
        S.barrier()

        a = Ar()
        NBC = 3
        cvf = [a.f(3328) for _ in range(NBC)]
        cvb = [a.b(3328) for _ in range(NBC)]
        jobs = []
        for (src, dst, ncol, gi) in ((win_d, win_s, 3328, 0), (wout_d, wout_s, D, 1), (wq_d, wq_s, D, 2), (gw_d, gw_s, D, 3)):
            for dc in range(16):
                jobs.append((src, dst, ncol, gi, dc))

        def cv_store(k):
            src, dst, ncol, gi, dc = jobs[k]
            s = k % NBC
            A('sp', lambda e: e.dma_start(out=dst[dc * 128:(dc + 1) * 128, :], in_=cvb[s][:, 0:ncol]), r=['cvb%d' % s], dma='cs%d' % s)

        for k, (src, dst, ncol, gi, dc) in enumerate(jobs):
            s = k % NBC
            A('sp', lambda e: e.dma_start(out=cvf[s][:, 0:ncol], in_=src[dc * 128:(dc + 1) * 128, :]), w=['cvf%d' % s], dma='cv%d' % s)
            if k >= 2:
                cv_store(k - 2)
            if dc % 2:
                A('act', lambda e: e.activation(out=cvb[s][:, 0:ncol], in_=cvf[s][:, 0:ncol], func=AF.Copy, scale=gcol[:, gi, dc:dc + 1]),
                  r=['cvf%d' % s, 'gcol'], w=['cvb%d' % s])
            else:
                A('dve', lambda e: e.tensor_scalar(out=cvb[s][:, 0:ncol], in0=cvf[s][:, 0:ncol], scalar1=gcol[:, gi, dc:dc + 1], scalar2=None, op0=ALU.mult),
                  r=['cvf%d' % s, 'gcol'], w=['cvb%d' % s])
        cv_store(len(jobs) - 2)
        cv_store(len(jobs) - 1)
        S.barrier()
        a = Ar()
        NBU = 3
        uf = [a.f(D) for _ in range(NBU)]
        ub = [a.b(D) for _ in range(NBU)]
        ut = [a.b(D) for _ in range(NBU)]
        vf_ = [a.f(D) for _ in range(NBU)]
        vb_ = [a.b(D) for _ in range(NBU)]
        gfr2 = a.f(D)
        A('sp', lambda e: e.dma_start(out=gfr2, in_=gffn_d.partition_broadcast(128)), w=['gfr2'], dma='c6')
        Uv = U_d.rearrange("(i j) d -> j i d", j=128)
        Vv = V_d.rearrange("(i j) d -> j i d", j=128)

        def uv_store(j):
            s = j % NBU
            A('sp', lambda e: e.dma_start(out=UT_s[j], in_=ut[s]), r=['ut%d' % s], dma='us%d' % s)
            A('sp', lambda e: e.dma_start(out=VB_s[j], in_=vb_[s]), r=['vb%d' % s], dma='vs%d' % s)

        for j in range(128):
            s = j % NBU
            A('sp', lambda e: e.dma_start(out=uf[s], in_=Uv[j]), w=['uf%d' % s], dma='uf%d' % s)
            A('sp', lambda e: e.dma_start(out=vf_[s], in_=Vv[j]), w=['vf%d' % s], dma='vf%d' % s)
            if j >= 2:
                uv_store(j - 2)
            A('dve', lambda e: e.tensor_tensor(out=ub[s], in0=uf[s], in1=gfr2, op=ALU.mult), r=['uf%d' % s, 'gfr2'], w=['ub%d' % s])
            pk = 2 * s
            pb = bankb(pk, 2)
            for dc in range(16):
                A('pe', lambda e, dc=dc: e.transpose(out=pb[:, dc * 128:(dc + 1) * 128], in_=ub[s][:, dc * 128:(dc + 1) * 128], identity=identb[:]),
                  r=['ub%d' % s, 'identb'], w=['pb%d' % pk, 'pb%d' % (pk + 1)])
            A('act', lambda e: e.copy(out=ut[s], in_=pb), r=['pb%d' % pk, 'pb%d' % (pk + 1)], w=['ut%d' % s])
            A('act', lambda e: e.copy(out=vb_[s], in_=vf_[s]), r=['vf%d' % s], w=['vb%d' % s])
        uv_store(126)
        uv_store(127)
        S.barrier()

        win_v = win_s.rearrange("(dc p) c -> p dc c", p=128)
        wout_v = wout_s.rearrange("(dc p) c -> p dc c", p=128)
        wq_v = wq_s.rearrange("(dc p) c -> p dc c", p=128)
        gw_v = gw_s.rearrange("(dc p) c -> p dc c", p=128)

        def load_w(view, c0, w):
            i = next_slot()
            ws = wslot[i][:, 0:16 * w].rearrange("p (a b) -> p a b", b=w)
            A('sp', lambda e: e.dma_start(out=ws, in_=view[:, :, c0:c0 + w]), r=['scr'], w=['ws%d' % i], dma='ws%d' % i)
            return ws, 'ws%d' % i

        for sb in sb_list:
            sample = (sb == 16)
            ntb = 1 if sample else 2
            T = 128 * ntb
            r0 = sb * 256
            a = Ar()
            xT = a.b(16 * 256).rearrange("p (a b) -> p a b", b=256)
            mixT_f = a.f(16 * 128)
            mixT = mixT_f.bitcast(BF16).rearrange("p (a b) -> p a b", b=256)
            tmpb = a.b(D)
            junk = a.f(D)
            qf = a.f(2 * 1024).rearrange("p (a b) -> p a b", b=1024)
            qb = a.b(2 * 1024).rearrange("p (a b) -> p a b", b=1024)
            kvf = a.f(2 * 256).rearrange("p (a b) -> p a b", b=256)
            kb = a.b(2 * 128).rearrange("p (a b) -> p a b", b=128)
            su = a.f(2 * 1024).rearrange("p (a b) -> p a b", b=1024)
            svf = a.f(2 * 1024).rearrange("p (a b) -> p a b", b=1024)
            svb = a.b(2 * 1024).rearrange("p (a b) -> p a b", b=1024)
            tmpf = a.f(1024)
            rt = a.f(128)
            tk = [a.f(128) for _ in range(2)]
            qT = a.b(2 * 1024).rearrange("p (t h q) -> p t h q", t=2, h=8)
            pT = [a.b(16 * 128).rearrange("p (h q) -> p h q", q=128) for _ in range(2)]
            attnf = a.f(1024)
            mixed_f = a.f(D)
            mixed = mixed_f.bitcast(BF16).rearrange("p (a b) -> p a b", b=D)
            tq = [mixT_f[:, tb * 1024:(tb + 1) * 1024] for tb in range(2)]
            TQT = ['tq%d' % t for t in range(2)] + ['tq%d_%d' % (t, i) for t in range(2) for i in range(4)]
            tsv = [mixed_f[:, tb * 1024:(tb + 1) * 1024] for tb in range(2)]

            for tb in range(ntb):
                A('sp', lambda e, tb=tb: e.dma_start(out=xin[:, tb, :], in_=x_d[r0 + tb * 128:r0 + (tb + 1) * 128, :]), w=['xin%d' % tb], dma='xi%d' % tb)
                A('sp', lambda e, tb=tb: e.dma_start(out=pin[:, tb, :], in_=p_d[r0 + tb * 128:r0 + (tb + 1) * 128, :]), w=['pin%d' % tb], dma='pi%d' % tb)
            for tb in range(ntb):
                X = 'xin%d' % tb
                A('act', lambda e, tb=tb: e.activation(out=junk, in_=xin[:, tb, :], func=AF.Square, accum_out=stat[:, 16 + tb:17 + tb]), r=[X], w=['junk', 'st_a%d' % tb])
                rsqrt_ops(stat[:, tb:tb + 1], stat[:, 16 + tb:17 + tb], D, rr=['st_a%d' % tb], ww=['rstd1_%d' % tb])
                A('dve', lambda e, tb=tb: e.tensor_copy(out=tmpb, in_=xin[:, tb, :]), r=[X], w=['tmpb'])
                transposes16(tmpb, xT, slice(tb * 128, (tb + 1) * 128), 0, ['tmpb'], ['xT'], 'dve')
            slices = [(0, 512, 'q'), (512, 512, 'q'), (1024, 256, 'kv'), (2304, 512, 'sv'), (2816, 512, 'sv'), (1280, 512, 'su'), (1792, 512, 'su')]
            for si, (c0, w, kind) in enumerate(slices):
                ws, wr = load_w(win_v, c0, w)
                for tb in range(ntb):
                    pk = 2 + (si * 2 + tb) % 2
                    ps = bank(pk)[:, 0:w]
                    for dc in range(16):
                        A('pe', lambda e, dc=dc, tb=tb, ps=ps, ws=ws: e.matmul(out=ps, lhsT=xT[:, dc, tb * 128:(tb + 1) * 128], rhs=ws[:, dc, :], start=(dc == 0), stop=(dc == 15)),
                          r=['xT', wr], w=['pb%d' % pk])
                    rs = stat[:, tb:tb + 1]
                    if kind == 'q':
                        dst, fn, wn = qf[:, tb, c0:c0 + w], AF.Copy, 'qf%d' % tb
                    elif kind == 'kv':
                        dst, fn, wn = kvf[:, tb, :], AF.Copy, 'kvf%d' % tb
                    elif kind == 'su':
                        dst, fn, wn = su[:, tb, c0 - 1280:c0 - 1280 + w], AF.Gelu_apprx_tanh, 'su%d' % tb
                    else:
                        dst, fn, wn = svf[:, tb, c0 - 2304:c0 - 2304 + w], AF.Gelu_apprx_tanh, 'svf%d' % tb
                    A('act', lambda e, dst=dst, fn=fn, ps=ps, rs=rs: e.activation(out=dst, in_=ps, func=fn, scale=rs), r=['pb%d' % pk, 'rstd1_%d' % tb], w=[wn])

            TF = ['tmpf', 'tmpf1', 'tmpf2', 'tmpf3']

            def headnorm_ops(buf, nh, hd, gain, gtag, btag, scr, stag, rtc, rtag, extra_w=()):
                v3 = buf.rearrange("p (h c) -> p h c", c=hd)
                sc_ = scr[:, 0:nh * hd]
                t3 = sc_.rearrange("p (h c) -> p h c", c=hd)
                return [
                    ('dve', lambda e: e.tensor_tensor(out=sc_, in0=buf, in1=buf, op=ALU.mult), [btag], [stag] + list(extra_w)),
                    ('dve', lambda e: e.tensor_reduce(out=rtc, in_=t3, axis=AX.X, op=ALU.add), [stag], [rtag]),
                    ('dve', lambda e: e.tensor_scalar(out=rtc, in0=rtc, scalar1=1.0 / hd, scalar2=EPS, op0=ALU.mult, op1=ALU.add), [rtag], [rtag]),
                    ('pool', lambda e: e.tensor_tensor(out=rtc, in0=rtc, in1=mhalf[:, 0:nh], op=ALU.pow), [rtag, 'mhalf'], [rtag]),
                    ('dve', lambda e: e.tensor_tensor(out=v3, in0=v3, in1=bc(rtc, [128, nh, hd], 2), op=ALU.mult), [btag, rtag], [btag]),
                    ('dve', lambda e: e.tensor_tensor(out=v3, in0=v3, in1=bc(gain, [128, nh, hd], 1), op=ALU.mult), [btag, gtag], [btag]),
                ]

            def rope_ops(buf, nh, cs, btag, scr, stag):
                v3 = buf.rearrange("p (h c) -> p h c", c=64)
                x1, x2 = v3[:, :, 0:8], v3[:, :, 8:16]
                cb = bc(cs[:, 0, :], [128, nh, 8], 1)
                sn = bc(cs[:, 1, :], [128, nh, 8], 1)
                n8 = nh * 8
                t = [scr[:, i * n8:(i + 1) * n8].rearrange("p (h c) -> p h c", c=8) for i in range(4)]
                st = [stag + '_%d' % i for i in range(4)]
                return [
                    ('dve', lambda e: e.tensor_tensor(out=t[0], in0=x1, in1=cb, op=ALU.mult), [btag, stag], [st[0]]),
                    ('dve', lambda e: e.tensor_tensor(out=t[1], in0=x2, in1=sn, op=ALU.mult), [btag, stag], [st[1]]),
                    ('dve', lambda e: e.tensor_tensor(out=t[2], in0=x2, in1=cb, op=ALU.mult), [btag, stag], [st[2]]),
                    ('dve', lambda e: e.tensor_tensor(out=t[3], in0=x1, in1=sn, op=ALU.mult), [btag, stag], [st[3]]),
                    ('dve', lambda e: e.tensor_tensor(out=x1, in0=t[0], in1=t[1], op=ALU.subtract), st, [btag]),
                    ('dve', lambda e: e.tensor_tensor(out=x2, in0=t[2], in1=t[3], op=ALU.add), st, [btag]),
                ]

            chains, delays = [], []
            for tb in range(ntb):
                gb = sb * 2 + tb
                blk = gb % 16
                cs = css[:] if sample else csp[:, blk, :, :]
                slot = gb % 3
                QF, KVF, SVF = 'qf%d' % tb, 'kvf%d' % tb, 'svf%d' % tb
                cq = headnorm_ops(qf[:, tb, :], 16, 64, qg[:], 'qg', QF, tq[tb], 'tq%d' % tb, rt[:, tb * 16:tb * 16 + 16], 'rtq%d' % tb)
                cq += rope_ops(qf[:, tb, :], 16, cs, QF, tq[tb], 'tq%d' % tb)
                cq.append(('dve', lambda e, tb=tb: e.tensor_copy(out=qb[:, tb, :].rearrange("p (h g c) -> p g h c", g=2, c=64),
                                                                 in_=qf[:, tb, :].rearrange("p (g h c) -> p g h c", g=2, c=64)), [QF], ['qb%d' % tb]))
                ck = headnorm_ops(kvf[:, tb, 0:128], 2, 64, kg[:], 'kg', KVF, tk[tb], 'tk%d' % tb, rt[:, 32 + tb * 2:34 + tb * 2], 'rtk%d' % tb)
                ck += rope_ops(kvf[:, tb, 0:128], 2, cs, KVF, tk[tb], 'tk%d' % tb)
                ck.append(('dve', lambda e, tb=tb: e.tensor_copy(out=kb[:, tb, :], in_=kvf[:, tb, 0:128]), [KVF], ['kb%d' % tb]))
                ck.append(('dve', lambda e, tb=tb, slot=slot: e.tensor_copy(out=vaugr[:, slot, :, 0:64], in_=kvf[:, tb, 128:256].rearrange("p (k c) -> p k c", c=64)),
                           [KVF], ['vaug%d' % slot]))
                csv_ = headnorm_ops(svf[:, tb, :], 8, 128, sgn[:], 'sgn', SVF, tsv[tb], 'mixed%d' % tb, rt[:, 40 + tb * 8:48 + tb * 8], 'rtsv%d' % tb)
                csv_.append(('pool', lambda e, tb=tb: e.tensor_copy(out=svb[:, tb, :], in_=svf[:, tb, :]), [SVF], ['svb%d' % tb]))
                chains += [cq, ck, csv_]
                delays += [0, 4, 9]
            rnd = 0
            while any(chains):
                for ch, dl in zip(chains, delays):
                    if ch and rnd >= dl:
                        eng_, fn_, r_, w_ = ch.pop(0)
                        A(eng_, fn_, r=r_, w=w_)
                rnd += 1

            for tb in range(ntb):
                gb = sb * 2 + tb
                blk = gb % 16
                slot = gb % 3
                if sample:
                    for b in range(16):
                        A('sp', lambda e, b=b: e.dma_start(out=wks_d[b, 120:128, :], in_=kvf[b * 8:(b + 1) * 8, 0, 0:128]), r=['kvf0'], dma='o_w')
                        A('sp', lambda e, b=b: e.dma_start(out=wvs_d[b, 120:128, :], in_=kvf[b * 8:(b + 1) * 8, 0, 128:256]), r=['kvf0'], dma='o_w')
                    A('sp', lambda e: e.dma_start(out=svs_d, in_=svf[:, 0, :]), r=['svf0'], dma='o_w')
                elif blk == 15:
                    seq = gb // 16
                    A('sp', lambda e, tb=tb, seq=seq: e.dma_start(out=wkp_d[seq], in_=kvf[:, tb, 0:128]), r=['kvf%d' % tb], dma='o_w')
                    A('sp', lambda e, tb=tb, seq=seq: e.dma_start(out=wvp_d[seq], in_=kvf[:, tb, 128:256]), r=['kvf%d' % tb], dma='o_w')
                pbq = bankb(0)
                for h in range(8):
                    A('pe', lambda e, h=h, tb=tb: e.transpose(out=pbq[:, h * 128:(h + 1) * 128], in_=qb[:, tb, h * 128:(h + 1) * 128], identity=identb[:]),
                      r=['qb%d' % tb, 'identb'], w=['pb0'])
                A('act', lambda e, tb=tb: e.copy(out=qT[:, tb, :, :], in_=pbq.rearrange("p (h q) -> p h q", q=128)), r=['pb0'], w=['qT%d' % tb])
                A('pe', lambda e, tb=tb: e.transpose(out=bankb(1)[:, 0:128], in_=kb[:, tb, :], identity=identb[:]), r=['kb%d' % tb, 'identb'], w=['pb1'])
                A('act', lambda e, slot=slot: e.copy(out=kTr[:, slot, :], in_=bankb(1)[:, 0:128]), r=['pb1'], w=['kT%d' % slot])

                sT = sguTs if sample else sguTp
                bT = bTs if sample else bTp
                zp = bank(0, 2)
                for h in range(8):
                    A('pe', lambda e, h=h, tb=tb, sT=sT: e.matmul(out=zp[:, h * 128:(h + 1) * 128], lhsT=sT[:, h, :], rhs=svb[:, tb, h * 128:(h + 1) * 128], start=True, stop=True),
                      r=['svb%d' % tb, 'sguTp', 'sguTs'], w=['pb0', 'pb1'])
                t3 = tmpf.rearrange("p (h c) -> p h c", c=128)
                A('dve', lambda e, bT=bT: e.tensor_tensor(out=t3, in0=zp.rearrange("p (h c) -> p h c", c=128), in1=bc(bT[:], [128, 8, 128], 2), op=ALU.add),
                  r=['pb0', 'pb1', 'bTp', 'bTs'], w=TF)
                A('dve', lambda e, tb=tb: e.tensor_tensor(out=tmpf, in0=tmpf, in1=su[:, tb, :], op=ALU.mult), r=TF + ['su%d' % tb], w=TF)
                A('act', lambda e, tb=tb: e.activation(out=junk[:, 0:1024], in_=tmpf, func=AF.Square, accum_out=stat[:, 20 + tb:21 + tb]), r=TF, w=['junk', 'st_c%d' % tb])
                rsqrt_ops(stat[:, 4 + tb:5 + tb], stat[:, 20 + tb:21 + tb], 1024, rr=['st_c%d' % tb], ww=['rstd_s%d' % tb])
                A('dve', lambda e, tb=tb: e.tensor_scalar(out=mixed[:, tb, 1024:2048], in0=tmpf, scalar1=stat[:, 4 + tb:5 + tb], scalar2=None, op0=ALU.mult),
                  r=TF + ['rstd_s%d' % tb], w=['mixed%d' % tb])

                if sample:
                    kbl = [(kTc[:, b, :], vaugc[:, b, :, :], masks[:, b, :], ['kTc'], ['vaugc']) for b in range(16)]
                    kbl.append((kTr[:, slot, :], vaugr[:, slot, :, :], masks[:, 16, :], ['kT%d' % slot], ['vaug%d' % slot]))
                else:
                    kbl = []
                    if blk > 0:
                        ps_ = (gb - 1) % 3
                        kbl.append((kTr[:, ps_, :], vaugr[:, ps_, :, :], maskp[:, 0, :], ['kT%d' % ps_], ['vaug%d' % ps_]))
                    kbl.append((kTr[:, slot, :], vaugr[:, slot, :, :], maskp[:, 1, :], ['kT%d' % slot], ['vaug%d' % slot]))
                nkb = len(kbl)
                for ki, (kT_, va_, mk_, kres, vres) in enumerate(kbl):
                    pti = ki % 2
                    for kh in range(2):
                        for half in range(2):
                            pk = 2 + (kh * 2 + half) % 2
                            ps = bank(pk)
                            A('pe', lambda e, kh=kh, half=half, ps=ps, kT_=kT_, tb=tb: e.matmul(
                                out=ps, lhsT=kT_[kh * 64:(kh + 1) * 64, :], rhs=qT[kh * 64:(kh + 1) * 64, tb, half * 4:(half + 1) * 4, :], start=True, stop=False),
                              r=kres + ['qT%d' % tb], w=['pb%d' % pk])
                            for hh in range(4):
                                A('pe', lambda e, ps=ps, mk_=mk_, hh=hh: e.matmul(out=ps[:, hh * 128:(hh + 1) * 128], lhsT=identb[:], rhs=mk_, start=False, stop=True),
                                  r=['identb', 'maskp', 'masks'], w=['pb%d' % pk])
                            h0 = kh * 8 + half * 4
                            A('act', lambda e, ps=ps, pti=pti, h0=h0: e.activation(out=pT[pti][:, h0:h0 + 4, :], in_=ps.rearrange("p (h q) -> p h q", q=128), func=AF.Exp, scale=0.125),
                              r=['pb%d' % pk], w=['pT%d_%d' % (pti, h0)])
                    for h in range(16):
                        pk = 4 + h // 4
                        o = bank(pk)[:, (h % 4) * 128:(h % 4) * 128 + 65]
                        h0 = (h // 4) * 4
                        A('pe', lambda e, h=h, o=o, pti=pti, va_=va_, ki=ki: e.matmul(out=o, lhsT=pT[pti][:, h, :], rhs=va_[:, h // 8, :], start=(ki == 0 and h % 4 == 0), stop=(ki == nkb - 1)),
                          r=['pT%d_%d' % (pti, h0)] + vres, w=['pb%d' % pk])
                o4 = bank(4, 4).rearrange("p (h c) -> p h c", c=128)
                ob = ['pb4', 'pb5', 'pb6', 'pb7']
                A('dve', lambda e: e.tensor_tensor(out=rt[:, 64:80], in0=o4[:, :, 64], in1=esink[:], op=ALU.add), r=ob + ['esink'], w=['rt2'])
                A('dve', lambda e: e.reciprocal(out=rt[:, 64:80], in_=rt[:, 64:80]), r=['rt2'], w=['rt2'])
                A('dve', lambda e: e.tensor_tensor(out=attnf.rearrange("p (h c) -> p h c", c=64), in0=o4[:, :, 0:64], in1=bc(rt[:, 64:80], [128, 16, 64], 2), op=ALU.mult),
                  r=ob + ['rt2'], w=['attnf'])
                A('act', lambda e, tb=tb: e.activation(out=junk[:, 1024:2048], in_=attnf, func=AF.Square, accum_out=stat[:, 18 + tb:19 + tb]), r=['attnf'], w=['junkb', 'st_b%d' % tb])
                rsqrt_ops(stat[:, 2 + tb:3 + tb], stat[:, 18 + tb:19 + tb], 1024, rr=['st_b%d' % tb], ww=['rstd_a%d' % tb])
                A('dve', lambda e, tb=tb: e.tensor_scalar(out=mixed[:, tb, 0:1024], in0=attnf, scalar1=stat[:, 2 + tb:3 + tb], scalar2=None, op0=ALU.mult),
                  r=['attnf', 'rstd_a%d' % tb], w=['mixed%d' % tb])
            for tb in range(ntb):
                transposes16(mixed[:, tb, :], mixT, slice(tb * 128, (tb + 1) * 128), 0, ['mixed%d' % tb], ['mixT'] + TQT, 'act')
            for si in range(4):
                ws, wr = load_w(wout_v, si * 512, 512)
                for tb in range(ntb):
                    pk = 2 + (si * 2 + tb) % 2
                    ps = bank(pk)
                    for dc in range(16):
                        A('pe', lambda e, dc=dc, tb=tb, ps=ps, ws=ws: e.matmul(out=ps, lhsT=mixT[:, dc, tb * 128:(tb + 1) * 128], rhs=ws[:, dc, :], start=(dc == 0), stop=(dc == 15)),
                          r=['mixT', wr], w=['pb%d' % pk])
                    A('dve', lambda e, tb=tb, si=si, ps=ps: e.tensor_tensor(out=xin[:, tb, si * 512:(si + 1) * 512], in0=xin[:, tb, si * 512:(si + 1) * 512], in1=ps, op=ALU.add),
                      r=['pb%d' % pk, 'xin%d' % tb], w=['xin%d' % tb])
            if dbg and sb == 0:
                for tb in range(ntb):
                    A('sp', lambda e, tb=tb: e.dma_start(out=dh_d[tb * 128:(tb + 1) * 128, :], in_=xin[:, tb, :]), r=['xin%d' % tb], dma='o_w')
            S.barrier()

            a = Ar()
            hnT = a.b(16 * 256).rearrange("p (a b) -> p a b", b=256)
            selT = a.b(3 * 256).rearrange("p (a b) -> p a b", b=256)
            off_keep = a.o
            tmpb = a.b(D)
            junk = a.f(D)
            qTp = a.b(16 * 256).rearrange("p (g t) -> p g t", t=256)
            sc = a.f(D)
            work = a.f(D)
            vv = a.f(256)
            ixu = a.f(256).bitcast(U32)
            ixf = a.f(256)
            cand = a.f(D)
            cv_ = a.f(128); ciu = a.f(128).bitcast(U32); cif = a.f(128)
            k1f = a.f(128); k2f = a.f(128); ee = a.f(128)
            eq = a.f(D)
            sel = a.b(3 * 128).rearrange("p (a b) -> p a b", b=128)

            for tb in range(ntb):
                X = 'xin%d' % tb
                A('act', lambda e, tb=tb: e.activation(out=junk, in_=xin[:, tb, :], func=AF.Square, accum_out=stat[:, 22 + tb:23 + tb]), r=[X], w=['junk', 'st_d%d' % tb])
                rsqrt_ops(stat[:, 6 + tb:7 + tb], stat[:, 22 + tb:23 + tb], D, rr=['st_d%d' % tb], ww=['rstd2_%d' % tb])
                A('dve', lambda e, tb=tb: e.tensor_scalar(out=tmpb, in0=xin[:, tb, :], scalar1=stat[:, 6 + tb:7 + tb], scalar2=None, op0=ALU.mult), r=[X, 'rstd2_%d' % tb], w=['tmpb'])
                transposes16(tmpb, hnT, slice(tb * 128, (tb + 1) * 128), 0, ['tmpb'], ['hnT'], 'act')
            for si in range(4):
                ws, wr = load_w(wq_v, si * 512, 512)
                for gl in range(4):
                    g = si * 4 + gl
                    pk = 2 + g % 2
                    ps = bank(pk)[:, 0:T]
                    for dc in range(16):
                        A('pe', lambda e, dc=dc, gl=gl, ps=ps, ws=ws: e.matmul(out=ps, lhsT=ws[:, dc, gl * 128:(gl + 1) * 128], rhs=hnT[:, dc, 0:T], start=(dc == 0), stop=(dc == 15)),
                          r=['hnT', wr], w=['pb%d' % pk])
                    ev = 'act' if g % 2 else 'dve'
                    A(ev, lambda e, g=g, ps=ps, ev=ev: (e.copy(out=qTp[:, g, 0:T], in_=ps) if ev == 'act' else e.tensor_copy(out=qTp[:, g, 0:T], in_=ps)),
                      r=['pb%d' % pk], w=['qTp'])
            for tb in range(ntb):
                sp4 = bank(4, 4)
                for g in range(16):
                    A('pe', lambda e, g=g, tb=tb: e.matmul(out=sp4[:, g * 128:(g + 1) * 128], lhsT=qTp[:, g, tb * 128:(tb + 1) * 128], rhs=k12T[:, g % 2, :], start=True, stop=True),
                      r=['qTp', 'k12T'], w=['pb4', 'pb5', 'pb6', 'pb7'])
                A('act', lambda e: e.copy(out=sc, in_=sp4), r=['pb4', 'pb5', 'pb6', 'pb7'], w=['sc'])
                v3 = vv.rearrange("p (g k) -> p g k", k=16)
                i3 = ixu.rearrange("p (g k) -> p g k", k=16)
                SG = [sc[:, g * 128:(g + 1) * 128] for g in range(16)]
                WG = [work[:, g * 128:(g + 1) * 128] for g in range(16)]
                R0 = ['sc']
                for g in range(16):
                    A('dve', lambda e, g=g: e.max(out=v3[:, g, 0:8], in_=SG[g]), r=R0, w=['vv%d' % g])
                for g in range(16):
                    A('dve', lambda e, g=g: e.max_index(out=i3[:, g, 0:8], in_max=v3[:, g, 0:8], in_values=SG[g]), r=R0 + ['vv%d' % g], w=['ix%d' % g])
                for g in range(16):
                    A('dve', lambda e, g=g: e.match_replace(out=WG[g], in_to_replace=v3[:, g, 0:8], in_values=SG[g], imm_value=-1e30), r=R0 + ['vv%d' % g], w=['wk%d' % g])
                for g in range(16):
                    A('dve', lambda e, g=g: e.max(out=v3[:, g, 8:16], in_=WG[g]), r=['wk%d' % g], w=['vv%d' % g])
                for g in range(16):
                    A('dve', lambda e, g=g: e.max_index(out=i3[:, g, 8:16], in_max=v3[:, g, 8:16], in_values=WG[g]), r=['wk%d' % g, 'vv%d' % g], w=['ix%d' % g])
                allv = ['vv%d' % g for g in range(16)]
                alli = ['ix%d' % g for g in range(16)]
                A('dve', lambda e: e.tensor_copy(out=ixf, in_=ixu), r=alli, w=['ixf'])
                v4 = vv.rearrange("p (h s k) -> p h s k", s=2, k=16)
                c4 = cand.rearrange("p (h a b) -> p h a b", a=16, b=16)
                A('dve', lambda e: e.tensor_tensor(out=c4, in0=bc(v4[:, :, 0, :], [128, 8, 16, 16], 3), in1=bc(v4[:, :, 1, :], [128, 8, 16, 16], 2), op=ALU.add), r=allv, w=['cand'])
                cv3 = cv_.rearrange("p (h k) -> p h k", k=16)
                ci3 = ciu.rearrange("p (h k) -> p h k", k=16)
                CH = [cand[:, h * 256:(h + 1) * 256] for h in range(8)]
                WH = [work[:, h * 256:(h + 1) * 256] for h in range(8)]
                for h in range(8):
                    A('dve', lambda e, h=h: e.max(out=cv3[:, h, 0:8], in_=CH[h]), r=['cand'], w=['cv%d' % h])
                for h in range(8):
                    A('dve', lambda e, h=h: e.max_index(out=ci3[:, h, 0:8], in_max=cv3[:, h, 0:8], in_values=CH[h]), r=['cand', 'cv%d' % h], w=['ci%d' % h])
                for h in range(8):
                    A('dve', lambda e, h=h: e.match_replace(out=WH[h], in_to_replace=cv3[:, h, 0:8], in_values=CH[h], imm_value=-1e30),
                      r=['cand', 'cv%d' % h], w=['wk%d' % (2 * h), 'wk%d' % (2 * h + 1)])
                for h in range(8):
                    A('dve', lambda e, h=h: e.max(out=cv3[:, h, 8:16], in_=WH[h]), r=['wk%d' % (2 * h)], w=['cv%d' % h])
                for h in range(8):
                    A('dve', lambda e, h=h: e.max_index(out=ci3[:, h, 8:16], in_max=cv3[:, h, 8:16], in_values=WH[h]), r=['wk%d' % (2 * h), 'cv%d' % h], w=['ci%d' % h])
                allc = ['cv%d' % h for h in range(8)]
                allci = ['ci%d' % h for h in range(8)]
                e3 = ee.rearrange("p (h k) -> p h k", k=16)
                A('dve', lambda e: e.tensor_tensor(out=e3, in0=cv3, in1=bc(cv3[:, :, 0], [128, 8, 16], 2), op=ALU.subtract), r=allc, w=['ee'])
                A('act', lambda e: e.activation(out=ee, in_=ee, func=AF.Exp), r=['ee'], w=['ee'])
                A('dve', lambda e: e.tensor_reduce(out=rt[:, 32:40], in_=e3, axis=AX.X, op=ALU.add), r=['ee'], w=['rt3'])
                A('dve', lambda e: e.reciprocal(out=rt[:, 32:40], in_=rt[:, 32:40]), r=['rt3'], w=['rt3'])
                A('dve', lambda e: e.tensor_tensor(out=sel[:, 2, :].rearrange("p (h k) -> p h k", k=16), in0=e3, in1=bc(rt[:, 32:40], [128, 8, 16], 2), op=ALU.mult),
                  r=['ee', 'rt3'], w=['sel'])
                cifu = cif.bitcast(U32)
                A('dve', lambda e: e.tensor_scalar(out=cifu, in0=ciu, scalar1=15, scalar2=None, op0=ALU.bitwise_and), r=allci, w=['cif'])
                A('dve', lambda e: e.tensor_copy(out=k2f, in_=cifu), r=['cif'], w=['k2f'])
                A('dve', lambda e: e.tensor_scalar(out=cifu, in0=ciu, scalar1=4, scalar2=None, op0=ALU.logical_shift_right), r=allci + ['k2f'], w=['cif'])
                A('dve', lambda e: e.tensor_copy(out=k1f, in_=cifu), r=['cif'], w=['k1f'])
                q4 = eq.rearrange("p (h a b) -> p h a b", a=16, b=16)
                if4 = ixf.rearrange("p (h s k) -> p h s k", s=2, k=16)
                for side, kf_ in ((0, k1f), (1, k2f)):
                    kk = kf_.rearrange("p (h k) -> p h k", k=16)
                    A('dve', lambda e, kk=kk: e.tensor_tensor(out=q4, in0=bc(kk, [128, 8, 16, 16], 3), in1=bc(bc(iota16[:], [128, 16, 16], 1), [128, 8, 16, 16], 1), op=ALU.is_equal),
                      r=['k1f', 'k2f', 'iota16'], w=['eq'])
                    A('dve', lambda e, side=side: e.tensor_tensor(out=q4, in0=q4, in1=bc(if4[:, :, side, :], [128, 8, 16, 16], 2), op=ALU.mult), r=['eq', 'ixf'], w=['eq'])
                    A('dve', lambda e: e.tensor_reduce(out=k1f if False else work[:, 0:128], in_=q4.rearrange("p h a b -> p (h a) b"), axis=AX.X, op=ALU.add), r=['eq'], w=['wk0'])
                    A('dve', lambda e, side=side: e.tensor_copy(out=sel[:, side, :], in_=work[:, 0:128]), r=['wk0'], w=['sel'])
                for i in range(3):
                    A('pe', lambda e, i=i: e.transpose(out=bankb(0)[:, i * 128:(i + 1) * 128], in_=sel[:, i, :], identity=identb[:]), r=['sel', 'identb'], w=['pb0'])
                A('act', lambda e, tb=tb: e.copy(out=selT[:, :, tb * 128:(tb + 1) * 128], in_=bankb(0)[:, 0:384].rearrange("p (a b) -> p a b", b=128)), r=['pb0'], w=['selT'])
            S.barrier()

            a = Ar()
            a.o = off_keep
            G = a.b(128 * 256).rearrange("p (j t) -> p j t", t=256)
            NPB_ = 4
            P1 = [a.b(TG * 128).rearrange("p (t i) -> p t i", i=128) for _ in range(NPB_)]
            P2 = [a.b(TG * 128).rearrange("p (t i) -> p t i", i=128) for _ in range(NPB_)]
            gtmp = [a.b(256) for _ in range(2)]
            ntg = T // TG
            for tg in range(0 if 'G' in SKIP else ntg):
                s = tg % NPB_
                t0 = tg * TG
                iob = bc(iotab[:], [128, TG, 128], 1)
                A('dve', lambda e, s=s, t0=t0, iob=iob: e.tensor_tensor(out=P1[s], in0=iob, in1=bc(selT[:, 0, t0:t0 + TG], [128, TG, 128], 2), op=ALU.is_equal),
                  r=['selT', 'iotab'], w=['P1_%d' % s])
                A('dve', lambda e, s=s, t0=t0, iob=iob: e.tensor_tensor(out=P2[s], in0=iob, in1=bc(selT[:, 1, t0:t0 + TG], [128, TG, 128], 2), op=ALU.is_equal),
                  r=['selT', 'iotab'], w=['P2_%d' % s])
                A('dve', lambda e, s=s, t0=t0: e.tensor_tensor(out=P2[s], in0=P2[s], in1=bc(selT[:, 2, t0:t0 + TG], [128, TG, 128], 2), op=ALU.mult),
                  r=['selT', 'P2_%d' % s], w=['P2_%d' % s])
                for q in range(TG // 4):
                    pk = (tg * (TG // 4) + q) % 2
                    for tt in range(4):
                        t = q * 4 + tt
                        A('pe', lambda e, s=s, t=t, tt=tt, pk=pk: e.matmul(out=bank(pk)[:, tt * 128:(tt + 1) * 128], lhsT=P1[s][:, t, :], rhs=P2[s][:, t, :], start=True, stop=True),
                          r=['P1_%d' % s, 'P2_%d' % s], w=['pb%d' % pk])
                    tok0 = t0 + q * 4
                    A('act', lambda e, pk=pk, tok0=tok0: e.copy(out=G[:, :, tok0:tok0 + 4], in_=bank(pk).rearrange("p (t j) -> p j t", j=128)), r=['pb%d' % pk], w=['G'])
            for cg in range(0 if 'A' in SKIP else 128 // CG):
                i = next_slot()
                ws = wslot[i][:, 0:CG * D].rearrange("p (j x) -> p j x", x=D)
                A('sp', lambda e, cg=cg, ws=ws: e.dma_start(out=ws, in_=UT_s[cg * CG:(cg + 1) * CG].rearrange("j p x -> p j x")), r=['scr'], w=['ws%d' % i], dma='ws%d' % i)
                for jl in range(CG):
                    j = cg * CG + jl
                    pk = 2 + j % 2
                    ps = bank(pk)[:, 0:T]
                    for dc in range(16):
                        A('pe', lambda e, dc=dc, jl=jl, ps=ps, ws=ws: e.matmul(out=ps, lhsT=ws[:, jl, dc * 128:(dc + 1) * 128], rhs=hnT[:, dc, 0:T], start=(dc == 0), stop=(dc == 15)),
                          r=['hnT', 'ws%d' % i], w=['pb%d' % pk])
                    gs = j % 2
                    A('act', lambda e, ps=ps, gs=gs: e.activation(out=gtmp[gs][:, 0:T], in_=ps, func=AF.Gelu_apprx_tanh), r=['pb%d' % pk], w=['gtmp%d' % gs])
                    me = 'dve' if j % 2 == 0 else 'pool'
                    A(me, lambda e, j=j, gs=gs: e.tensor_tensor(out=G[:, j, 0:T], in0=G[:, j, 0:T], in1=gtmp[gs][:, 0:T], op=ALU.mult), r=['G', 'gtmp%d' % gs], w=['Gw%d' % j])
            allG = ['Gw%d' % j for j in range(128)]
            for dh in range(0 if 'B' in SKIP else 2):
                for cg in range(128 // CG):
                    i = next_slot()
                    ws = wslot[i][:, 0:CG * 1024].rearrange("p (j x) -> p j x", x=1024)
                    A('sp', lambda e, cg=cg, ws=ws, dh=dh: e.dma_start(out=ws, in_=VB_s[cg * CG:(cg + 1) * CG, :, dh * 1024:(dh + 1) * 1024].rearrange("j i x -> i j x")),
                      r=['scr'], w=['ws%d' % i], dma='ws%d' % i)
                    for jl in range(CG):
                        j = cg * CG + jl
                        for tb in range(ntb):
                            for ds in range(2):
                                pk = 4 + tb * 2 + ds
                                A('pe', lambda e, j=j, jl=jl, tb=tb, ds=ds, pk=pk, ws=ws: e.matmul(out=bank(pk), lhsT=G[:, j, tb * 128:(tb + 1) * 128], rhs=ws[:, jl, ds * 512:(ds + 1) * 512],
                                                                                                       start=(j == 0), stop=(j == 127)),
                                  r=(allG if j == 0 else ['Gw%d' % j]) + ['ws%d' % i], w=['pb%d' % pk])
                for tb in range(ntb):
                    for ds in range(2):
                        pk = 4 + tb * 2 + ds
                        c0 = dh * 1024 + ds * 512
                        A('dve', lambda e, tb=tb, pk=pk, c0=c0: e.tensor_tensor(out=xin[:, tb, c0:c0 + 512], in0=xin[:, tb, c0:c0 + 512], in1=bank(pk), op=ALU.add),
                          r=['pb%d' % pk, 'xin%d' % tb], w=['xin%d' % tb])
            if dbg and sb == 0:
                for tb in range(ntb):
                    A('sp', lambda e, tb=tb: e.dma_start(out=dh2_d[tb * 128:(tb + 1) * 128, :], in_=xin[:, tb, :]), r=['xin%d' % tb], dma='o_w')
            S.barrier()

            a = Ar()
            h3T = a.b(16 * 256).rearrange("p (a b) -> p a b", b=256)
            tmpb = a.b(D)
            junk = a.f(D)
            pb_ = a.b(256)
            pT_ = a.b(2 * 256).rearrange("p (a b) -> p a b", b=256)
            gate = a.f(2 * D).rearrange("p (a b) -> p a b", b=D)
            tmp2 = a.f(512)
            for tb in range(ntb):
                X = 'xin%d' % tb
                A('act', lambda e, tb=tb: e.activation(out=junk, in_=xin[:, tb, :], func=AF.Square, accum_out=stat[:, 24 + tb:25 + tb]), r=[X], w=['junk', 'st_e%d' % tb])
                rsqrt_ops(stat[:, 8 + tb:9 + tb], stat[:, 24 + tb:25 + tb], D, rr=['st_e%d' % tb], ww=['rstd3_%d' % tb])
                A('dve', lambda e, tb=tb: e.tensor_copy(out=tmpb, in_=xin[:, tb, :]), r=[X], w=['tmpb'])
                transposes16(tmpb, h3T, slice(tb * 128, (tb + 1) * 128), 0, ['tmpb'], ['h3T'], 'act')
                A('dve', lambda e, tb=tb: e.tensor_copy(out=pb_, in_=pin[:, tb, :]), r=['pin%d' % tb], w=['pb_'])
                for kc in range(2):
                    A('pe', lambda e, kc=kc: e.transpose(out=bankb(1)[:, kc * 128:(kc + 1) * 128], in_=pb_[:, kc * 128:(kc + 1) * 128], identity=identb[:]), r=['pb_', 'identb'], w=['pb1'])
                A('dve', lambda e, tb=tb: e.tensor_copy(out=pT_[:, :, tb * 128:(tb + 1) * 128], in_=bankb(1)[:, 0:256].rearrange("p (a b) -> p a b", b=128)), r=['pb1'], w=['pT_'])
            for si in range(4):
                ws, wr = load_w(gw_v, si * 512, 512)
                for tb in range(ntb):
                    pk = 2 + (si * 2 + tb) % 2
                    ps = bank(pk)
                    for dc in range(16):
                        A('pe', lambda e, dc=dc, tb=tb, ps=ps, ws=ws: e.matmul(out=ps, lhsT=h3T[:, dc, tb * 128:(tb + 1) * 128], rhs=ws[:, dc, :], start=(dc == 0), stop=(dc == 15)),
                          r=['h3T', wr], w=['pb%d' % pk])
                    A('act', lambda e, tb=tb, si=si, ps=ps: e.activation(out=gate[:, tb, si * 512:(si + 1) * 512], in_=ps, func=AF.Sigmoid, scale=stat[:, 8 + tb:9 + tb]),
                      r=['pb%d' % pk, 'rstd3_%d' % tb], w=['gate%d' % tb])
            for tb in range(ntb):
                for si in range(4):
                    pk = 4 + si
                    ps = bank(pk)
                    for kc in range(2):
                        A('pe', lambda e, kc=kc, tb=tb, si=si, ps=ps: e.matmul(out=ps, lhsT=pT_[:, kc, tb * 128:(tb + 1) * 128], rhs=plewb[:, kc, si * 512:(si + 1) * 512], start=(kc == 0), stop=(kc == 1)),
                          r=['pT_', 'plewb'], w=['pb%d' % pk])
                    A('dve', lambda e, tb=tb, si=si, ps=ps: e.tensor_tensor(out=tmp2, in0=ps, in1=gate[:, tb, si * 512:(si + 1) * 512], op=ALU.mult), r=['pb%d' % pk, 'gate%d' % tb], w=['tmp2'])
                    A('dve', lambda e, tb=tb, si=si: e.tensor_tensor(out=xin[:, tb, si * 512:(si + 1) * 512], in0=xin[:, tb, si * 512:(si + 1) * 512], in1=tmp2, op=ALU.add),
                      r=['tmp2', 'xin%d' % tb], w=['xin%d' % tb])
                A('sp', lambda e, tb=tb: e.dma_start(out=y_d[r0 + tb * 128:r0 + (tb + 1) * 128, :], in_=xin[:, tb, :]), r=['xin%d' % tb], dma='o_y%d' % tb)
            S.barrier()

        block = es.enter_context(nc.Block())
        S.emit(block)
    return nc


def _consts():
    c = {}
    c["ident"] = np.eye(128, dtype=np.float32)
    c["iota"] = np.tile(np.arange(128, dtype=np.float32)[None, :], (128, 1))
    s = np.arange(128)[:, None]
    q = np.arange(128)[None, :]
    mprev = np.where(s > q, 0.0, NEG)
    mcur = np.where(s <= q, 0.0, NEG)
    c["maskp"] = np.stack([mprev, mcur], axis=1).astype(np.float32)
    ms = np.full((128, 17, 128), NEG, dtype=np.float32)
    qb, ql = np.arange(128) // 8, np.arange(128) % 8
    for b in range(16):
        vis = (qb[None, :] == b) & (np.arange(128)[:, None] > ql[None, :])
        ms[:, b, :] = np.where(vis, 0.0, NEG)
    sb_, sl = np.arange(128) // 8, np.arange(128) % 8
    vis = (sb_[:, None] == qb[None, :]) & (sl[:, None] <= ql[None, :])
    ms[:, 16, :] = np.where(vis, 0.0, NEG)
    c["masks"] = ms
    c["tril"] = (np.arange(128)[None, :] <= np.arange(128)[:, None]).astype(np.float32)
    c["bdT"] = vis.astype(np.float32)
    c["e8"] = (np.arange(8)[:, None] == (np.arange(128) % 8)[None, :]).astype(np.float32)
    half = 8
    inv = np.power(np.float32(500000.0), -np.arange(half, dtype=np.float32) * np.float32(2.0) / np.float32(16)).astype(np.float32)
    pos_p = (np.arange(16)[None, :] * 128 + np.arange(128)[:, None]).astype(np.float32)
    ang = (pos_p[:, :, None] * inv[None, None, :]).astype(np.float32)
    c["csp"] = np.stack([np.cos(ang), np.sin(ang)], axis=2).astype(np.float32)
    pos_s = (16384 + np.arange(128) % 8).astype(np.float32)
    angs = (pos_s[:, None] * inv[None, :]).astype(np.float32)
    c["css"] = np.stack([np.cos(angs), np.sin(angs)], axis=1).astype(np.float32)
    return c


def make_in_maps(inputs, cores=range(NCORE)):
    f = lambda k: np.ascontiguousarray(np.asarray(inputs[k], dtype=np.float32)[0])
    shared = dict(
        w_in=f("w_in"), w_out=f("w_out"), wq=f("peer_wq"), gw=f("ple_gate_w"), plew=f("ple_w"),
        U=f("peer_u"), V=f("peer_v"), k1=f("peer_k1"), k2=f("peer_k2"),
        gffn=f("norm_ffn")[None, :], qn=f("q_norm")[None, :], kn=f("k_norm")[None, :], sgn=f("sgu_norm")[None, :],
        sinks=f("sinks")[None, :], sguw=f("sgu_w"), sgub=f("sgu_b"),
    )
    gout = np.concatenate([f("attn_out_norm"), f("sgu_out_norm")])
    cols = [f("norm_mix"), gout, f("norm_ffn"), f("ple_gate_norm")]
    shared["gcol"] = np.ascontiguousarray(np.stack([g.reshape(16, 128).T for g in cols], axis=1))
    shared.update(_consts())
    xp = np.asarray(inputs["x_prompt"], dtype=np.float32)
    xs = np.asarray(inputs["x_sample"], dtype=np.float32)
    pp = np.asarray(inputs["p_prompt"], dtype=np.float32)[0]
    ps = np.asarray(inputs["p_sample"], dtype=np.float32)[0]
    ck = np.asarray(inputs["cache_k"], dtype=np.float32)[0]
    cv = np.asarray(inputs["cache_v"], dtype=np.float32)[0]
    maps = []
    for c in cores:
        m = dict(shared)
        m["x"] = np.concatenate([xp[2 * c:2 * c + 2].reshape(4096, D), xs[16 * c:16 * c + 16].reshape(128, D)], axis=0)
        m["p"] = np.concatenate([pp[2 * c:2 * c + 2].reshape(4096, 256), ps[16 * c:16 * c + 16].reshape(128, 256)], axis=0)
        m["ck"] = np.ascontiguousarray(ck[16 * c:16 * c + 16].reshape(16, 128, 128))
        m["cv"] = np.ascontiguousarray(cv[16 * c:16 * c + 16].reshape(16, 128, 128))
        maps.append(m)
    return maps


def kernel(**inputs):
    nc = build_nc()
    maps = make_in_maps(inputs)
    res = run_bass_kernel_spmd(nc, maps, core_ids=list(range(NCORE)))
    R = res.results
    y = np.stack([r["y"] for r in R])
    y_prompt = y[:, :4096].reshape(16, 2048, D)
    y_sample = y[:, 4096:].reshape(128, 8, D)
    wkp = np.concatenate([r["wkp"] for r in R]).reshape(1, 16, 128, 2, 64)
    wvp = np.concatenate([r["wvp"] for r in R]).reshape(1, 16, 128, 2, 64)
    wks = np.concatenate([r["wks"] for r in R]).reshape(1, 128, 128, 2, 64)
    wvs = np.concatenate([r["wvs"] for r in R]).reshape(1, 128, 128, 2, 64)
    svs = np.concatenate([r["svs"] for r in R]).reshape(1, 128, 8, 8, 128)
    return (y_prompt.astype(np.float32), y_sample.astype(np.float32), wkp.astype(np.float32), wvp.astype(np.float32),
            wks.astype(np.float32), wvs.astype(np.float32), svs.astype(np.float32))
```
